# Optimizing a Trainium2 kernel written in Bass

```python
import math
import jax, jax.numpy as jnp
from jax import lax
import numpy as np

D_MODEL = 2048
BATCH = 4
SEQ = 2048
DEPTH = 2

PLE_DIM = 256
HEAD_DIM = 64
GROUP_W = D_MODEL // 4
FOX_HEADS = GROUP_W // HEAD_DIM
DIFF_HEADS = GROUP_W // (2 * HEAD_DIM)
POOL_GROUPS = 4
POOL_CH = GROUP_W // POOL_GROUPS
POOL_WINDOWS = (2, 4, 8, 16)
SWA_HEADS = GROUP_W // HEAD_DIM
SWA_KV_HEADS = 2
SWA_GROUP = SWA_HEADS // SWA_KV_HEADS
SWA_WINDOW = 128
Q_BLOCK = 128
MIX_W = 4 * GROUP_W
IN_SIZES = (GROUP_W, GROUP_W, GROUP_W, FOX_HEADS,
            GROUP_W, GROUP_W, GROUP_W,
            GROUP_W,
            SWA_HEADS * HEAD_DIM, SWA_KV_HEADS * HEAD_DIM, SWA_KV_HEADS * HEAD_DIM)
IN_W = sum(IN_SIZES)
REL_BUCKETS = 32
REL_MAX_DIST = 128
REL_HEADS = DIFF_HEADS + SWA_HEADS
MOE_GROUPS = 4
EXPERTS_PER_GROUP = 4
N_EXPERTS = MOE_GROUPS * EXPERTS_PER_GROUP
TOP_K_IN_GROUP = 2
D_EXPERT = D_MODEL // 4
ALPHA = (2 * DEPTH) ** 0.25
BETA = (8 * DEPTH) ** -0.25
LN_EPS = 1e-5

kernel_name = "hymba_style_fox_diff_pool_swa_hmoe"


def _layer_norm(x, g, b):
    xf = x.astype(jnp.float32)
    mu = xf.mean(-1, keepdims=True)
    var = jnp.square(xf - mu).mean(-1, keepdims=True)
    return ((xf - mu) * lax.rsqrt(var + LN_EPS) * g + b).astype(x.dtype)


def _rms_norm(x, g):
    xf = x.astype(jnp.float32)
    return (xf * lax.rsqrt(jnp.mean(xf * xf, -1, keepdims=True) + LN_EPS) * g).astype(x.dtype)


def _t5_bucket(n):
    max_exact = REL_BUCKETS // 2
    nf = jnp.maximum(n, 1).astype(jnp.float32)
    large = max_exact + (jnp.log(nf / max_exact) / math.log(REL_MAX_DIST / max_exact)
                         * (REL_BUCKETS - max_exact)).astype(jnp.int32)
    large = jnp.minimum(large, REL_BUCKETS - 1)
    return jnp.where(n < max_exact, n, large)


def _split_cols(proj):
    bounds, acc = [], 0
    for s in IN_SIZES[:-1]:
        acc += s
        bounds.append(acc)
    return jnp.split(proj, bounds, axis=-1)


def _sweep(block_fn, seq):
    out = lax.map(block_fn, jnp.arange(seq // Q_BLOCK) * Q_BLOCK)
    nb, b, qb, h, dv = out.shape
    return jnp.moveaxis(out, 0, 1).reshape(b, nb * qb, h, dv)


def _forgetting_attention(q, k, v, f_logit):
    _, S_, _, d = q.shape
    c = jnp.cumsum(jax.nn.log_sigmoid(f_logit.astype(jnp.float32)), axis=1)
    c_t = jnp.transpose(c, (0, 2, 1))
    kpos = jnp.arange(S_)
    scale = d ** -0.5

    def block(start):
        qb = lax.dynamic_slice_in_dim(q, start, Q_BLOCK, axis=1)
        cq = lax.dynamic_slice_in_dim(c_t, start, Q_BLOCK, axis=2)
        logits = jnp.einsum('bqhd,bkhd->bhqk', qb, k, preferred_element_type=jnp.float32) * scale
        logits = logits + cq[..., :, None] - c_t[..., None, :]
        qpos = start + jnp.arange(Q_BLOCK)
        mask = kpos[None, :] <= qpos[:, None]
        probs = jax.nn.softmax(jnp.where(mask, logits, -jnp.inf), axis=-1)
        return jnp.einsum('bhqk,bkhd->bqhd', probs.astype(v.dtype), v)

    return _sweep(block, S_)


def _diff_attention(q, k, v, rel_bias, lam, lam_init, sub_g):
    _, S_, _, _, d = q.shape
    kpos = jnp.arange(S_)
    scale = d ** -0.5

    def block(start):
        qb = lax.dynamic_slice_in_dim(q, start, Q_BLOCK, axis=1)
        logits = jnp.einsum('bqhmd,bkhmd->bhmqk', qb, k, preferred_element_type=jnp.float32) * scale
        dist = (start + jnp.arange(Q_BLOCK))[:, None] - kpos[None, :]
        bias = jnp.transpose(rel_bias[jnp.maximum(dist, 0)], (2, 0, 1))
        logits = logits + bias[None, :, None].astype(jnp.float32)
        probs = jax.nn.softmax(jnp.where(dist >= 0, logits, -jnp.inf), axis=-1)
        attn = probs[:, :, 0] - lam * probs[:, :, 1]
        return jnp.einsum('bhqk,bkhe->bqhe', attn.astype(v.dtype), v)

    o = _sweep(block, S_)
    o = _rms_norm(o, sub_g) * (1.0 - lam_init)
    return o.reshape(o.shape[0], S_, -1)


def _pool_mixer(u, pool_w, pool_scale):
    B_, S_, C = u.shape
    uf = u.astype(jnp.float32)
    cs = jnp.pad(jnp.cumsum(uf, axis=1), ((0, 0), (1, 0), (0, 0)))
    win = jnp.repeat(jnp.array(POOL_WINDOWS, dtype=jnp.int32), POOL_CH)
    t1 = jnp.arange(1, S_ + 1, dtype=jnp.int32)[:, None]
    lo = jnp.maximum(t1 - win[None, :], 0)
    lower = jnp.take_along_axis(cs, jnp.broadcast_to(lo[None], (B_, S_, C)), axis=1)
    cnt = jnp.minimum(t1, win[None, :]).astype(jnp.float32)
    pooled = ((cs[:, 1:] - lower) / cnt - uf).astype(u.dtype).reshape(B_, S_, POOL_GROUPS, POOL_CH)
    y = jnp.einsum('bsgc,gcd->bsgd', pooled, pool_w).reshape(B_, S_, C)
    return y * pool_scale


def _sliding_window_attention(q, k, v, rel_bias, sinks):
    B_, S_, Hq, d = q.shape
    nb = S_ // Q_BLOCK
    qb = q.reshape(B_, nb, Q_BLOCK, SWA_KV_HEADS, SWA_GROUP, d)

    def windows(t):
        tp = jnp.pad(t, ((0, 0), (Q_BLOCK, 0), (0, 0), (0, 0))).reshape(B_, nb + 1, Q_BLOCK, SWA_KV_HEADS, d)
        return jnp.concatenate([tp[:, :-1], tp[:, 1:]], axis=2)

    kw, vw = windows(k), windows(v)
    logits = jnp.einsum('bnqhgd,bnkhd->bnhgqk', qb, kw, preferred_element_type=jnp.float32) * d ** -0.5
    rel = jnp.arange(Q_BLOCK)[:, None] + Q_BLOCK - jnp.arange(2 * Q_BLOCK)[None, :]
    in_win = (rel >= 0) & (rel < SWA_WINDOW)
    kvalid = (jnp.arange(nb)[:, None] * Q_BLOCK - Q_BLOCK + jnp.arange(2 * Q_BLOCK)[None, :]) >= 0
    mask = in_win[None] & kvalid[:, None, :]
    bias = jnp.transpose(rel_bias[jnp.clip(rel, 0, SWA_WINDOW - 1)], (2, 0, 1))
    bias = bias.reshape(SWA_KV_HEADS, SWA_GROUP, Q_BLOCK, 2 * Q_BLOCK).astype(jnp.float32)
    logits = jnp.where(mask[None, :, None, None], logits + bias[None, None], -jnp.inf)
    sink = sinks.astype(jnp.float32).reshape(SWA_KV_HEADS, SWA_GROUP)[None, None, :, :, None, None]
    m = jnp.maximum(logits.max(-1, keepdims=True), sink)
    e = jnp.exp(logits - m)
    probs = e / (e.sum(-1, keepdims=True) + jnp.exp(sink - m))
    out = jnp.einsum('bnhgqk,bnkhd->bnqhgd', probs.astype(v.dtype), vw)
    return out.reshape(B_, S_, Hq * d)


def _hier_moe(h, rg_w, rg_b, re_w, re_b, w_gate, w_up, w_down):
    B_, S_, D = h.shape
    t = h.reshape(-1, D)
    g_probs = jax.nn.softmax((t @ rg_w).astype(jnp.float32) + rg_b, axis=-1)
    g_sel = jnp.argmax(g_probs, axis=-1)
    g_p = jnp.take_along_axis(g_probs, g_sel[:, None], axis=-1)
    e_logits = ((t @ re_w).astype(jnp.float32) + re_b).reshape(-1, MOE_GROUPS, EXPERTS_PER_GROUP)
    e_in = jnp.take_along_axis(e_logits, g_sel[:, None, None], axis=1)[:, 0]
    top_v, top_i = lax.top_k(e_in, TOP_K_IN_GROUP)
    w = jax.nn.softmax(top_v, axis=-1) * g_p
    ids = g_sel[:, None] * EXPERTS_PER_GROUP + top_i
    gates = jnp.sum(jax.nn.one_hot(ids, N_EXPERTS, dtype=jnp.float32) * w[..., None], axis=1)
    a = jnp.einsum('td,edf->tef', t, w_gate)
    u = jnp.einsum('td,edf->tef', t, w_up)
    hid = jax.nn.silu(a) * u * gates[..., None].astype(t.dtype)
    y = jnp.einsum('tef,efd->td', hid, w_down)
    return y.reshape(B_, S_, D)


def setup_inputs(seed: int = 0) -> dict:
    key = jax.random.key(seed)
    ks = jax.random.split(key, 27)
    n = lambda k, s: jax.random.normal(k, s, dtype=jnp.float32)
    L, D = DEPTH, D_MODEL
    return {
        "x": n(ks[0], (BATCH, SEQ, D)),
        "p": n(ks[1], (L, BATCH, SEQ, PLE_DIM)),
        "rel_table": 0.3 * n(ks[2], (REL_BUCKETS, REL_HEADS)),
        "w_in": n(ks[3], (L, D, IN_W)) * D ** -0.5,
        "b_f": 0.1 * n(ks[4], (L, FOX_HEADS)),
        "lam_q1": 0.1 * n(ks[5], (L, HEAD_DIM)),
        "lam_k1": 0.1 * n(ks[6], (L, HEAD_DIM)),
        "lam_q2": 0.1 * n(ks[7], (L, HEAD_DIM)),
        "lam_k2": 0.1 * n(ks[8], (L, HEAD_DIM)),
        "diff_norm_g": 1.0 + 0.02 * n(ks[9], (L, 2 * HEAD_DIM)),
        "pool_w": n(ks[10], (L, POOL_GROUPS, POOL_CH, POOL_CH)) * POOL_CH ** -0.5,
        "pool_scale": 1.0 + 0.02 * n(ks[11], (L, GROUP_W)),
        "sinks": 0.5 * n(ks[12], (L, SWA_HEADS)),
        "w_o": n(ks[13], (L, MIX_W, D)) * MIX_W ** -0.5 * BETA,
        "ln1_g": 1.0 + 0.02 * n(ks[14], (L, D)),
        "ln1_b": 0.02 * n(ks[15], (L, D)),
        "router_g_w": n(ks[16], (L, D, MOE_GROUPS)) * D ** -0.5,
        "router_g_b": 0.01 * n(ks[17], (L, MOE_GROUPS)),
        "router_e_w": n(ks[18], (L, D, N_EXPERTS)) * D ** -0.5,
        "router_e_b": 0.01 * n(ks[19], (L, N_EXPERTS)),
        "w_gate": n(ks[20], (L, N_EXPERTS, D, D_EXPERT)) * D ** -0.5,
        "w_up": n(ks[21], (L, N_EXPERTS, D, D_EXPERT)) * D ** -0.5,
        "w_down": n(ks[22], (L, N_EXPERTS, D_EXPERT, D)) * D_EXPERT ** -0.5 * BETA,
        "ple_gate_w": n(ks[23], (L, D, D)) * D ** -0.5,
        "ple_up_w": n(ks[24], (L, PLE_DIM, D)) * PLE_DIM ** -0.5 * BETA,
        "ln2_g": 1.0 + 0.02 * n(ks[25], (L, D)),
        "ln2_b": 0.02 * n(ks[26], (L, D)),
    }


def reference(x, p, rel_table, w_in, b_f, lam_q1, lam_k1, lam_q2, lam_k2, diff_norm_g,
              pool_w, pool_scale, sinks, w_o, ln1_g, ln1_b, router_g_w, router_g_b,
              router_e_w, router_e_b, w_gate, w_up, w_down, ple_gate_w, ple_up_w,
              ln2_g, ln2_b):
    B_, S_, _ = x.shape
    bias_by_dist = rel_table[_t5_bucket(jnp.arange(S_, dtype=jnp.int32))]
    diff_bias = bias_by_dist[:, :DIFF_HEADS]
    swa_bias = bias_by_dist[:SWA_WINDOW, DIFF_HEADS:]
    for i in range(DEPTH):
        proj = x @ w_in[i]
        fq, fk, fv, ff, dq, dk, dv, pu, sq, sk, sv = _split_cols(proj)
        y_fox = _forgetting_attention(
            fq.reshape(B_, S_, FOX_HEADS, HEAD_DIM), fk.reshape(B_, S_, FOX_HEADS, HEAD_DIM),
            fv.reshape(B_, S_, FOX_HEADS, HEAD_DIM), ff + b_f[i]).reshape(B_, S_, GROUP_W)
        lam_init = 0.8 - 0.6 * math.exp(-0.3 * i)
        lam = (jnp.exp(jnp.sum(lam_q1[i].astype(jnp.float32) * lam_k1[i].astype(jnp.float32)))
               - jnp.exp(jnp.sum(lam_q2[i].astype(jnp.float32) * lam_k2[i].astype(jnp.float32)))
               + lam_init)
        y_diff = _diff_attention(
            dq.reshape(B_, S_, DIFF_HEADS, 2, HEAD_DIM), dk.reshape(B_, S_, DIFF_HEADS, 2, HEAD_DIM),
            dv.reshape(B_, S_, DIFF_HEADS, 2 * HEAD_DIM), diff_bias, lam, lam_init, diff_norm_g[i])
        y_pool = _pool_mixer(pu, pool_w[i], pool_scale[i])
        y_swa = _sliding_window_attention(
            sq.reshape(B_, S_, SWA_HEADS, HEAD_DIM), sk.reshape(B_, S_, SWA_KV_HEADS, HEAD_DIM),
            sv.reshape(B_, S_, SWA_KV_HEADS, HEAD_DIM), swa_bias, sinks[i])
        mix = jnp.concatenate([y_fox, y_diff, y_pool, y_swa], axis=-1)
        h = _layer_norm(ALPHA * x + mix @ w_o[i], ln1_g[i], ln1_b[i])
        y_moe = _hier_moe(h, router_g_w[i], router_g_b[i], router_e_w[i], router_e_b[i],
                          w_gate[i], w_up[i], w_down[i])
        ple = jax.nn.sigmoid(h @ ple_gate_w[i]) * (p[i] @ ple_up_w[i])
        x = _layer_norm(ALPHA * h + y_moe + ple, ln2_g[i], ln2_b[i])
    return x
```

```python
import math
import os
import numpy as np
import concourse.bass as bass
import concourse.mybir as mybir
from concourse.bass_utils import run_bass_kernel_spmd

F32 = mybir.dt.float32
BF16 = mybir.dt.bfloat16
AF = mybir.ActivationFunctionType
ALU = mybir.AluOpType
AX = mybir.AxisListType

D = 2048
SEQ = 2048
NT = 1024
DEPTH = 2
IN_W = 4360
OFF = dict(fq=0, fk=512, fv=1024, ff=1536, dq=1544, dk=2056, dv=2568, pu=3080, sq=3592, sk=4104, sv=4232)
ALPHA = (2 * DEPTH) ** 0.25
LN_EPS = 1e-5
NEG = -30000.0
FUSED = True


class Buf:
    __slots__ = ("name", "last_w", "readers")

    def __init__(self, name):
        self.name = name
        self.last_w = None
        self.readers = {}


class Sched:
    def __init__(self, nc, n_dma_sems=6):
        self.nc = nc
        self.sems = {}
        self.count = {}
        self.known = {}
        self.engs = {"pe": nc.tensor, "act": nc.scalar, "dve": nc.vector, "pool": nc.gpsimd, "sp": nc.sync}
        self._ctx = []
        for e in self.engs:
            self.known[e] = {}
        for e in ("pe", "act", "dve", "pool"):
            self.sems[e] = self._sem("s_" + e)
            self.count[e] = 0
        self.dma_n = {}
        self.dma_last = {}
        self.n_dma_sems = n_dma_sems
        for q in ("sp", "pool"):
            self.dma_n[q] = 0
            for i in range(n_dma_sems):
                self.sems[(q, i)] = self._sem("d_%s%d" % (q, i))
                self.dma_last[(q, i)] = 0

    def _sem(self, name):
        cm = self.nc.semaphore(name)
        h = cm.__enter__()
        self._ctx.append(cm)
        return h

    def close(self):
        for cm in reversed(self._ctx):
            cm.__exit__(None, None, None)

    def _deps(self, reads, writes):
        deps = {}

        def add(ev):
            if ev is None:
                return
            k, v = ev
            if deps.get(k, 0) < v:
                deps[k] = v
        for b in reads:
            add(b.last_w)
        for b in writes:
            add(b.last_w)
            for k, v in b.readers.items():
                add((k, v))
        return deps

    def _wait(self, ename, deps, skip_self=False):
        eng = self.engs[ename]
        kn = self.known[ename]
        for k, v in deps.items():
            if skip_self and k == ename:
                continue
            if kn.get(k, 0) >= v:
                continue
            eng.wait_ge(self.sems[k], v)
            kn[k] = v

    def _mark(self, ev, reads, writes):
        k, v = ev
        for b in writes:
            b.last_w = ev
            b.readers = {}
        for b in reads:
            if b.readers.get(k, 0) < v:
                b.readers[k] = v

    def op(self, ename, fn, reads=(), writes=()):
        deps = self._deps(reads, writes)
        self._wait(ename, deps, skip_self=(ename == "pe"))
        ins = fn()
        self.count[ename] += 1
        ins.then_inc(self.sems[ename], 1)
        ev = (ename, self.count[ename])
        self._mark(ev, reads, writes)
        return ev

    def dma(self, q, out, in_, reads=(), writes=()):
        deps = self._deps(reads, writes)
        n = self.dma_n[q]
        slot = n % self.n_dma_sems
        rnd = n // self.n_dma_sems
        key = (q, slot)
        if rnd > 0:
            deps[key] = max(deps.get(key, 0), 16 * rnd)
        self._wait(q, deps)
        self.engs[q].dma_start(out=out, in_=in_).then_inc(self.sems[key], 16)
        self.dma_n[q] = n + 1
        self.dma_last[key] = 16 * (rnd + 1)
        ev = (key, 16 * (rnd + 1))
        self._mark(ev, reads, writes)
        return ev

    def barrier(self):
        tgt = {}
        for e in ("pe", "act", "dve", "pool"):
            if self.count[e] > 0:
                tgt[e] = self.count[e]
        for k, v in self.dma_last.items():
            if v > 0:
                tgt[k] = v
        for e in self.engs:
            self._wait(e, tgt, skip_self=False)


class Alloc:
    def __init__(self, nc):
        self.nc = nc
        self.cms = []

    _n = [0]

    def __call__(self, name, shape, dt=F32):
        Alloc._n[0] += 1
        cm = self.nc.sbuf_tensor("%s_%d" % (name, Alloc._n[0]), list(shape), dt)
        t = cm.__enter__()
        self.cms.append(cm)
        return t

    def __enter__(self):
        return self

    def __exit__(self, *a):
        for cm in reversed(self.cms):
            cm.__exit__(None, None, None)
        return False


def _t5_bucket_np(n):
    n = np.asarray(n, dtype=np.int32)
    max_exact = 16
    nf = np.maximum(n, 1).astype(np.float32)
    large = max_exact + (np.log(nf / np.float32(max_exact)) / np.float32(math.log(128 / max_exact))
                         * np.float32(32 - max_exact)).astype(np.int32)
    large = np.minimum(large, 31)
    return np.where(n < max_exact, n, large)


def _static_consts():
    c = {}
    c["ident"] = np.eye(128, dtype=np.float32)
    s = np.arange(128)[:, None]
    t = np.arange(128)[None, :]
    c["cmask"] = np.where(s <= t, 0.0, NEG).astype(np.float32)
    c["tri"] = (s <= t).astype(np.float32)
    c["ones"] = np.ones((128, 128), np.float32)
    sel = np.zeros((128, 8, 128), np.float32)
    for h in range(8):
        for b in (0, 32, 64):
            sel[b + h, h, :] = 1.0
    c["sel"] = sel
    wins = (2, 4, 8, 16)
    Ac = np.zeros((4, 128, 128), np.float32)
    Ap = np.zeros((4, 128, 128), np.float32)
    Af = np.zeros((4, 128, 128), np.float32)
    for g, w in enumerate(wins):
        for tt in range(128):
            for ss in range(tt - w + 1, tt + 1):
                if ss >= 0:
                    Ac[g, ss, tt] += 1.0 / w
                    Af[g, ss, tt] += 1.0 / min(tt + 1, w)
                else:
                    Ap[g, 128 + ss, tt] += 1.0 / w
            Ac[g, tt, tt] -= 1.0
            Af[g, tt, tt] -= 1.0
    c["poolAc"], c["poolAp"], c["poolAf"] = Ac, Ap, Af
    return c


def _bias_tiles(rel_table):
    bk = _t5_bucket_np(np.arange(256))
    bbd = rel_table[bk]
    s = np.arange(128)[:, None]
    t = np.arange(128)[None, :]
    dD = t - s
    dA = 128 + t - s
    diffB = np.empty((128, 4, 2, 128), np.float32)
    for h in range(4):
        diffB[:, h, 0, :] = np.where(dD >= 0, bbd[np.clip(dD, 0, 255), h], NEG)
        diffB[:, h, 1, :] = bbd[np.clip(dA, 0, 255), h]
    swaB = np.empty((128, 2, 8, 128), np.float32)
    for h in range(8):
        swaB[:, 0, h, :] = np.where(dA < 128, bbd[np.clip(dA, 0, 127), 4 + h], NEG)
        swaB[:, 1, h, :] = np.where(dD >= 0, bbd[np.clip(dD, 0, 127), 4 + h], NEG)
    b31 = np.broadcast_to(rel_table[31, :4][None, :], (128, 4)).astype(np.float32).copy()
    return diffB, swaB, b31


class StopBuild(Exception):
    pass


def build_program(layers, fused, stop=None):
    nc = bass.Bass("TRN2", target_bir_lowering=False)
    dbg = nc.dram_tensor("dbg", [128, 16 * NT], BF16, kind="ExternalOutput").ap() if stop else None
    nL = len(layers)

    def din(name, shape):
        return nc.dram_tensor(name, list(shape), F32, kind="ExternalInput").ap()

    x_own = din("x_own", [NT, D])
    x_prev = din("x_prev", [NT, D])
    p_own = din("p_own", [nL, NT, 256])
    w_in = din("w_in", [nL, D, IN_W])
    w_o = din("w_o", [nL, D, D])
    small_w = stop is not None and stop != "p3"
    w_gate = din("w_gate", [nL, 1 if small_w else 16, D, 512])
    w_up = din("w_up", [nL, 1 if small_w else 16, D, 512])
    w_down = din("w_down", [nL, 1 if small_w else 16, 512, D])
    ple_gw = din("ple_gw", [nL, D, D])
    ple_uw = din("ple_uw", [nL, 256, D])
    pool_w = din("pool_w", [nL, 4, 128, 128])
    w_r = din("w_r", [nL, D, 20])
    lnp = din("lnp", [nL, 4, 128, D])
    small = din("small", [nL, 128, 448])
    cst = {}
    for nm, shp in (("ident", [128, 128]), ("cmask", [128, 128]), ("tri", [128, 128]), ("ones", [128, 128]),
                    ("sel", [128, 8, 128]), ("poolAc", [4, 128, 128]), ("poolAp", [4, 128, 128]),
                    ("poolA0", [4, 128, 128]), ("poolAp0", [4, 128, 128]),
                    ("diffB", [128, 4, 2, 128]), ("swaB", [128, 2, 8, 128]), ("b31", [128, 4]), ("pm", [128, 1])):
        cst[nm] = din("c_" + nm, shp)
    y_out = nc.dram_tensor("y_out", [NT, D], F32, kind="ExternalOutput").ap()
    if fused and nL == 2:
        xmid = [nc.dram_tensor("xmid%d" % i, [128, D], F32) for i in range(8)]
        xmid_b = [nc.dram_tensor("xmidb%d" % i, [128, D], BF16) for i in range(8)]
        xall_b = [nc.dram_tensor("xallb%d" % i, [256, D], BF16) for i in range(8)]

    def rows_f(ap):
        return lambda i, c0, c1: ap[i * 128:(i + 1) * 128, c0:c1]

    S = Sched(nc)
    T, V, A, P = nc.tensor, nc.vector, nc.scalar, nc.gpsimd
    es = []

    def sb(name, shape, dt=F32):
        cm = nc.sbuf_tensor(name, list(shape), dt)
        t = cm.__enter__()
        es.append(cm)
        return t

    def psum(name):
        cm = nc.psum_tensor(name, [128, 512], F32)
        t = cm.__enter__()
        es.append(cm)
        return t

    PB = [psum("pb%d" % i) for i in range(8)]
    PBb = [Buf("pb%d" % i) for i in range(8)]

    actA = sb("actA", [128, 16, NT], BF16)
    actB = sb("actB", [128, 16, NT], BF16)
    b_actA, b_actB = Buf("actA"), Buf("actB")
    ident = sb("ident", [128, 128]); b_const = Buf("const")
    ident_b = sb("ident_b", [128, 128], BF16)
    cmask_b = sb("cmask_b", [128, 128], BF16)
    tri = sb("tri", [128, 128], BF16)
    ones = sb("ones", [128, 128], BF16)
    sel_b = sb("sel_b", [128, 8, 128], BF16)
    poolM = sb("poolM", [128, 4, 4, 128], BF16)
    DA2 = sb("DA2", [128, 4, 2, 256], BF16)
    swaB_b = sb("swaB_b", [128, 2, 8, 128], BF16)
    b31 = sb("b31", [128, 4])
    pm = sb("pm", [128, 1])
    bcol = sb("bcol", [128, 2, 4])
    zcol = sb("zcol", [128, 1])
    smalls = sb("smalls", [128, 448])
    b_small = Buf("small")
    lamc = sb("lamc", [128, 8])
    esink = sb("esink", [128, 8])
    gsub = sb("gsub", [128, 128])

    lamtmp = sb("lamtmp", [128, 128])
    b_stage = Buf("stage")
    stage_cm = nc.sbuf_tensor("stage", [128, 2048], F32)
    stage = stage_cm.__enter__()

    def load_const_f32(dst, src_ap):
        S.dma("sp", dst, src_ap, writes=[b_const])

    def load_cast(dst_ap, src_ap, n_free, view=None):
        st = stage[:src_ap.shape[0], 0:n_free]
        if view is not None:
            st = view(st)
        S.dma("sp", st, src_ap, writes=[b_stage])
        S.op("dve", lambda: V.tensor_copy(dst_ap, st), reads=[b_stage], writes=[b_const])

    load_const_f32(ident[:], cst["ident"][:, :])
    load_cast(tri[:], cst["tri"][:, :], 128)
    load_cast(ones[:], cst["ones"][:, :], 128)
    load_const_f32(b31[:], cst["b31"][:, :])
    load_const_f32(pm[:], cst["pm"][:, :])
    load_cast(ident_b[:], cst["ident"][:, :], 128)
    load_cast(cmask_b[:], cst["cmask"][:, :], 128)
    load_cast(sel_b[:], cst["sel"][:, :, :], 1024, view=lambda a: a.rearrange("p (a b) -> p a b", a=8))
    for k, nm in enumerate(("poolAc", "poolAp", "poolA0", "poolAp0")):
        load_cast(poolM[:, k, :, :], cst[nm].rearrange("g s t -> s g t"), 512,
                  view=lambda a: a.rearrange("p (a b) -> p a b", a=4))
    load_cast(swaB_b[:], cst["swaB"][:, :, :, :], 2048,
              view=lambda a: a.rearrange("p (a b c) -> p a b c", a=2, b=8))
    S.dma("sp", stage[:, 0:1024].rearrange("p (h k t) -> p h k t", h=4, k=2), cst["diffB"][:, :, :, :], writes=[b_stage])
    for h in range(4):
        for m in range(2):
            S.op("dve", lambda: V.tensor_scalar(
                DA2[:, h, m, :], stage[:, h * 256:(h + 1) * 256], b31[:, h:h + 1], None, ALU.subtract),
                reads=[b_stage, b_const], writes=[b_const])
    S.op("dve", lambda: V.memset(zcol[:], 0.0), writes=[b_const])
    S.op("dve", lambda: V.tensor_copy(bcol[:, 0, :], b31[:]), reads=[b_const], writes=[b_const])
    S.op("dve", lambda: V.tensor_scalar(bcol[:, 1, :], b31[:], pm[:, 0:1], None, ALU.add), reads=[b_const], writes=[b_const])

    S.barrier()
    stage_cm.__exit__(None, None, None)
    bank_rr = [0]

    def transposes_to_T(src_tile_fn, nchunks, dst, dst_buf, tok0, src_bufs, banks, extra=None):
        for g in range(nchunks // 4):
            bi = banks[g % len(banks)]
            for j in range(4):
                c = g * 4 + j
                S.op("pe", lambda: T.transpose(PB[bi][:, j * 128:(j + 1) * 128], src_tile_fn(c), ident[:]),
                     reads=src_bufs + [b_const], writes=[PBb[bi]])
            pv = PB[bi][:].rearrange("p (a b) -> p a b", a=4)
            if extra is not None:
                extra(g, pv, PBb[bi])
            eng = "act" if g % 2 == 0 else "dve"
            if eng == "act":
                S.op("act", lambda: A.copy(dst[:, g * 4:(g + 1) * 4, tok0:tok0 + 128], pv), writes=[PBb[bi], dst_buf])
            else:
                S.op("dve", lambda: V.tensor_copy(dst[:, g * 4:(g + 1) * 4, tok0:tok0 + 128], pv), writes=[PBb[bi], dst_buf])

    def ln_stats_all(hb, b_hs, b_ln, stats, mvA, rsA, eps):
        for i in range(8):
            for c in range(4):
                S.op("dve", lambda: V.bn_stats(stats[:, c, :], hb[:, i, c * 512:(c + 1) * 512]), reads=[b_hs[i]], writes=[b_ln])
            S.op("dve", lambda: V.bn_aggr(mvA[:, i, :], stats[:]), writes=[b_ln])
        S.op("dve", lambda: V.tensor_scalar(rsA[:, :, 0], mvA[:, :, 1], eps, None, ALU.add), writes=[b_ln])
        S.op("act", lambda: A.activation(rsA[:, :, 0], rsA[:, :, 0], AF.Ln), writes=[b_ln])
        S.op("act", lambda: A.activation(rsA[:, :, 0], rsA[:, :, 0], AF.Exp, scale=-0.5), writes=[b_ln])
        S.op("dve", lambda: V.scalar_tensor_tensor(rsA[:, :, 1], mvA[:, :, 0], -1.0, rsA[:, :, 0], ALU.mult, ALU.mult), writes=[b_ln])

    def ln_apply_tile(hb, i, gt, bt, b_h, b_ln, rsA, b_lnw):
        S.op("act", lambda: A.activation(hb[:, i, :], hb[:, i, :], AF.Identity, bias=rsA[:, i, 1:2], scale=rsA[:, i, 0:1]),
             reads=[b_ln], writes=[b_h])
        S.op("dve", lambda: V.tensor_tensor(hb[:, i, :], hb[:, i, :], gt[:], ALU.mult), reads=[b_lnw], writes=[b_h])
        S.op("dve", lambda: V.tensor_tensor(hb[:, i, :], hb[:, i, :], bt[:], ALU.add), reads=[b_lnw], writes=[b_h])

    def layer(li, l, xo_f, xp_f, out_f, xT, b_xT, mixT, b_mixT, xp_b=None, post_out=None, xo_T_f=None):
        lam_init = 0.8 - 0.6 * math.exp(-0.3 * l)
        hT, b_hT = xT, b_xT

        S.dma("sp", smalls[:], small[li], writes=[b_small])
        bf_t = smalls[:, 0:8]
        sink_t = smalls[:, 8:16]
        lam_v = smalls[:, 16:272].rearrange("p (a b) -> p a b", a=4)
        gsub_raw = smalls[:, 272:400]
        pscale = smalls[:, 400:404]
        rb_t = smalls[:, 404:424]
        S.op("dve", lambda: V.tensor_tensor(lamtmp[:, 0:64], lam_v[:, 0, :], lam_v[:, 1, :], ALU.mult), reads=[b_small], writes=[b_stage])
        S.op("dve", lambda: V.tensor_tensor(lamtmp[:, 64:128], lam_v[:, 2, :], lam_v[:, 3, :], ALU.mult), reads=[b_small], writes=[b_stage])
        S.op("dve", lambda: V.tensor_reduce(lamc[:, 0:2], lamtmp[:, 0:128].rearrange("p (a b) -> p a b", a=2), AX.X, ALU.add),
             reads=[b_stage], writes=[b_small])
        S.op("act", lambda: A.activation(lamc[:, 2:4], lamc[:, 0:2], AF.Exp), writes=[b_small])
        S.op("dve", lambda: V.tensor_tensor(lamc[:, 4:5], lamc[:, 3:4], lamc[:, 2:3], ALU.subtract), writes=[b_small])
        S.op("dve", lambda: V.tensor_scalar(lamc[:, 5:6], lamc[:, 4:5], -lam_init, None, ALU.add), writes=[b_small])
        S.op("act", lambda: A.activation(esink[:], sink_t, AF.Exp), reads=[b_small], writes=[b_small])
        S.op("dve", lambda: V.tensor_scalar(gsub[:], gsub_raw, 1.0 - lam_init, None, ALU.mult), writes=[b_small])
        neglam = lamc[:, 5:6]

        with Alloc(nc) as al:
            xTp = al("xTp", [128, 16, NT], BF16)
            wr0 = al("wr0", [128, 16, 256], BF16)
            wr1 = al("wr1", [128, 16, 256], BF16)
            b_xTp = Buf("xTp")
            wring = [wr0, wr1]
            b_wring = [Buf("wr0"), Buf("wr1")]
            wn = [0]

            def load_w(c0, ncols):
                k = wn[0] % 2
                wn[0] += 1
                S.dma("pool", wring[k][:, :, 0:ncols], w_in[li][:, c0:c0 + ncols].rearrange("(c p) f -> p c f", p=128),
                      writes=[b_wring[k]])
                return wring[k], b_wring[k]

            with Alloc(nc) as al:
                xin = [al("xin%d" % j, [128, D], BF16) for j in range(4)]
                b_xin = [Buf("xin%d" % j) for j in range(4)]
                nt = 0
                for which, (src, dst, bdst) in enumerate((((xo_T_f if xo_T_f is not None else (lambda i: xo_f(i, 0, D))), xT, b_xT), (xp_f, xTp, b_xTp))):
                    for i in range(8):
                        k = (which * 8 + i) % 4
                        S.dma("pool", xin[k][:], src(i), reads=([xp_b[i]] if (which == 1 and xp_b is not None) else []), writes=[b_xin[k]])
                        for g in range(2):
                            bi = nt % 2
                            nt += 1
                            pbb = PB[bi][:].bitcast(BF16)
                            for j in range(8):
                                c = g * 8 + j
                                S.op("pe", lambda: T.transpose(pbb[:, j * 128:(j + 1) * 128], xin[k][:, c * 128:(c + 1) * 128], ident_b[:]),
                                     reads=[b_xin[k], b_const], writes=[PBb[bi]])
                            pv = pbb.rearrange("p (a b) -> p a b", a=8)
                            if g == 0:
                                S.op("act", lambda: A.copy(dst[:, 0:8, i * 128:(i + 1) * 128], pv), writes=[PBb[bi], bdst])
                            else:
                                S.op("dve", lambda: V.tensor_copy(dst[:, 8:16, i * 128:(i + 1) * 128], pv), writes=[PBb[bi], bdst])
                S.barrier()
                if stop == "p0":
                    S.dma("sp", dbg, xT[:].rearrange("p a b -> p (a b)"))
                    raise StopBuild()

            def proj_fm(c0, M, srcT, b_src, tok0, ntok, dst_fn, scale, wt, b_w, wc0, bank):
                for c in range(16):
                    S.op("pe", lambda: T.matmul(PB[bank][0:M, 0:ntok], wt[:, c, wc0:wc0 + M], srcT[:, c, tok0:tok0 + ntok],
                                                start=(c == 0), stop=(c == 15)),
                         reads=[b_w, b_src], writes=[PBb[bank]])
                dst, b_dst = dst_fn()
                if isinstance(dst, tuple):
                    S.op("act", lambda: A.activation(dst[0], PB[bank][0:64, 0:ntok], AF.Copy, scale=scale),
                         writes=[PBb[bank], b_dst])
                    S.op("act", lambda: A.activation(dst[1], PB[bank][64:128, 0:ntok], AF.Copy, scale=scale),
                         writes=[PBb[bank], b_dst])
                else:
                    S.op("act", lambda: A.activation(dst, PB[bank][0:M, 0:ntok], AF.Copy, scale=scale),
                         writes=[PBb[bank], b_dst])

            def proj_tm(srcT, b_src, tile, ncols, wt, b_w, wc0, bank):
                for c in range(16):
                    S.op("pe", lambda: T.matmul(PB[bank][:, 0:ncols], srcT[:, c, tile * 128:(tile + 1) * 128], wt[:, c, wc0:wc0 + ncols],
                                                start=(c == 0), stop=(c == 15)),
                         reads=[b_w, b_src], writes=[PBb[bank]])

            def nb():
                bank_rr[0] ^= 1
                return bank_rr[0]

            SBANKS = (2, 3, 0)

            def pipeline(iters, fa, fb, depth=2):
                N = len(iters)
                for n in range(min(depth, N)):
                    fa(iters[n])
                for n in range(N):
                    if n + depth < N:
                        fa(iters[n + depth])
                    fb(iters[n])

            def ystage_to_mixT(ystage, b_ys, base_chunk):
                pbb = PB[7][:].bitcast(BF16)
                for i in range(8):
                    for cc in range(4):
                        S.op("pe", lambda: T.transpose(pbb[:, cc * 128:(cc + 1) * 128], ystage[:, i, cc * 128:(cc + 1) * 128], ident_b[:]),
                             reads=[b_ys, b_const], writes=[PBb[7]])
                    S.op("dve", lambda: V.tensor_copy(mixT[:, base_chunk:base_chunk + 4, i * 128:(i + 1) * 128],
                                                      pbb[:, 0:512].rearrange("p (a b) -> p a b", a=4)),
                         writes=[PBb[7], b_mixT])

            with Alloc(nc) as al:
                fkT = al("fkT", [128, 4, 2 * NT], BF16)
                fvA = al("fvA", [128, 16, 8, 66], BF16)
                fqT = al("fqT", [128, 4, 2, NT], BF16)
                zf = al("zf", [128, 16, 8], F32)
                lsf = al("lsf", [128, 16, 8], F32)
                ls3 = al("ls3", [128, 16, 3, 8], BF16)
                tmpf = al("tmpf", [128, 16, 8], F32)
                cf = al("cf", [128, 16, 8], F32)
                negc = al("negc", [128, 16, 8], F32)
                cpad = al("cpad", [128, 8, 128], BF16)
                c3 = al("c3", [128, NT], BF16)
                PT0 = al("PT0", [128, 512], BF16)
                PT1 = al("PT1", [128, 512], BF16)
                PT2 = al("PT2", [128, 512], BF16)
                rec = al("rec", [128, 8], F32)
                ystage = al("ystage", [128, 8, 512], BF16)
                b_fkT, b_fvA, b_fqT, b_c, b_c3, b_rec, b_ys = [Buf(n) for n in "fkT fvA fqT c c3 rec ys".split()]
                PTs = [PT0, PT1, PT2]
                b_PT = [Buf("PT%d" % i) for i in range(3)]
                S.op("dve", lambda: V.memset(fvA[:, :, :, 64:65], 1.0), writes=[b_fvA])
                S.op("dve", lambda: V.memset(fqT[:], 0.0), writes=[b_fqT])
                for grp in range(2):
                    wt, b_w = load_w(OFF["fq"] + grp * 256, 256)
                    for sl in range(2):
                        pr = grp * 2 + sl
                        for tc in range(2):
                            proj_fm(0, 128, xT, b_xT, tc * 512, 512,
                                    lambda: ((fqT[0:64, pr, 0, tc * 512:(tc + 1) * 512], fqT[64:128, pr, 1, tc * 512:(tc + 1) * 512]), b_fqT),
                                    0.125, wt, b_w, sl * 128, nb())
                for grp in range(2):
                    wt, b_w = load_w(OFF["fk"] + grp * 256, 256)
                    for sl in range(2):
                        pr = grp * 2 + sl
                        for half, (src, bs) in enumerate(((xTp, b_xTp), (xT, b_xT))):
                            for tc in range(2):
                                proj_fm(0, 128, src, bs, tc * 512, 512,
                                        lambda: (fkT[:, pr, half * NT + tc * 512: half * NT + (tc + 1) * 512], b_fkT),
                                        1.0, wt, b_w, sl * 128, nb())
                for grp in range(2):
                    wt, b_w = load_w(OFF["fv"] + grp * 256, 256)
                    for kt in range(16):
                        src, bs = (xTp, b_xTp) if kt < 8 else (xT, b_xT)
                        bk = nb()
                        proj_tm(src, bs, kt % 8, 256, wt, b_w, 0, bk)
                        S.op("act", lambda: A.copy(fvA[:, kt, grp * 4:(grp + 1) * 4, 0:64],
                                                   PB[bk][:, 0:256].rearrange("p (a b) -> p a b", a=4)),
                             writes=[PBb[bk], b_fvA])
                wt, b_w = load_w(OFF["ff"] + 8 - 256, 256)
                for kt in range(16):
                    src, bs = (xTp, b_xTp) if kt < 8 else (xT, b_xT)
                    bk = nb()
                    proj_tm(src, bs, kt % 8, 8, wt, b_w, 248, bk)
                    S.op("dve", lambda: V.tensor_tensor(zf[:, kt, :], PB[bk][:, 0:8], bf_t, ALU.add),
                         reads=[b_small], writes=[PBb[bk], b_c])
                if stop == "fox_proj":
                    S.barrier()
                    S.dma("sp", dbg[:, 0:4 * NT], fqT[:].rearrange("p a b -> p (a b)"))
                    S.dma("sp", dbg[:, 4 * NT:12 * NT], fkT[:].rearrange("p a b -> p (a b)"))
                    raise StopBuild()
                S.op("dve", lambda: V.tensor_scalar(tmpf[:], zf[:], -1.0, None, ALU.mult), writes=[b_c])
                S.op("dve", lambda: V.tensor_tensor(tmpf[:], tmpf[:], zf[:], ALU.max), writes=[b_c])
                S.op("act", lambda: A.activation(tmpf[:], tmpf[:], AF.Exp, scale=-1.0), writes=[b_c])
                S.op("act", lambda: A.activation(tmpf[:], tmpf[:], AF.Ln, bias=1.0), writes=[b_c])
                S.op("dve", lambda: V.tensor_scalar(lsf[:], zf[:], 0.0, None, ALU.min), writes=[b_c])
                S.op("dve", lambda: V.tensor_tensor(lsf[:], lsf[:], tmpf[:], ALU.subtract), writes=[b_c])
                S.op("dve", lambda: V.tensor_copy(ls3[:, :, 0, :], lsf[:]), writes=[b_c])
                S.op("dve", lambda: V.tensor_tensor(tmpf[:], lsf[:], ls3[:, :, 0, :], ALU.subtract), writes=[b_c])
                S.op("dve", lambda: V.tensor_copy(ls3[:, :, 1, :], tmpf[:]), writes=[b_c])
                S.op("dve", lambda: V.tensor_tensor(tmpf[:], tmpf[:], ls3[:, :, 1, :], ALU.subtract), writes=[b_c])
                S.op("dve", lambda: V.tensor_copy(ls3[:, :, 2, :], tmpf[:]), writes=[b_c])
                first = True
                for kt in range(16):
                    for k2 in range(kt + 1):
                        lhs = tri if k2 == kt else ones
                        S.op("pe", lambda: T.matmul(PB[6][:, kt * 24:(kt + 1) * 24], lhs[:], ls3[:, k2, :, :], start=first, stop=False,
                                                    skip_group_check=True),
                             reads=[b_c, b_const], writes=[PBb[6]])
                        first = False
                p6v = PB[6][:, 0:384].rearrange("p (a k b) -> p a k b", a=16, k=3)
                S.op("dve", lambda: V.tensor_copy(cf[:], p6v[:, :, 0, :]), writes=[PBb[6], b_c])
                S.op("dve", lambda: V.tensor_tensor(cf[:], cf[:], p6v[:, :, 1, :], ALU.add), writes=[PBb[6], b_c])
                S.op("dve", lambda: V.tensor_tensor(cf[:], cf[:], p6v[:, :, 2, :], ALU.add), writes=[PBb[6], b_c])
                S.op("dve", lambda: V.tensor_scalar(negc[:, 0:8, :], cf[:, 0:8, :], -1.0, pm[:, 0:1], ALU.mult, ALU.add),
                     reads=[b_const], writes=[b_c])
                S.op("dve", lambda: V.tensor_scalar(negc[:, 8:16, :], cf[:, 8:16, :], -1.0, None, ALU.mult), writes=[b_c])
                S.op("dve", lambda: V.memset(cpad[:], 0.0), writes=[b_c3])
                cown = cf[:, 8:16, :]
                S.op("dve", lambda: V.tensor_copy(cpad[:, :, 0:8], cown), reads=[b_c], writes=[b_c3])
                S.op("dve", lambda: V.tensor_tensor(tmpf[:, 0:8, :], cown, cpad[:, :, 0:8], ALU.subtract), reads=[b_c], writes=[b_c3])
                S.op("dve", lambda: V.tensor_copy(cpad[:, :, 32:40], tmpf[:, 0:8, :]), writes=[b_c3])
                S.op("dve", lambda: V.tensor_tensor(tmpf[:, 0:8, :], tmpf[:, 0:8, :], cpad[:, :, 32:40], ALU.subtract), writes=[b_c3])
                S.op("dve", lambda: V.tensor_copy(cpad[:, :, 64:72], tmpf[:, 0:8, :]), writes=[b_c3])
                pb6b = PB[6][:].bitcast(BF16)
                for i in range(8):
                    S.op("pe", lambda: T.transpose(pb6b[:, i * 128:(i + 1) * 128], cpad[:, i, :], ident_b[:]),
                         reads=[b_c3, b_const], writes=[PBb[6]])
                S.op("dve", lambda: V.tensor_copy(c3[:], pb6b[:, :]), writes=[PBb[6], b_c3])
                if stop == "fox_c":
                    S.barrier()
                    S.dma("sp", dbg[:, 0:NT], c3[:, :])
                    raise StopBuild()
                def fox_A(itm):
                    n, h, cq, kt, nkt, grp = itm
                    pr, hh = h // 2, h % 2
                    rel = kt - (8 + 4 * cq)
                    t_lo = 0 if rel <= 0 else 128 * rel
                    sbk = SBANKS[n % 3]
                    pk = n % 3
                    q0 = cq * 512 + t_lo
                    S.op("pe", lambda: T.matmul(PB[sbk][:, t_lo:512], fkT[:, pr, kt * 128:(kt + 1) * 128], fqT[:, pr, hh, q0:cq * 512 + 512],
                                                start=True, stop=False, skip_group_check=True),
                         reads=[b_fkT, b_fqT], writes=[PBb[sbk]])
                    S.op("pe", lambda: T.matmul(PB[sbk][:, t_lo:512], sel_b[:, h, :], c3[:, q0:cq * 512 + 512],
                                                start=False, stop=(rel < 0), skip_group_check=True),
                         reads=[b_c3, b_const], writes=[PBb[sbk]])
                    if rel >= 0:
                        S.op("pe", lambda: T.matmul(PB[sbk][:, t_lo:t_lo + 128], ident_b[:], cmask_b[:],
                                                    start=False, stop=True, skip_group_check=True),
                             reads=[b_const], writes=[PBb[sbk]])
                    S.op("act", lambda: A.activation(PTs[pk][:, t_lo:512], PB[sbk][:, t_lo:512], AF.Exp, bias=negc[:, kt, h:h + 1]),
                         reads=[b_c], writes=[PBb[sbk], b_PT[pk]])

                def fox_B(itm):
                    n, h, cq, kt, nkt, grp = itm
                    rel = kt - (8 + 4 * cq)
                    t_lo = 0 if rel <= 0 else 128 * rel
                    pk = n % 3
                    ob = 4 + (grp % 2)
                    for qb in range(t_lo // 128, 4):
                        S.op("pe", lambda: T.matmul(PB[ob][:, qb * 65:qb * 65 + 65], PTs[pk][:, qb * 128:(qb + 1) * 128], fvA[:, kt, h, 0:65],
                                                    start=(kt == 0 and qb == 0), stop=False, skip_group_check=True),
                             reads=[b_PT[pk], b_fvA], writes=[PBb[ob]])
                    if kt == nkt - 1:
                        ov = PB[ob][:, 0:260].rearrange("p (a b) -> p a b", a=4)
                        S.op("dve", lambda: V.reciprocal(rec[:, 0:4], ov[:, :, 64]), writes=[PBb[ob], b_rec])
                        for qb in range(4):
                            S.op("dve", lambda: V.tensor_scalar(ystage[:, cq * 4 + qb, h * 64:(h + 1) * 64], ov[:, qb, 0:64],
                                                                rec[:, qb:qb + 1], None, ALU.mult),
                                 reads=[b_rec], writes=[PBb[ob], b_ys])

                iters = []
                for h in range(int(os.environ.get("FOXH", "8"))):
                    for cq in range(2):
                        nkt = 8 + 4 * cq + 4
                        for kt in range(nkt):
                            iters.append((len(iters), h, cq, kt, nkt, h * 2 + cq))
                pipeline(iters, fox_A, fox_B)
                if os.environ.get("FOXT", "1") == "1":
                    ystage_to_mixT(ystage, b_ys, 0)
                S.barrier()
                if stop == "fox":
                    S.dma("sp", dbg, mixT[:].rearrange("p a b -> p (a b)"))
                    raise StopBuild()

            with Alloc(nc) as al:
                dkT = al("dkT", [128, 4, 2 * NT], BF16)
                dvA = al("dvA", [128, 16, 4, 130], BF16)
                dqT = al("dqT", [128, 4, 2, NT], BF16)
                dPT0 = al("dPT0", [128, 2, 256], BF16)
                dPT1 = al("dPT1", [128, 2, 256], BF16)
                dPT2 = al("dPT2", [128, 2, 256], BF16)
                drec = al("drec", [128, 4], F32)
                dob = al("dob", [128, 128], F32)
                dsq = al("dsq", [128, 128], F32)
                dss = al("dss", [128, 8, 4], F32)
                y32 = al("y32", [128, 8, 512], F32)
                b_dss = Buf("dss")
                ystage = al("ystage_d", [128, 8, 512], BF16)
                b_dkT, b_dvA, b_dqT, b_rec, b_ys, b_ob = [Buf(n) for n in "dkT dvA dqT drec dys dob".split()]
                PTs = [dPT0, dPT1, dPT2]
                b_PT = [Buf("dPT%d" % i) for i in range(3)]
                S.op("dve", lambda: V.memset(dvA[:, :, :, 128:129], 1.0), writes=[b_dvA])
                S.op("dve", lambda: V.memset(dqT[:], 0.0), writes=[b_dqT])
                for grp in range(2):
                    wt, b_w = load_w(OFF["dq"] + grp * 256, 256)
                    for sl in range(2):
                        hd = grp * 2 + sl
                        for tc in range(2):
                            proj_fm(0, 128, xT, b_xT, tc * 512, 512,
                                    lambda: ((dqT[0:64, hd, 0, tc * 512:(tc + 1) * 512], dqT[64:128, hd, 1, tc * 512:(tc + 1) * 512]), b_dqT),
                                    0.125, wt, b_w, sl * 128, nb())
                for grp in range(2):
                    wt, b_w = load_w(OFF["dk"] + grp * 256, 256)
                    for sl in range(2):
                        hd = grp * 2 + sl
                        for half, (src, bs) in enumerate(((xTp, b_xTp), (xT, b_xT))):
                            for tc in range(2):
                                proj_fm(0, 128, src, bs, tc * 512, 512,
                                        lambda: (dkT[:, hd, half * NT + tc * 512: half * NT + (tc + 1) * 512], b_dkT),
                                        1.0, wt, b_w, sl * 128, nb())
                for grp in range(2):
                    wt, b_w = load_w(OFF["dv"] + grp * 256, 256)
                    for kt in range(16):
                        src, bs = (xTp, b_xTp) if kt < 8 else (xT, b_xT)
                        bk = nb()
                        proj_tm(src, bs, kt % 8, 256, wt, b_w, 0, bk)
                        S.op("act", lambda: A.copy(dvA[:, kt, grp * 2:(grp + 1) * 2, 0:128],
                                                   PB[bk][:, 0:256].rearrange("p (a b) -> p a b", a=2)),
                             writes=[PBb[bk], b_dvA])
                def diff_A(itm):
                    n, h, c2, kt, nkt, grp = itm
                    rel = kt - (8 + 2 * c2)
                    t_lo = 128 if rel == 1 else 0
                    sbk = SBANKS[n % 3]
                    pk = n % 3
                    q0 = c2 * 256 + t_lo
                    q1 = c2 * 256 + 256
                    Sv = PB[sbk][:].rearrange("p (m t) -> p m t", m=2)
                    for m in range(2):
                        S.op("pe", lambda: T.matmul(Sv[:, m, t_lo:256], dkT[:, h, kt * 128:(kt + 1) * 128], dqT[:, h, m, q0:q1],
                                                    start=(m == 0), stop=False, skip_group_check=True),
                             reads=[b_dkT, b_dqT], writes=[PBb[sbk]])
                    brange = {-1: (0, 128, 128, 256), 0: (0, 256, 0, 256), 1: (128, 256, 0, 128)}.get(rel)
                    if brange is not None:
                        o0, o1, r0, r1 = brange
                        for m in range(2):
                            S.op("pe", lambda: T.matmul(Sv[:, m, o0:o1], ident_b[:], DA2[:, h, m, r0:r1], start=False, stop=(m == 1),
                                                        skip_group_check=True), reads=[b_const], writes=[PBb[sbk]])
                    bias_ap = bcol[:, 1, h:h + 1] if kt < 8 else bcol[:, 0, h:h + 1]
                    S.op("act", lambda: A.activation(PTs[pk][:, :, t_lo:256], Sv[:, :, t_lo:256], AF.Exp, bias=bias_ap),
                         reads=[b_const], writes=[PBb[sbk], b_PT[pk]])

                def diff_B(itm):
                    n, h, c2, kt, nkt, grp = itm
                    rel = kt - (8 + 2 * c2)
                    t_lo = 128 if rel == 1 else 0
                    pk = n % 3
                    obs = (4, 5) if grp % 2 == 0 else (6, 7)
                    for qb in range(t_lo // 128, 2):
                        for m in range(2):
                            S.op("pe", lambda: T.matmul(PB[obs[qb]][:, m * 129:m * 129 + 129], PTs[pk][:, m, qb * 128:(qb + 1) * 128],
                                                        dvA[:, kt, h, 0:129], start=(kt == 0 and m == 0), stop=False, skip_group_check=True),
                                 reads=[b_PT[pk], b_dvA], writes=[PBb[obs[qb]]])
                    if kt == nkt - 1:
                        for qb in range(2):
                            ob = obs[qb]
                            ov = PB[ob][:, 0:258].rearrange("p (a b) -> p a b", a=2)
                            S.op("dve", lambda: V.reciprocal(drec[:, 0:2], ov[:, :, 128]), writes=[PBb[ob], b_rec])
                            S.op("dve", lambda: V.tensor_tensor(drec[:, 2:3], drec[:, 1:2], neglam, ALU.mult), reads=[b_small], writes=[b_rec])
                            S.op("dve", lambda: V.tensor_scalar(dob[:], ov[:, 0, 0:128], drec[:, 0:1], None, ALU.mult),
                                 reads=[b_rec], writes=[PBb[ob], b_ob])
                            S.op("dve", lambda: V.scalar_tensor_tensor(dob[:], ov[:, 1, 0:128], drec[:, 2:3], dob[:], ALU.mult, ALU.add),
                                 reads=[b_rec], writes=[PBb[ob], b_ob])
                            tl = c2 * 2 + qb
                            S.op("dve", lambda: V.scalar_tensor_tensor(dsq[:], dob[:], 1.0, dob[:], ALU.mult, ALU.mult, accum_out=dss[:, tl, h:h + 1]),
                                 reads=[b_ob], writes=[b_dss])
                            S.op("dve", lambda: V.tensor_copy(y32[:, tl, h * 128:(h + 1) * 128], dob[:]), reads=[b_ob], writes=[b_dss])

                iters = []
                for h in range(4):
                    for c2 in range(4):
                        nkt = 8 + 2 * c2 + 2
                        for kt in range(nkt):
                            iters.append((len(iters), h, c2, kt, nkt, h * 4 + c2))
                S.op("dve", lambda: V.memset(dss[:], 0.0), writes=[b_dss])
                pipeline(iters, diff_A, diff_B)
                S.op("dve", lambda: V.tensor_scalar(dss[:], dss[:], 1.0 / 128.0, LN_EPS, ALU.mult, ALU.add), writes=[b_dss])
                S.op("act", lambda: A.activation(dss[:], dss[:], AF.Ln), writes=[b_dss])
                S.op("act", lambda: A.activation(dss[:], dss[:], AF.Exp, scale=-0.5), writes=[b_dss])
                for tl in range(8):
                    for h in range(4):
                        S.op("dve", lambda: V.scalar_tensor_tensor(ystage[:, tl, h * 128:(h + 1) * 128], y32[:, tl, h * 128:(h + 1) * 128],
                                                                   dss[:, tl, h:h + 1], gsub[:], ALU.mult, ALU.mult),
                             reads=[b_dss, b_small], writes=[b_ys])
                ystage_to_mixT(ystage, b_ys, 4)
                S.barrier()
                if stop == "diff":
                    S.dma("sp", dbg, mixT[:].rearrange("p a b -> p (a b)"))
                    raise StopBuild()

            with Alloc(nc) as al:
                sqT = al("sqT", [128, 8, NT], BF16)
                wpad = al("wpad", [128, 16, 128], BF16)
                skT = al("skT", [128, NT + 128], BF16)
                svA = al("svA", [128, 9, 2, 66], BF16)
                ut = al("ut", [128, 9, 512], BF16)
                pw = al("pw", [128, 4, 128], BF16)
                sPT0 = al("sPT0", [128, 4, 128], BF16)
                sPT1 = al("sPT1", [128, 4, 128], BF16)
                sPT2 = al("sPT2", [128, 4, 128], BF16)
                pooledT = al("pooledT", [128, 512], BF16)
                srec = al("srec", [128, 4], F32)
                ystage = al("ystage_s", [128, 8, 512], BF16)
                b_sqT, b_skT, b_svA, b_ut, b_pw, b_rec, b_ys, b_pl = [Buf(n) for n in "sqT skT svA ut pw srec sys pl".split()]
                PTs = [sPT0, sPT1, sPT2]
                b_PT = [Buf("sPT0"), Buf("sPT1"), Buf("sPT2")]
                S.op("dve", lambda: V.memset(svA[:, :, :, 64:65], 1.0), writes=[b_svA])
                S.dma("pool", pw[:], pool_w[li].rearrange("g c d -> c g d"), writes=[b_pw])
                b_wpad = Buf("wpad")
                S.op("dve", lambda: V.memset(wpad[:], 0.0), writes=[b_wpad])
                for grp in range(2):
                    wt, b_w = load_w(OFF["sq"] + grp * 256, 256)
                    for sl in range(4):
                        hq = grp * 4 + sl
                        g = hq // 4
                        if grp == 1 and sl == 0:
                            S.op("dve", lambda: V.memset(wpad[:], 0.0), writes=[b_wpad])
                        S.op("dve", lambda: V.tensor_copy(wpad[:, :, g * 64:(g + 1) * 64], wt[:, :, sl * 64:(sl + 1) * 64]),
                             reads=[b_w], writes=[b_wpad])
                        for tc in range(2):
                            proj_fm(0, 128, xT, b_xT, tc * 512, 512,
                                    lambda: (sqT[:, hq, tc * 512:(tc + 1) * 512], b_sqT), 0.125, wpad, b_wpad, 0, nb())
                wt, b_w = load_w(OFF["sk"], 256)
                proj_fm(0, 128, xTp, b_xTp, 7 * 128, 128, lambda: (skT[:, 0:128], b_skT), 1.0, wt, b_w, 0, nb())
                for tc in range(2):
                    proj_fm(0, 128, xT, b_xT, tc * 512, 512,
                            lambda: (skT[:, 128 + tc * 512:128 + (tc + 1) * 512], b_skT), 1.0, wt, b_w, 0, nb())
                for kt in range(9):
                    src, bs, tl = (xTp, b_xTp, 7) if kt == 0 else (xT, b_xT, kt - 1)
                    bk = nb()
                    proj_tm(src, bs, tl, 128, wt, b_w, 128, bk)
                    S.op("act", lambda: A.copy(svA[:, kt, :, 0:64], PB[bk][:, 0:128].rearrange("p (a b) -> p a b", a=2)),
                         writes=[PBb[bk], b_svA])
                for grp in range(2):
                    wt, b_w = load_w(OFF["pu"] + grp * 256, 256)
                    for kt in range(9):
                        src, bs, tl = (xTp, b_xTp, 7) if kt == 0 else (xT, b_xT, kt - 1)
                        bk = nb()
                        proj_tm(src, bs, tl, 256, wt, b_w, 0, bk)
                        S.op("act", lambda: A.copy(ut[:, kt, grp * 256:(grp + 1) * 256], PB[bk][:, 0:256]), writes=[PBb[bk], b_ut])
                def swa_A(itm):
                    n, i, g, blk = itm
                    ktl = i + blk
                    sbk = SBANKS[n % 3]
                    pk = n % 3
                    S.op("pe", lambda: T.matmul(PB[sbk][:], skT[:, ktl * 128:(ktl + 1) * 128], sqT[:, g * 4:(g + 1) * 4, i * 128:(i + 1) * 128],
                                                start=True, stop=False, skip_group_check=True),
                         reads=[b_skT, b_sqT], writes=[PBb[sbk]])
                    S.op("pe", lambda: T.matmul(PB[sbk][:], ident_b[:], swaB_b[:, blk, g * 4:(g + 1) * 4, :].rearrange("p a b -> p (a b)"),
                                                start=False, stop=True, skip_group_check=True),
                         reads=[b_const], writes=[PBb[sbk]])
                    bias_ap = pm[:, 0:1] if (i == 0 and blk == 0) else zcol[:, 0:1]
                    S.op("act", lambda: A.activation(PTs[pk][:].rearrange("p a b -> p (a b)"), PB[sbk][:], AF.Exp, bias=bias_ap),
                         reads=[b_const], writes=[PBb[sbk], b_PT[pk]])

                def swa_B(itm):
                    n, i, g, blk = itm
                    ktl = i + blk
                    pk = n % 3
                    ob = 4 + ((n // 2) % 2)
                    for hh in range(4):
                        S.op("pe", lambda: T.matmul(PB[ob][:, hh * 65:hh * 65 + 65], PTs[pk][:, hh, :], svA[:, ktl, g, 0:65],
                                                    start=(blk == 0 and hh == 0), stop=False, skip_group_check=True),
                             reads=[b_PT[pk], b_svA], writes=[PBb[ob]])
                    if blk == 1:
                        ov = PB[ob][:, 0:260].rearrange("p (a b) -> p a b", a=4)
                        S.op("dve", lambda: V.tensor_tensor(srec[:], ov[:, :, 64], esink[:, g * 4:(g + 1) * 4], ALU.add),
                             reads=[b_small], writes=[PBb[ob], b_rec])
                        S.op("dve", lambda: V.reciprocal(srec[:], srec[:]), writes=[b_rec])
                        for hh in range(4):
                            hq = g * 4 + hh
                            S.op("dve", lambda: V.tensor_scalar(ystage[:, i, hq * 64:(hq + 1) * 64], ov[:, hh, 0:64], srec[:, hh:hh + 1], None, ALU.mult),
                                 reads=[b_rec], writes=[PBb[ob], b_ys])

                iters = []
                for i in range(8):
                    for g in range(2):
                        for blk in range(2):
                            iters.append((len(iters), i, g, blk))
                pipeline(iters, swa_A, swa_B)
                ystage_to_mixT(ystage, b_ys, 12)
                for g in range(4):
                    for tc in range(2):
                        bk = nb()
                        first = True
                        for j in range(4):
                            i = tc * 4 + j
                            kc, kp = (2, 3) if i == 0 else (0, 1)
                            S.op("pe", lambda: T.matmul(PB[bk][:, j * 128:(j + 1) * 128], ut[:, i + 1, g * 128:(g + 1) * 128], poolM[:, kc, g, :],
                                                        start=first, stop=False, skip_group_check=True),
                                 reads=[b_ut, b_const], writes=[PBb[bk]])
                            first = False
                            S.op("pe", lambda: T.matmul(PB[bk][:, j * 128:(j + 1) * 128], ut[:, i, g * 128:(g + 1) * 128], poolM[:, kp, g, :],
                                                        start=False, stop=False, skip_group_check=True),
                                 reads=[b_ut, b_const], writes=[PBb[bk]])
                        S.op("act", lambda: A.copy(pooledT[:], PB[bk][:]), writes=[PBb[bk], b_pl])
                        bk2 = nb()
                        S.op("pe", lambda: T.matmul(PB[bk2][:], pw[:, g, :], pooledT[:], start=True, stop=True),
                             reads=[b_pw, b_pl], writes=[PBb[bk2]])
                        S.op("dve", lambda: V.tensor_scalar(mixT[:, 8 + g, tc * 512:(tc + 1) * 512], PB[bk2][:], pscale[:, g:g + 1], None, ALU.mult),
                             reads=[b_small], writes=[PBb[bk2], b_mixT])
                S.barrier()
                if stop == "swa":
                    S.dma("sp", dbg, mixT[:].rearrange("p a b -> p (a b)"))
                    raise StopBuild()

        with Alloc(nc) as al:
            hbuf = al("hbuf", [128, 8, D], F32)
            stats = al("stats", [128, 4, 6], F32)
            mvA = al("mvA", [128, 8, 2], F32)
            rsA = al("rsA", [128, 8, 2], F32)
            gates = al("gates", [128, 8, 16], F32)
            b_h = [Buf("h%d" % i) for i in range(8)]
            b_lnw, b_ln, b_gates = Buf("lnw"), Buf("ln"), Buf("gates")

            with Alloc(nc) as al:
                wo0 = al("wo0", [128, 16, 256], BF16)
                wo1 = al("wo1", [128, 16, 256], BF16)
                lng = al("lng", [128, D], F32)
                lnb = al("lnb", [128, D], F32)
                xr0 = al("xr0", [128, 256], F32)
                xr1 = al("xr1", [128, 256], F32)
                xr2 = al("xr2", [128, 256], F32)
                hTf = al("hTf", [128, 16, 128], F32)
                wr_s = al("wr_s", [128, 16, 20], F32)
                rl = al("rl", [128, 20], F32)
                rt = al("rt", [128, 64], F32)
                wos = [wo0, wo1]
                b_wos = [Buf("wo0"), Buf("wo1")]
                xrs = [xr0, xr1, xr2]
                b_xrs = [Buf("xr%d" % i) for i in range(3)]
                b_hTf, b_wr, b_rl = Buf("hTf"), Buf("wr"), Buf("rl")
                S.dma("sp", lng[:], lnp[li, 0], writes=[b_lnw])
                S.dma("sp", lnb[:], lnp[li, 1], writes=[b_lnw])
                S.dma("sp", wr_s[:], w_r[li].rearrange("(c p) f -> p c f", p=128), writes=[b_wr])
                n = 0
                for cg in range(8):
                    k = cg % 2
                    S.dma("pool", wos[k][:], w_o[li][:, cg * 256:(cg + 1) * 256].rearrange("(c p) f -> p c f", p=128), writes=[b_wos[k]])
                    for i in range(8):
                        xk = n % 3
                        n += 1
                        S.dma("sp", xrs[xk][:], xo_f(i, cg * 256, (cg + 1) * 256), writes=[b_xrs[xk]])
                        bk = n % 4
                        for c in range(16):
                            S.op("pe", lambda: T.matmul(PB[bk][:, 0:256], mixT[:, c, i * 128:(i + 1) * 128], wos[k][:, c, :],
                                                        start=(c == 0), stop=(c == 15)),
                                 reads=[b_mixT, b_wos[k]], writes=[PBb[bk]])
                        S.op("dve", lambda: V.scalar_tensor_tensor(hbuf[:, i, cg * 256:(cg + 1) * 256], xrs[xk][:], ALPHA, PB[bk][:, 0:256],
                                                                   ALU.mult, ALU.add),
                             reads=[b_xrs[xk]], writes=[PBb[bk], b_h[i]])
                def p2_rest(i):

                    for g in range(4):
                        bi = 4 + (g % 2)
                        for j in range(4):
                            c = g * 4 + j
                            S.op("pe", lambda: T.transpose(PB[bi][:, j * 128:(j + 1) * 128], hbuf[:, i, c * 128:(c + 1) * 128], ident[:]),
                                 reads=[b_h[i], b_const], writes=[PBb[bi]])
                        pv = PB[bi][:].rearrange("p (a b) -> p a b", a=4)
                        S.op("dve", lambda: V.tensor_copy(hTf[:, g * 4:(g + 1) * 4, :], pv), writes=[PBb[bi], b_hTf])
                        S.op("act", lambda: A.copy(hT[:, g * 4:(g + 1) * 4, i * 128:(i + 1) * 128], pv), writes=[PBb[bi], b_hT])
                    for c in range(16):
                        S.op("pe", lambda: T.matmul(PB[6][:, 0:20], hTf[:, c, :], wr_s[:, c, :], start=(c == 0), stop=(c == 15)),
                             reads=[b_hTf, b_wr], writes=[PBb[6]])
                    S.op("dve", lambda: V.tensor_tensor(rl[:], PB[6][:, 0:20], rb_t, ALU.add), reads=[b_small], writes=[PBb[6], b_rl])
                    gl, el = rl[:, 0:4], rl[:, 4:20]
                    gmax, gsum, pen, em, m1, k1, m2, k2, w2, den = (rt[:, 0:1], rt[:, 1:2], rt[:, 4:8], rt[:, 8:24], rt[:, 2:3],
                                                                    rt[:, 24:40], rt[:, 3:4], rt[:, 40:56], rt[:, 56:57], rt[:, 57:58])
                    ge = rt[:, 58:62]
                    ops = [
                        lambda: V.tensor_reduce(gmax, gl, AX.X, ALU.max),
                        lambda: V.tensor_scalar(pen, gl, gmax, None, ALU.is_equal),
                        lambda: V.tensor_scalar(pen, pen, -1.0, 1e30, ALU.add, ALU.mult),
                        lambda: V.tensor_scalar(ge, gl, gmax, None, ALU.subtract),
                    ]
                    for f in ops:
                        S.op("dve", f, writes=[b_rl])
                    S.op("dve", lambda: V.memset(gsum, 0.0), writes=[b_rl])
                    S.op("act", lambda: A.activation(ge, ge, AF.Exp, accum_out=gsum), writes=[b_rl])
                    ops = [
                        lambda: V.tensor_tensor(em.rearrange("p (g e) -> p g e", g=4), el.rearrange("p (g e) -> p g e", g=4),
                                                pen.unsqueeze(2).to_broadcast([128, 4, 4]), ALU.add),
                        lambda: V.tensor_reduce(m1, em, AX.X, ALU.max),
                        lambda: V.tensor_scalar(k1, em, m1, None, ALU.is_equal),
                        lambda: V.scalar_tensor_tensor(em, k1, -1e30, em, ALU.mult, ALU.add),
                        lambda: V.tensor_reduce(m2, em, AX.X, ALU.max),
                        lambda: V.tensor_scalar(k2, em, m2, None, ALU.is_equal),
                        lambda: V.tensor_tensor(w2, m2, m1, ALU.subtract),
                    ]
                    for f in ops:
                        S.op("dve", f, writes=[b_rl])
                    S.op("act", lambda: A.activation(w2, w2, AF.Exp), writes=[b_rl])
                    ops = [
                        lambda: V.tensor_scalar(den, w2, 1.0, ALPHA, ALU.add, ALU.mult),
                        lambda: V.tensor_tensor(den, den, gsum, ALU.mult),
                        lambda: V.reciprocal(den, den),
                        lambda: V.tensor_tensor(w2, w2, den, ALU.mult),
                        lambda: V.tensor_scalar(k1, k1, den, None, ALU.mult),
                        lambda: V.scalar_tensor_tensor(gates[:, i, :], k2, w2, k1, ALU.mult, ALU.add),
                    ]
                    for f in ops[:-1]:
                        S.op("dve", f, writes=[b_rl])
                    S.op("dve", ops[-1], reads=[b_rl], writes=[b_gates])
                ln_stats_all(hbuf, b_h, b_ln, stats, mvA, rsA, LN_EPS)
                for i in range(8):
                    ln_apply_tile(hbuf, i, lng, lnb, b_h[i], b_ln, rsA, b_lnw)
                    if i >= 1:
                        p2_rest(i - 1)
                p2_rest(7)
                S.barrier()
                if stop == "p2":
                    for i in range(8):
                        S.dma("sp", out_f(i), hbuf[:, i, :])
                    S.dma("sp", dbg, hT[:].rearrange("p a b -> p (a b)"))
                    raise StopBuild()

            with Alloc(nc) as al:
                gu0 = al("gu0", [128, 16, 256], BF16)
                gu1 = al("gu1", [128, 16, 256], BF16)
                gu2 = al("gu2", [128, 16, 256], BF16)
                gu3 = al("gu3", [128, 16, 256], BF16)
                hidT = al("hidT", [128, 4, NT], BF16)
                sa0 = al("sa0", [128, 512], F32)
                sa1 = al("sa1", [128, 512], F32)
                pT = al("pT", [128, 2, NT], BF16)
                puw = al("puw", [128, 2, D], BF16)
                pin = al("pin", [128, 256], F32)
                gus = [gu0, gu1, gu2, gu3]
                b_gus = [Buf("gu%d" % i) for i in range(4)]
                mflat = mixT[:].rearrange("p a b -> p (a b)")
                dws = [mflat[:, k * 8192:(k + 1) * 8192].rearrange("p (c d) -> p c d", c=4) for k in range(2)]
                b_dws = [Buf("dw0"), Buf("dw1")]
                sas = [sa0, sa1]
                b_sas = [Buf("sa0"), Buf("sa1")]
                b_hid = [Buf("hid%d" % i) for i in range(4)]
                b_pT, b_puw, b_pin = Buf("pT"), Buf("puw"), Buf("pin")
                gn = 0
                san = 0
                yb = 0
                for e in range(16):
                    for hf in range(2):
                        kg, ku = gn % 4, (gn + 1) % 4
                        gn += 2
                        S.dma("pool", gus[kg][:], w_gate[li, e][:, hf * 256:(hf + 1) * 256].rearrange("(c p) f -> p c f", p=128), writes=[b_gus[kg]])
                        S.dma("pool", gus[ku][:], w_up[li, e][:, hf * 256:(hf + 1) * 256].rearrange("(c p) f -> p c f", p=128), writes=[b_gus[ku]])
                        for fc in range(2):
                            f4 = hf * 2 + fc
                            for tc in range(2):
                                ba, bu = (0, 1) if (fc + tc) % 2 == 0 else (2, 3)
                                for c in range(16):
                                    S.op("pe", lambda: T.matmul(PB[ba][:], gus[kg][:, c, fc * 128:(fc + 1) * 128], hT[:, c, tc * 512:(tc + 1) * 512],
                                                                start=(c == 0), stop=(c == 15)),
                                         reads=[b_gus[kg], b_hT], writes=[PBb[ba]])
                                for c in range(16):
                                    S.op("pe", lambda: T.matmul(PB[bu][:], gus[ku][:, c, fc * 128:(fc + 1) * 128], hT[:, c, tc * 512:(tc + 1) * 512],
                                                                start=(c == 0), stop=(c == 15)),
                                         reads=[b_gus[ku], b_hT], writes=[PBb[bu]])
                                sk_ = san % 2
                                san += 1
                                S.op("act", lambda: A.activation(sas[sk_][:], PB[ba][:], AF.Silu), writes=[PBb[ba], b_sas[sk_]])
                                S.op("dve", lambda: V.tensor_tensor(hidT[:, f4, tc * 512:(tc + 1) * 512], sas[sk_][:], PB[bu][:], ALU.mult),
                                     reads=[b_sas[sk_]], writes=[PBb[bu], b_hid[f4]])
                    kd = e % 2
                    S.dma("pool", dws[kd], w_down[li, e].rearrange("(c p) d -> p c d", p=128), writes=[b_dws[kd]])
                    for i in range(8):
                        for cg in range(4):
                            bk = 4 + (yb % 4)
                            yb += 1
                            for fc in range(4):
                                S.op("pe", lambda: T.matmul(PB[bk][:], hidT[:, fc, i * 128:(i + 1) * 128], dws[kd][:, fc, cg * 512:(cg + 1) * 512],
                                                            start=(fc == 0), stop=(fc == 3)),
                                     reads=[b_hid[fc], b_dws[kd]], writes=[PBb[bk]])
                            S.op("dve", lambda: V.scalar_tensor_tensor(hbuf[:, i, cg * 512:(cg + 1) * 512], PB[bk][:], gates[:, i, e:e + 1],
                                                                       hbuf[:, i, cg * 512:(cg + 1) * 512], ALU.mult, ALU.add),
                                 reads=[b_gates], writes=[PBb[bk], b_h[i]])
                S.dma("pool", puw[:], ple_uw[li].rearrange("(c p) d -> p c d", p=128), writes=[b_puw])
                for i in range(8):
                    S.dma("sp", pin[:], p_own[li, i * 128:(i + 1) * 128, :], writes=[b_pin])
                    for k in range(2):
                        S.op("pe", lambda: T.transpose(PB[0][:, k * 128:(k + 1) * 128], pin[:, k * 128:(k + 1) * 128], ident[:]),
                             reads=[b_pin, b_const], writes=[PBb[0]])
                    S.op("act", lambda: A.copy(pT[:, :, i * 128:(i + 1) * 128], PB[0][:, 0:256].rearrange("p (a b) -> p a b", a=2)),
                         writes=[PBb[0], b_pT])
                for cg in range(4):
                    kd = cg % 2
                    wv = mflat[:, kd * 8192:(kd + 1) * 8192].rearrange("p (c f) -> p c f", c=16)
                    S.dma("pool", wv, ple_gw[li][:, cg * 512:(cg + 1) * 512].rearrange("(c p) f -> p c f", p=128), writes=[b_dws[kd]])
                    for i in range(8):
                        ba, bu = (0, 1) if i % 2 == 0 else (2, 3)
                        for c in range(16):
                            S.op("pe", lambda: T.matmul(PB[ba][:], hT[:, c, i * 128:(i + 1) * 128], wv[:, c, :], start=(c == 0), stop=(c == 15)),
                                 reads=[b_hT, b_dws[kd]], writes=[PBb[ba]])
                        for k in range(2):
                            S.op("pe", lambda: T.matmul(PB[bu][:], pT[:, k, i * 128:(i + 1) * 128], puw[:, k, cg * 512:(cg + 1) * 512],
                                                        start=(k == 0), stop=(k == 1)),
                                 reads=[b_pT, b_puw], writes=[PBb[bu]])
                        sk_ = san % 2
                        san += 1
                        S.op("act", lambda: A.activation(sas[sk_][:], PB[ba][:], AF.Sigmoid), writes=[PBb[ba], b_sas[sk_]])
                        S.op("dve", lambda: V.scalar_tensor_tensor(sas[sk_][:], sas[sk_][:], 1.0 / ALPHA, PB[bu][:], ALU.mult, ALU.mult), writes=[PBb[bu], b_sas[sk_]])
                        S.op("dve", lambda: V.tensor_tensor(hbuf[:, i, cg * 512:(cg + 1) * 512], hbuf[:, i, cg * 512:(cg + 1) * 512], sas[sk_][:], ALU.add),
                             reads=[b_sas[sk_]], writes=[b_h[i]])
                S.barrier()

            with Alloc(nc) as al:
                lng = al("lng2", [128, D], F32)
                lnb = al("lnb2", [128, D], F32)
                S.dma("sp", lng[:], lnp[li, 2], writes=[b_lnw])
                S.dma("sp", lnb[:], lnp[li, 3], writes=[b_lnw])
                ln_stats_all(hbuf, b_h, b_ln, stats, mvA, rsA, LN_EPS / (ALPHA * ALPHA))
                for i in range(8):
                    ln_apply_tile(hbuf, i, lng, lnb, b_h[i], b_ln, rsA, b_lnw)
                    ev = S.dma("sp", out_f(i), hbuf[:, i, :], reads=[b_h[i]])
                    if post_out is not None:
                        post_out(i, hbuf[:, i, :], b_h[i])
                S.barrier()

    xo0 = rows_f(x_own)
    xp0 = lambda i: x_prev[i * 128:(i + 1) * 128, :]
    yo = lambda i: y_out[i * 128:(i + 1) * 128, :]
    if nL == 1:
        try:
            layer(0, layers[0], xo0, xp0, yo, actA, b_actA, actB, b_actB)
        except StopBuild:
            pass
    else:
        S.sems["cc"] = S._sem("cc")
        b_xall = [Buf("xall%d" % i) for i in range(8)]

        def exchange_tile(i, h_ap, b_hi):
            ev = S.dma("pool", xmid_b[i].ap(), h_ap, reads=[b_hi])
            S._wait("pool", {ev[0]: ev[1]})
            P.collective_compute("AllGather", ALU.bypass, replica_groups=[[0, 1], [2, 3], [4, 5], [6, 7]],
                                 ins=[xmid_b[i].ap().opt()], outs=[xall_b[i].ap().opt()]).then_inc(S.sems["cc"], 1)
            b_xall[i].last_w = ("cc", i + 1)

        layer(0, layers[0], xo0, xp0, lambda i: xmid[i].ap(), actA, b_actA, actB, b_actB, post_out=exchange_tile)
        S.dma_last["cc"] = 8
        layer(1, layers[1], lambda i, c0, c1: xmid[i].ap()[:, c0:c1], lambda i: xall_b[i].ap()[0:128, :], yo,
              actA, b_actA, actB, b_actB, xp_b=b_xall, xo_T_f=lambda i: xmid_b[i].ap())
    S.barrier()
    S.close()
    for cm in reversed(es):
        cm.__exit__(None, None, None)
    return nc


def _prep_shared(inputs, layers):
    f = lambda a: np.ascontiguousarray(np.asarray(a, dtype=np.float32))
    L = list(layers)
    sh = {}
    sh["w_in"] = f(inputs["w_in"][L])
    sh["w_o"] = f(inputs["w_o"][L])
    sh["w_gate"] = f(inputs["w_gate"][L])
    sh["w_up"] = f(inputs["w_up"][L])
    sh["w_down"] = f(inputs["w_down"][L])
    sh["ple_gw"] = f(inputs["ple_gate_w"][L])
    sh["ple_uw"] = f(inputs["ple_up_w"][L])
    sh["pool_w"] = f(inputs["pool_w"][L])
    sh["w_r"] = f(np.concatenate([inputs["router_g_w"][L], inputs["router_e_w"][L]], axis=-1))
    nL = len(L)
    lnp = np.empty((nL, 4, 128, D), np.float32)
    small = np.zeros((nL, 128, 448), np.float32)
    for j, l in enumerate(L):
        for k, nm in enumerate(("ln1_g", "ln1_b", "ln2_g", "ln2_b")):
            lnp[j, k] = np.broadcast_to(inputs[nm][l][None, :], (128, D))
        small[j, :, 0:8] = inputs["b_f"][l][None, :]
        small[j, :, 8:16] = inputs["sinks"][l][None, :]
        for k, nm in enumerate(("lam_q1", "lam_k1", "lam_q2", "lam_k2")):
            small[j, :, 16 + 64 * k:16 + 64 * (k + 1)] = inputs[nm][l][None, :]
        small[j, :, 272:400] = inputs["diff_norm_g"][l][None, :]
        small[j, :, 400:404] = inputs["pool_scale"][l].reshape(4, 128).T
        small[j, :, 404:408] = inputs["router_g_b"][l][None, :]
        small[j, :, 408:424] = inputs["router_e_b"][l][None, :]
    sh["lnp"] = lnp
    sh["small"] = small
    cs = _static_consts()
    diffB, swaB, b31 = _bias_tiles(np.asarray(inputs["rel_table"], np.float32))
    for nm in ("ident", "cmask", "tri", "ones", "sel", "poolAc", "poolAp"):
        sh["c_" + nm] = cs[nm]
    sh["c_diffB"], sh["c_swaB"], sh["c_b31"] = diffB, swaB, b31
    return sh, cs


_PROG_CACHE = {}


def _run(layers, fused, x_full, inputs):
    key = (tuple(layers), fused)
    if key not in _PROG_CACHE:
        _PROG_CACHE[key] = build_program(list(layers), fused)
    nc = _PROG_CACHE[key]
    sh, cs = _prep_shared(inputs, layers)
    p = np.asarray(inputs["p"], np.float32)
    in_maps = []
    for c in range(8):
        b, half = c // 2, c % 2
        m = dict(sh)
        m["x_own"] = np.ascontiguousarray(x_full[b, half * NT:(half + 1) * NT])
        m["x_prev"] = np.ascontiguousarray(x_full[b, 0:NT])
        m["p_own"] = np.ascontiguousarray(p[list(layers), b, half * NT:(half + 1) * NT])
        m["c_pm"] = np.full((128, 1), 0.0 if half == 1 else NEG, np.float32)
        m["c_poolA0"] = cs["poolAc"] if half == 1 else cs["poolAf"]
        m["c_poolAp0"] = cs["poolAp"] if half == 1 else np.zeros_like(cs["poolAp"])
        in_maps.append(m)
    res = run_bass_kernel_spmd(nc, in_maps, core_ids=list(range(8)))
    out = np.empty((4, SEQ, D), np.float32)
    for c in range(8):
        b, half = c // 2, c % 2
        out[b, half * NT:(half + 1) * NT] = res.results[c]["y_out"]
    return out


def kernel(**inputs):
    x = np.asarray(inputs["x"], np.float32)
    if FUSED:
        return _run((0, 1), True, x, inputs)
    x1 = _run((0,), False, x, inputs)
    return _run((1,), False, x1, inputs)
```

```python
import math
import os
import numpy as np
import concourse.bass as bass
import concourse.mybir as mybir
from concourse.bass_utils import run_bass_kernel_spmd

F32 = mybir.dt.float32
BF16 = mybir.dt.bfloat16
AF = mybir.ActivationFunctionType
ALU = mybir.AluOpType
AX = mybir.AxisListType

D = 2048
SEQ = 2048
NT = 1024
DEPTH = 2
IN_W = 4360
OFF = dict(fq=0, fk=512, fv=1024, ff=1536, dq=1544, dk=2056, dv=2568, pu=3080, sq=3592, sk=4104, sv=4232)
ALPHA = (2 * DEPTH) ** 0.25
LN_EPS = 1e-5
NEG = -30000.0
FUSED = True


class Buf:
    __slots__ = ("name", "last_w", "readers")

    def __init__(self, name):
        self.name = name
        self.last_w = None
        self.readers = {}


class Sched:
    def __init__(self, nc, n_dma_sems=6):
        self.nc = nc
        self.sems = {}
        self.count = {}
        self.known = {}
        self.engs = {"pe": nc.tensor, "act": nc.scalar, "dve": nc.vector, "pool": nc.gpsimd, "sp": nc.sync}
        self._ctx = []
        for e in self.engs:
            self.known[e] = {}
        for e in ("pe", "act", "dve", "pool"):
            self.sems[e] = self._sem("s_" + e)
            self.count[e] = 0
        self.dma_n = {}
        self.dma_last = {}
        self.n_dma_sems = n_dma_sems
        for q in ("sp", "pool"):
            self.dma_n[q] = 0
            for i in range(n_dma_sems):
                self.sems[(q, i)] = self._sem("d_%s%d" % (q, i))
                self.dma_last[(q, i)] = 0

    def _sem(self, name):
        cm = self.nc.semaphore(name)
        h = cm.__enter__()
        self._ctx.append(cm)
        return h

    def close(self):
        for cm in reversed(self._ctx):
            cm.__exit__(None, None, None)

    def _deps(self, reads, writes):
        deps = {}

        def add(ev):
            if ev is None:
                return
            k, v = ev
            if deps.get(k, 0) < v:
                deps[k] = v
        for b in reads:
            add(b.last_w)
        for b in writes:
            add(b.last_w)
            for k, v in b.readers.items():
                add((k, v))
        return deps

    def _wait(self, ename, deps, skip_self=False):
        eng = self.engs[ename]
        kn = self.known[ename]
        for k, v in deps.items():
            if skip_self and k == ename:
                continue
            if kn.get(k, 0) >= v:
                continue
            eng.wait_ge(self.sems[k], v)
            kn[k] = v

    def _mark(self, ev, reads, writes):
        k, v = ev
        for b in writes:
            b.last_w = ev
            b.readers = {}
        for b in reads:
            if b.readers.get(k, 0) < v:
                b.readers[k] = v

    def op(self, ename, fn, reads=(), writes=()):
        deps = self._deps(reads, writes)
        self._wait(ename, deps, skip_self=(ename == "pe"))
        ins = fn()
        self.count[ename] += 1
        ins.then_inc(self.sems[ename], 1)
        ev = (ename, self.count[ename])
        self._mark(ev, reads, writes)
        return ev

    def dma(self, q, out, in_, reads=(), writes=()):
        deps = self._deps(reads, writes)
        n = self.dma_n[q]
        slot = n % self.n_dma_sems
        rnd = n // self.n_dma_sems
        key = (q, slot)
        if rnd > 0:
            deps[key] = max(deps.get(key, 0), 16 * rnd)
        self._wait(q, deps)
        self.engs[q].dma_start(out=out, in_=in_).then_inc(self.sems[key], 16)
        self.dma_n[q] = n + 1
        self.dma_last[key] = 16 * (rnd + 1)
        ev = (key, 16 * (rnd + 1))
        self._mark(ev, reads, writes)
        return ev

    def barrier(self):
        tgt = {}
        for e in ("pe", "act", "dve", "pool"):
            if self.count[e] > 0:
                tgt[e] = self.count[e]
        for k, v in self.dma_last.items():
            if v > 0:
                tgt[k] = v
        for e in self.engs:
            self._wait(e, tgt, skip_self=False)


class Alloc:
    def __init__(self, nc):
        self.nc = nc
        self.cms = []

    _n = [0]

    def __call__(self, name, shape, dt=F32):
        Alloc._n[0] += 1
        cm = self.nc.sbuf_tensor("%s_%d" % (name, Alloc._n[0]), list(shape), dt)
        t = cm.__enter__()
        self.cms.append(cm)
        return t

    def __enter__(self):
        return self

    def __exit__(self, *a):
        for cm in reversed(self.cms):
            cm.__exit__(None, None, None)
        return False


def _t5_bucket_np(n):
    n = np.asarray(n, dtype=np.int32)
    max_exact = 16
    nf = np.maximum(n, 1).astype(np.float32)
    large = max_exact + (np.log(nf / np.float32(max_exact)) / np.float32(math.log(128 / max_exact))
                         * np.float32(32 - max_exact)).astype(np.int32)
    large = np.minimum(large, 31)
    return np.where(n < max_exact, n, large)


def _static_consts():
    c = {}
    c["ident"] = np.eye(128, dtype=np.float32)
    s = np.arange(128)[:, None]
    t = np.arange(128)[None, :]
    c["cmask"] = np.where(s <= t, 0.0, NEG).astype(np.float32)
    c["tri"] = (s <= t).astype(np.float32)
    c["ones"] = np.ones((128, 128), np.float32)
    sel = np.zeros((128, 8, 128), np.float32)
    for h in range(8):
        for b in (0, 32, 64):
            sel[b + h, h, :] = 1.0
    c["sel"] = sel
    wins = (2, 4, 8, 16)
    Ac = np.zeros((4, 128, 128), np.float32)
    Ap = np.zeros((4, 128, 128), np.float32)
    Af = np.zeros((4, 128, 128), np.float32)
    for g, w in enumerate(wins):
        for tt in range(128):
            for ss in range(tt - w + 1, tt + 1):
                if ss >= 0:
                    Ac[g, ss, tt] += 1.0 / w
                    Af[g, ss, tt] += 1.0 / min(tt + 1, w)
                else:
                    Ap[g, 128 + ss, tt] += 1.0 / w
            Ac[g, tt, tt] -= 1.0
            Af[g, tt, tt] -= 1.0
    c["poolAc"], c["poolAp"], c["poolAf"] = Ac, Ap, Af
    return c


def _bias_tiles(rel_table):
    bk = _t5_bucket_np(np.arange(256))
    bbd = rel_table[bk]
    s = np.arange(128)[:, None]
    t = np.arange(128)[None, :]
    dD = t - s
    dA = 128 + t - s
    diffB = np.empty((128, 4, 2, 128), np.float32)
    for h in range(4):
        diffB[:, h, 0, :] = np.where(dD >= 0, bbd[np.clip(dD, 0, 255), h], NEG)
        diffB[:, h, 1, :] = bbd[np.clip(dA, 0, 255), h]
    swaB = np.empty((128, 2, 8, 128), np.float32)
    for h in range(8):
        swaB[:, 0, h, :] = np.where(dA < 128, bbd[np.clip(dA, 0, 127), 4 + h], NEG)
        swaB[:, 1, h, :] = np.where(dD >= 0, bbd[np.clip(dD, 0, 127), 4 + h], NEG)
    b31 = np.broadcast_to(rel_table[31, :4][None, :], (128, 4)).astype(np.float32).copy()
    return diffB, swaB, b31


class StopBuild(Exception):
    pass


def build_program(layers, fused, stop=None):
    nc = bass.Bass("TRN2", target_bir_lowering=False)
    dbg = nc.dram_tensor("dbg", [128, 16 * NT], BF16, kind="ExternalOutput").ap() if stop else None
    nL = len(layers)

    def din(name, shape):
        return nc.dram_tensor(name, list(shape), F32, kind="ExternalInput").ap()

    x_own = din("x_own", [NT, D])
    x_prev = din("x_prev", [NT, D])
    p_own = din("p_own", [nL, NT, 256])
    w_in = din("w_in", [nL, D, IN_W])
    w_o = din("w_o", [nL, D, D])
    small_w = stop is not None and stop != "p3"
    w_gate = din("w_gate", [nL, 1 if small_w else 16, D, 512])
    w_up = din("w_up", [nL, 1 if small_w else 16, D, 512])
    w_down = din("w_down", [nL, 1 if small_w else 16, 512, D])
    ple_gw = din("ple_gw", [nL, D, D])
    ple_uw = din("ple_uw", [nL, 256, D])
    pool_w = din("pool_w", [nL, 4, 128, 128])
    w_r = din("w_r", [nL, D, 20])
    lnp = din("lnp", [nL, 4, 128, D])
    small = din("small", [nL, 128, 448])
    cst = {}
    for nm, shp in (("ident", [128, 128]), ("cmask", [128, 128]), ("tri", [128, 128]), ("ones", [128, 128]),
                    ("sel", [128, 8, 128]), ("poolAc", [4, 128, 128]), ("poolAp", [4, 128, 128]),
                    ("poolA0", [4, 128, 128]), ("poolAp0", [4, 128, 128]),
                    ("diffB", [128, 4, 2, 128]), ("swaB", [128, 2, 8, 128]), ("b31", [128, 4]), ("pm", [128, 1])):
        cst[nm] = din("c_" + nm, shp)
    y_out = nc.dram_tensor("y_out", [NT, D], F32, kind="ExternalOutput").ap()
    if fused and nL == 2:
        xmid = [nc.dram_tensor("xmid%d" % i, [128, D], F32) for i in range(8)]
        xall = [nc.dram_tensor("xall%d" % i, [256, D], F32) for i in range(8)]

    def rows_f(ap):
        return lambda i, c0, c1: ap[i * 128:(i + 1) * 128, c0:c1]

    S = Sched(nc)
    T, V, A, P = nc.tensor, nc.vector, nc.scalar, nc.gpsimd
    es = []

    def sb(name, shape, dt=F32):
        cm = nc.sbuf_tensor(name, list(shape), dt)
        t = cm.__enter__()
        es.append(cm)
        return t

    def psum(name):
        cm = nc.psum_tensor(name, [128, 512], F32)
        t = cm.__enter__()
        es.append(cm)
        return t

    PB = [psum("pb%d" % i) for i in range(8)]
    PBb = [Buf("pb%d" % i) for i in range(8)]

    actA = sb("actA", [128, 16, NT], BF16)
    actB = sb("actB", [128, 16, NT], BF16)
    b_actA, b_actB = Buf("actA"), Buf("actB")
    ident = sb("ident", [128, 128]); b_const = Buf("const")
    ident_b = sb("ident_b", [128, 128], BF16)
    cmask_b = sb("cmask_b", [128, 128], BF16)
    tri = sb("tri", [128, 128], BF16)
    ones = sb("ones", [128, 128], BF16)
    sel_b = sb("sel_b", [128, 8, 128], BF16)
    poolM = sb("poolM", [128, 4, 4, 128], BF16)
    DA2 = sb("DA2", [128, 4, 2, 256], BF16)
    swaB_b = sb("swaB_b", [128, 2, 8, 128], BF16)
    b31 = sb("b31", [128, 4])
    pm = sb("pm", [128, 1])
    bcol = sb("bcol", [128, 2, 4])
    zcol = sb("zcol", [128, 1])
    smalls = sb("smalls", [128, 448])
    b_small = Buf("small")
    lamc = sb("lamc", [128, 8])
    esink = sb("esink", [128, 8])
    gsub = sb("gsub", [128, 128])

    lamtmp = sb("lamtmp", [128, 128])
    b_stage = Buf("stage")
    stage_cm = nc.sbuf_tensor("stage", [128, 2048], F32)
    stage = stage_cm.__enter__()

    def load_const_f32(dst, src_ap):
        S.dma("sp", dst, src_ap, writes=[b_const])

    def load_cast(dst_ap, src_ap, n_free, view=None):
        st = stage[:src_ap.shape[0], 0:n_free]
        if view is not None:
            st = view(st)
        S.dma("sp", st, src_ap, writes=[b_stage])
        S.op("dve", lambda: V.tensor_copy(dst_ap, st), reads=[b_stage], writes=[b_const])

    load_const_f32(ident[:], cst["ident"][:, :])
    load_cast(tri[:], cst["tri"][:, :], 128)
    load_cast(ones[:], cst["ones"][:, :], 128)
    load_const_f32(b31[:], cst["b31"][:, :])
    load_const_f32(pm[:], cst["pm"][:, :])
    load_cast(ident_b[:], cst["ident"][:, :], 128)
    load_cast(cmask_b[:], cst["cmask"][:, :], 128)
    load_cast(sel_b[:], cst["sel"][:, :, :], 1024, view=lambda a: a.rearrange("p (a b) -> p a b", a=8))
    for k, nm in enumerate(("poolAc", "poolAp", "poolA0", "poolAp0")):
        load_cast(poolM[:, k, :, :], cst[nm].rearrange("g s t -> s g t"), 512,
                  view=lambda a: a.rearrange("p (a b) -> p a b", a=4))
    load_cast(swaB_b[:], cst["swaB"][:, :, :, :], 2048,
              view=lambda a: a.rearrange("p (a b c) -> p a b c", a=2, b=8))
    S.dma("sp", stage[:, 0:1024].rearrange("p (h k t) -> p h k t", h=4, k=2), cst["diffB"][:, :, :, :], writes=[b_stage])
    for h in range(4):
        for m in range(2):
            S.op("dve", lambda: V.tensor_scalar(
                DA2[:, h, m, :], stage[:, h * 256:(h + 1) * 256], b31[:, h:h + 1], None, ALU.subtract),
                reads=[b_stage, b_const], writes=[b_const])
    S.op("dve", lambda: V.memset(zcol[:], 0.0), writes=[b_const])
    S.op("dve", lambda: V.tensor_copy(bcol[:, 0, :], b31[:]), reads=[b_const], writes=[b_const])
    S.op("dve", lambda: V.tensor_scalar(bcol[:, 1, :], b31[:], pm[:, 0:1], None, ALU.add), reads=[b_const], writes=[b_const])

    S.barrier()
    stage_cm.__exit__(None, None, None)
    bank_rr = [0]

    def transposes_to_T(src_tile_fn, nchunks, dst, dst_buf, tok0, src_bufs, banks, extra=None):
        for g in range(nchunks // 4):
            bi = banks[g % len(banks)]
            for j in range(4):
                c = g * 4 + j
                S.op("pe", lambda: T.transpose(PB[bi][:, j * 128:(j + 1) * 128], src_tile_fn(c), ident[:]),
                     reads=src_bufs + [b_const], writes=[PBb[bi]])
            pv = PB[bi][:].rearrange("p (a b) -> p a b", a=4)
            if extra is not None:
                extra(g, pv, PBb[bi])
            eng = "act" if g % 2 == 0 else "dve"
            if eng == "act":
                S.op("act", lambda: A.copy(dst[:, g * 4:(g + 1) * 4, tok0:tok0 + 128], pv), writes=[PBb[bi], dst_buf])
            else:
                S.op("dve", lambda: V.tensor_copy(dst[:, g * 4:(g + 1) * 4, tok0:tok0 + 128], pv), writes=[PBb[bi], dst_buf])

    def ln_stats_all(hb, b_hs, b_ln, stats, mvA, rsA, eps):
        for i in range(8):
            for c in range(4):
                S.op("dve", lambda: V.bn_stats(stats[:, c, :], hb[:, i, c * 512:(c + 1) * 512]), reads=[b_hs[i]], writes=[b_ln])
            S.op("dve", lambda: V.bn_aggr(mvA[:, i, :], stats[:]), writes=[b_ln])
        S.op("dve", lambda: V.tensor_scalar(rsA[:, :, 0], mvA[:, :, 1], eps, None, ALU.add), writes=[b_ln])
        S.op("act", lambda: A.activation(rsA[:, :, 0], rsA[:, :, 0], AF.Ln), writes=[b_ln])
        S.op("act", lambda: A.activation(rsA[:, :, 0], rsA[:, :, 0], AF.Exp, scale=-0.5), writes=[b_ln])
        S.op("dve", lambda: V.scalar_tensor_tensor(rsA[:, :, 1], mvA[:, :, 0], -1.0, rsA[:, :, 0], ALU.mult, ALU.mult), writes=[b_ln])

    def ln_apply_tile(hb, i, gt, bt, b_h, b_ln, rsA, b_lnw):
        S.op("act", lambda: A.activation(hb[:, i, :], hb[:, i, :], AF.Identity, bias=rsA[:, i, 1:2], scale=rsA[:, i, 0:1]),
             reads=[b_ln], writes=[b_h])
        S.op("dve", lambda: V.tensor_tensor(hb[:, i, :], hb[:, i, :], gt[:], ALU.mult), reads=[b_lnw], writes=[b_h])
        S.op("dve", lambda: V.tensor_tensor(hb[:, i, :], hb[:, i, :], bt[:], ALU.add), reads=[b_lnw], writes=[b_h])

    def layer(li, l, xo_f, xp_f, out_f, xT, b_xT, mixT, b_mixT, xp_b=None, post_out=None):
        lam_init = 0.8 - 0.6 * math.exp(-0.3 * l)
        hT, b_hT = xT, b_xT

        S.dma("sp", smalls[:], small[li], writes=[b_small])
        bf_t = smalls[:, 0:8]
        sink_t = smalls[:, 8:16]
        lam_v = smalls[:, 16:272].rearrange("p (a b) -> p a b", a=4)
        gsub_raw = smalls[:, 272:400]
        pscale = smalls[:, 400:404]
        rb_t = smalls[:, 404:424]
        S.op("dve", lambda: V.tensor_tensor(lamtmp[:, 0:64], lam_v[:, 0, :], lam_v[:, 1, :], ALU.mult), reads=[b_small], writes=[b_stage])
        S.op("dve", lambda: V.tensor_tensor(lamtmp[:, 64:128], lam_v[:, 2, :], lam_v[:, 3, :], ALU.mult), reads=[b_small], writes=[b_stage])
        S.op("dve", lambda: V.tensor_reduce(lamc[:, 0:2], lamtmp[:, 0:128].rearrange("p (a b) -> p a b", a=2), AX.X, ALU.add),
             reads=[b_stage], writes=[b_small])
        S.op("act", lambda: A.activation(lamc[:, 2:4], lamc[:, 0:2], AF.Exp), writes=[b_small])
        S.op("dve", lambda: V.tensor_tensor(lamc[:, 4:5], lamc[:, 3:4], lamc[:, 2:3], ALU.subtract), writes=[b_small])
        S.op("dve", lambda: V.tensor_scalar(lamc[:, 5:6], lamc[:, 4:5], -lam_init, None, ALU.add), writes=[b_small])
        S.op("act", lambda: A.activation(esink[:], sink_t, AF.Exp), reads=[b_small], writes=[b_small])
        S.op("dve", lambda: V.tensor_scalar(gsub[:], gsub_raw, 1.0 - lam_init, None, ALU.mult), writes=[b_small])
        neglam = lamc[:, 5:6]

        with Alloc(nc) as al:
            xTp = al("xTp", [128, 16, NT], BF16)
            wr0 = al("wr0", [128, 16, 256], BF16)
            wr1 = al("wr1", [128, 16, 256], BF16)
            b_xTp = Buf("xTp")
            wring = [wr0, wr1]
            b_wring = [Buf("wr0"), Buf("wr1")]
            wn = [0]

            pref = {}

            def load_w(c0, ncols):
                if (c0, ncols) in pref:
                    return pref.pop((c0, ncols))
                k = wn[0] % 2
                wn[0] += 1
                S.dma("pool", wring[k][:, :, 0:ncols], w_in[li][:, c0:c0 + ncols].rearrange("(c p) f -> p c f", p=128),
                      writes=[b_wring[k]])
                return wring[k], b_wring[k]

            def prefetch_w(c0, ncols):
                r = load_w(c0, ncols)
                pref[(c0, ncols)] = r

            with Alloc(nc) as al:
                xin = [al("xin%d" % j, [128, D], BF16) for j in range(4)]
                b_xin = [Buf("xin%d" % j) for j in range(4)]
                nt = 0
                for which, (src, dst, bdst) in enumerate(((lambda i: xo_f(i, 0, D), xT, b_xT), (xp_f, xTp, b_xTp))):
                    for i in range(8):
                        k = (which * 8 + i) % 4
                        S.dma("pool", xin[k][:], src(i), reads=([xp_b[i]] if (which == 1 and xp_b is not None) else []), writes=[b_xin[k]])
                        for g in range(2):
                            bi = nt % 2
                            nt += 1
                            pbb = PB[bi][:].bitcast(BF16)
                            for j in range(8):
                                c = g * 8 + j
                                S.op("pe", lambda: T.transpose(pbb[:, j * 128:(j + 1) * 128], xin[k][:, c * 128:(c + 1) * 128], ident_b[:]),
                                     reads=[b_xin[k], b_const], writes=[PBb[bi]])
                            pv = pbb.rearrange("p (a b) -> p a b", a=8)
                            if g == 0:
                                S.op("act", lambda: A.copy(dst[:, 0:8, i * 128:(i + 1) * 128], pv), writes=[PBb[bi], bdst])
                            else:
                                S.op("dve", lambda: V.tensor_copy(dst[:, 8:16, i * 128:(i + 1) * 128], pv), writes=[PBb[bi], bdst])
                prefetch_w(OFF["fq"], 256)
                S.barrier()
                if stop == "p0":
                    S.dma("sp", dbg, xT[:].rearrange("p a b -> p (a b)"))
                    raise StopBuild()

            def proj_fm(c0, M, srcT, b_src, tok0, ntok, dst_fn, scale, wt, b_w, wc0, bank):
                for c in range(16):
                    S.op("pe", lambda: T.matmul(PB[bank][0:M, 0:ntok], wt[:, c, wc0:wc0 + M], srcT[:, c, tok0:tok0 + ntok],
                                                start=(c == 0), stop=(c == 15)),
                         reads=[b_w, b_src], writes=[PBb[bank]])
                dst, b_dst = dst_fn()
                if isinstance(dst, tuple):
                    S.op("act", lambda: A.activation(dst[0], PB[bank][0:64, 0:ntok], AF.Copy, scale=scale),
                         writes=[PBb[bank], b_dst])
                    S.op("act", lambda: A.activation(dst[1], PB[bank][64:128, 0:ntok], AF.Copy, scale=scale),
                         writes=[PBb[bank], b_dst])
                else:
                    S.op("act", lambda: A.activation(dst, PB[bank][0:M, 0:ntok], AF.Copy, scale=scale),
                         writes=[PBb[bank], b_dst])

            def proj_tm(srcT, b_src, tile, ncols, wt, b_w, wc0, bank):
                for c in range(16):
                    S.op("pe", lambda: T.matmul(PB[bank][:, 0:ncols], srcT[:, c, tile * 128:(tile + 1) * 128], wt[:, c, wc0:wc0 + ncols],
                                                start=(c == 0), stop=(c == 15)),
                         reads=[b_w, b_src], writes=[PBb[bank]])

            def nb():
                bank_rr[0] ^= 1
                return bank_rr[0]

            SBANKS = (2, 3, 0)

            def pipeline(iters, fa, fb, depth=2):
                N = len(iters)
                for n in range(min(depth, N)):
                    fa(iters[n])
                for n in range(N):
                    if n + depth < N:
                        fa(iters[n + depth])
                    fb(iters[n])

            def ystage_to_mixT(ystage, b_ys, base_chunk):
                pbb = PB[7][:].bitcast(BF16)
                for i in range(8):
                    for cc in range(4):
                        S.op("pe", lambda: T.transpose(pbb[:, cc * 128:(cc + 1) * 128], ystage[:, i, cc * 128:(cc + 1) * 128], ident_b[:]),
                             reads=[b_ys, b_const], writes=[PBb[7]])
                    S.op("dve", lambda: V.tensor_copy(mixT[:, base_chunk:base_chunk + 4, i * 128:(i + 1) * 128],
                                                      pbb[:, 0:512].rearrange("p (a b) -> p a b", a=4)),
                         writes=[PBb[7], b_mixT])

            with Alloc(nc) as al:
                fkT = al("fkT", [128, 4, 2 * NT], BF16)
                fvA = al("fvA", [128, 16, 8, 66], BF16)
                fqT = al("fqT", [128, 4, 2, NT], BF16)
                zf = al("zf", [128, 16, 8], F32)
                lsf = al("lsf", [128, 16, 8], F32)
                ls3 = al("ls3", [128, 16, 3, 8], BF16)
                tmpf = al("tmpf", [128, 16, 8], F32)
                cf = al("cf", [128, 16, 8], F32)
                negc = al("negc", [128, 16, 8], F32)
                cpad = al("cpad", [128, 8, 128], BF16)
                c3 = al("c3", [128, NT], BF16)
                PT0 = al("PT0", [128, 512], BF16)
                PT1 = al("PT1", [128, 512], BF16)
                PT2 = al("PT2", [128, 512], BF16)
                rec = al("rec", [128, 8], F32)
                ystage = al("ystage", [128, 8, 512], BF16)
                b_fkT, b_fvA, b_fqT, b_c, b_c3, b_rec, b_ys = [Buf(n) for n in "fkT fvA fqT c c3 rec ys".split()]
                PTs = [PT0, PT1, PT2]
                b_PT = [Buf("PT%d" % i) for i in range(3)]
                S.op("dve", lambda: V.memset(fvA[:, :, :, 64:65], 1.0), writes=[b_fvA])
                S.op("dve", lambda: V.memset(fqT[:], 0.0), writes=[b_fqT])
                for grp in range(2):
                    wt, b_w = load_w(OFF["fq"] + grp * 256, 256)
                    for sl in range(2):
                        pr = grp * 2 + sl
                        for tc in range(2):
                            proj_fm(0, 128, xT, b_xT, tc * 512, 512,
                                    lambda: ((fqT[0:64, pr, 0, tc * 512:(tc + 1) * 512], fqT[64:128, pr, 1, tc * 512:(tc + 1) * 512]), b_fqT),
                                    0.125, wt, b_w, sl * 128, nb())
                for grp in range(2):
                    wt, b_w = load_w(OFF["fk"] + grp * 256, 256)
                    for sl in range(2):
                        pr = grp * 2 + sl
                        for half, (src, bs) in enumerate(((xTp, b_xTp), (xT, b_xT))):
                            for tc in range(2):
                                proj_fm(0, 128, src, bs, tc * 512, 512,
                                        lambda: (fkT[:, pr, half * NT + tc * 512: half * NT + (tc + 1) * 512], b_fkT),
                                        1.0, wt, b_w, sl * 128, nb())
                for grp in range(2):
                    wt, b_w = load_w(OFF["fv"] + grp * 256, 256)
                    for kt in range(16):
                        src, bs = (xTp, b_xTp) if kt < 8 else (xT, b_xT)
                        bk = nb()
                        proj_tm(src, bs, kt % 8, 256, wt, b_w, 0, bk)
                        S.op("act", lambda: A.copy(fvA[:, kt, grp * 4:(grp + 1) * 4, 0:64],
                                                   PB[bk][:, 0:256].rearrange("p (a b) -> p a b", a=4)),
                             writes=[PBb[bk], b_fvA])
                wt, b_w = load_w(OFF["ff"] + 8 - 256, 256)
                for kt in range(16):
                    src, bs = (xTp, b_xTp) if kt < 8 else (xT, b_xT)
                    bk = nb()
                    proj_tm(src, bs, kt % 8, 8, wt, b_w, 248, bk)
                    S.op("dve", lambda: V.tensor_tensor(zf[:, kt, :], PB[bk][:, 0:8], bf_t, ALU.add),
                         reads=[b_small], writes=[PBb[bk], b_c])
                if stop == "fox_proj":
                    S.barrier()
                    S.dma("sp", dbg[:, 0:4 * NT], fqT[:].rearrange("p a b -> p (a b)"))
                    S.dma("sp", dbg[:, 4 * NT:12 * NT], fkT[:].rearrange("p a b -> p (a b)"))
                    raise StopBuild()
                S.op("dve", lambda: V.tensor_scalar(tmpf[:], zf[:], -1.0, None, ALU.mult), writes=[b_c])
                S.op("dve", lambda: V.tensor_tensor(tmpf[:], tmpf[:], zf[:], ALU.max), writes=[b_c])
                S.op("act", lambda: A.activation(tmpf[:], tmpf[:], AF.Exp, scale=-1.0), writes=[b_c])
                S.op("act", lambda: A.activation(tmpf[:], tmpf[:], AF.Ln, bias=1.0), writes=[b_c])
                S.op("dve", lambda: V.tensor_scalar(lsf[:], zf[:], 0.0, None, ALU.min), writes=[b_c])
                S.op("dve", lambda: V.tensor_tensor(lsf[:], lsf[:], tmpf[:], ALU.subtract), writes=[b_c])
                S.op("dve", lambda: V.tensor_copy(ls3[:, :, 0, :], lsf[:]), writes=[b_c])
                S.op("dve", lambda: V.tensor_tensor(tmpf[:], lsf[:], ls3[:, :, 0, :], ALU.subtract), writes=[b_c])
                S.op("dve", lambda: V.tensor_copy(ls3[:, :, 1, :], tmpf[:]), writes=[b_c])
                S.op("dve", lambda: V.tensor_tensor(tmpf[:], tmpf[:], ls3[:, :, 1, :], ALU.subtract), writes=[b_c])
                S.op("dve", lambda: V.tensor_copy(ls3[:, :, 2, :], tmpf[:]), writes=[b_c])
                first = True
                for kt in range(16):
                    for k2 in range(kt + 1):
                        lhs = tri if k2 == kt else ones
                        S.op("pe", lambda: T.matmul(PB[6][:, kt * 24:(kt + 1) * 24], lhs[:], ls3[:, k2, :, :], start=first, stop=False,
                                                    skip_group_check=True),
                             reads=[b_c, b_const], writes=[PBb[6]])
                        first = False
                p6v = PB[6][:, 0:384].rearrange("p (a k b) -> p a k b", a=16, k=3)
                S.op("dve", lambda: V.tensor_copy(cf[:], p6v[:, :, 0, :]), writes=[PBb[6], b_c])
                S.op("dve", lambda: V.tensor_tensor(cf[:], cf[:], p6v[:, :, 1, :], ALU.add), writes=[PBb[6], b_c])
                S.op("dve", lambda: V.tensor_tensor(cf[:], cf[:], p6v[:, :, 2, :], ALU.add), writes=[PBb[6], b_c])
                S.op("dve", lambda: V.tensor_scalar(negc[:, 0:8, :], cf[:, 0:8, :], -1.0, pm[:, 0:1], ALU.mult, ALU.add),
                     reads=[b_const], writes=[b_c])
                S.op("dve", lambda: V.tensor_scalar(negc[:, 8:16, :], cf[:, 8:16, :], -1.0, None, ALU.mult), writes=[b_c])
                S.op("dve", lambda: V.memset(cpad[:], 0.0), writes=[b_c3])
                cown = cf[:, 8:16, :]
                S.op("dve", lambda: V.tensor_copy(cpad[:, :, 0:8], cown), reads=[b_c], writes=[b_c3])
                S.op("dve", lambda: V.tensor_tensor(tmpf[:, 0:8, :], cown, cpad[:, :, 0:8], ALU.subtract), reads=[b_c], writes=[b_c3])
                S.op("dve", lambda: V.tensor_copy(cpad[:, :, 32:40], tmpf[:, 0:8, :]), writes=[b_c3])
                S.op("dve", lambda: V.tensor_tensor(tmpf[:, 0:8, :], tmpf[:, 0:8, :], cpad[:, :, 32:40], ALU.subtract), writes=[b_c3])
                S.op("dve", lambda: V.tensor_copy(cpad[:, :, 64:72], tmpf[:, 0:8, :]), writes=[b_c3])
                pb6b = PB[6][:].bitcast(BF16)
                for i in range(8):
                    S.op("pe", lambda: T.transpose(pb6b[:, i * 128:(i + 1) * 128], cpad[:, i, :], ident_b[:]),
                         reads=[b_c3, b_const], writes=[PBb[6]])
                S.op("dve", lambda: V.tensor_copy(c3[:], pb6b[:, :]), writes=[PBb[6], b_c3])
                if stop == "fox_c":
                    S.barrier()
                    S.dma("sp", dbg[:, 0:NT], c3[:, :])
                    raise StopBuild()
                def fox_A(itm):
                    n, h, cq, kt, nkt, grp = itm
                    pr, hh = h // 2, h % 2
                    rel = kt - (8 + 4 * cq)
                    t_lo = 0 if rel <= 0 else 128 * rel
                    sbk = SBANKS[n % 3]
                    pk = n % 3
                    q0 = cq * 512 + t_lo
                    S.op("pe", lambda: T.matmul(PB[sbk][:, t_lo:512], fkT[:, pr, kt * 128:(kt + 1) * 128], fqT[:, pr, hh, q0:cq * 512 + 512],
                                                start=True, stop=False, skip_group_check=True),
                         reads=[b_fkT, b_fqT], writes=[PBb[sbk]])
                    S.op("pe", lambda: T.matmul(PB[sbk][:, t_lo:512], sel_b[:, h, :], c3[:, q0:cq * 512 + 512],
                                                start=False, stop=(rel < 0), skip_group_check=True),
                         reads=[b_c3, b_const], writes=[PBb[sbk]])
                    if rel >= 0:
                        S.op("pe", lambda: T.matmul(PB[sbk][:, t_lo:t_lo + 128], ident_b[:], cmask_b[:],
                                                    start=False, stop=True, skip_group_check=True),
                             reads=[b_const], writes=[PBb[sbk]])
                    S.op("act", lambda: A.activation(PTs[pk][:, t_lo:512], PB[sbk][:, t_lo:512], AF.Exp, bias=negc[:, kt, h:h + 1]),
                         reads=[b_c], writes=[PBb[sbk], b_PT[pk]])

                def fox_B(itm):
                    n, h, cq, kt, nkt, grp = itm
                    rel = kt - (8 + 4 * cq)
                    t_lo = 0 if rel <= 0 else 128 * rel
                    pk = n % 3
                    ob = 4 + (grp % 2)
                    for qb in range(t_lo // 128, 4):
                        S.op("pe", lambda: T.matmul(PB[ob][:, qb * 65:qb * 65 + 65], PTs[pk][:, qb * 128:(qb + 1) * 128], fvA[:, kt, h, 0:65],
                                                    start=(kt == 0 and qb == 0), stop=False, skip_group_check=True),
                             reads=[b_PT[pk], b_fvA], writes=[PBb[ob]])
                    if kt == nkt - 1:
                        ov = PB[ob][:, 0:260].rearrange("p (a b) -> p a b", a=4)
                        S.op("dve", lambda: V.reciprocal(rec[:, 0:4], ov[:, :, 64]), writes=[PBb[ob], b_rec])
                        for qb in range(4):
                            S.op("dve", lambda: V.tensor_scalar(ystage[:, cq * 4 + qb, h * 64:(h + 1) * 64], ov[:, qb, 0:64],
                                                                rec[:, qb:qb + 1], None, ALU.mult),
                                 reads=[b_rec], writes=[PBb[ob], b_ys])

                iters = []
                for h in range(int(os.environ.get("FOXH", "8"))):
                    for cq in range(2):
                        nkt = 8 + 4 * cq + 4
                        for kt in range(nkt):
                            iters.append((len(iters), h, cq, kt, nkt, h * 2 + cq))
                pipeline(iters, fox_A, fox_B)
                if os.environ.get("FOXT", "1") == "1":
                    ystage_to_mixT(ystage, b_ys, 0)
                prefetch_w(OFF["dq"], 256)
                S.barrier()
                if stop == "fox":
                    S.dma("sp", dbg, mixT[:].rearrange("p a b -> p (a b)"))
                    raise StopBuild()

            with Alloc(nc) as al:
                dkT = al("dkT", [128, 4, 2 * NT], BF16)
                dvA = al("dvA", [128, 16, 4, 130], BF16)
                dqT = al("dqT", [128, 4, 2, NT], BF16)
                dPT0 = al("dPT0", [128, 2, 256], BF16)
                dPT1 = al("dPT1", [128, 2, 256], BF16)
                dPT2 = al("dPT2", [128, 2, 256], BF16)
                drec = al("drec", [128, 4], F32)
                dob = al("dob", [128, 128], F32)
                dsq = al("dsq", [128, 128], F32)
                dss = al("dss", [128, 8, 4], F32)
                y32 = al("y32", [128, 8, 512], F32)
                b_dss = Buf("dss")
                ystage = al("ystage_d", [128, 8, 512], BF16)
                b_dkT, b_dvA, b_dqT, b_rec, b_ys, b_ob = [Buf(n) for n in "dkT dvA dqT drec dys dob".split()]
                PTs = [dPT0, dPT1, dPT2]
                b_PT = [Buf("dPT%d" % i) for i in range(3)]
                S.op("dve", lambda: V.memset(dvA[:, :, :, 128:129], 1.0), writes=[b_dvA])
                S.op("dve", lambda: V.memset(dqT[:], 0.0), writes=[b_dqT])
                for grp in range(2):
                    wt, b_w = load_w(OFF["dq"] + grp * 256, 256)
                    for sl in range(2):
                        hd = grp * 2 + sl
                        for tc in range(2):
                            proj_fm(0, 128, xT, b_xT, tc * 512, 512,
                                    lambda: ((dqT[0:64, hd, 0, tc * 512:(tc + 1) * 512], dqT[64:128, hd, 1, tc * 512:(tc + 1) * 512]), b_dqT),
                                    0.125, wt, b_w, sl * 128, nb())
                for grp in range(2):
                    wt, b_w = load_w(OFF["dk"] + grp * 256, 256)
                    for sl in range(2):
                        hd = grp * 2 + sl
                        for half, (src, bs) in enumerate(((xTp, b_xTp), (xT, b_xT))):
                            for tc in range(2):
                                proj_fm(0, 128, src, bs, tc * 512, 512,
                                        lambda: (dkT[:, hd, half * NT + tc * 512: half * NT + (tc + 1) * 512], b_dkT),
                                        1.0, wt, b_w, sl * 128, nb())
                for grp in range(2):
                    wt, b_w = load_w(OFF["dv"] + grp * 256, 256)
                    for kt in range(16):
                        src, bs = (xTp, b_xTp) if kt < 8 else (xT, b_xT)
                        bk = nb()
                        proj_tm(src, bs, kt % 8, 256, wt, b_w, 0, bk)
                        S.op("act", lambda: A.copy(dvA[:, kt, grp * 2:(grp + 1) * 2, 0:128],
                                                   PB[bk][:, 0:256].rearrange("p (a b) -> p a b", a=2)),
                             writes=[PBb[bk], b_dvA])
                def diff_A(itm):
                    n, h, c2, kt, nkt, grp = itm
                    rel = kt - (8 + 2 * c2)
                    t_lo = 128 if rel == 1 else 0
                    sbk = SBANKS[n % 3]
                    pk = n % 3
                    q0 = c2 * 256 + t_lo
                    q1 = c2 * 256 + 256
                    Sv = PB[sbk][:].rearrange("p (m t) -> p m t", m=2)
                    for m in range(2):
                        S.op("pe", lambda: T.matmul(Sv[:, m, t_lo:256], dkT[:, h, kt * 128:(kt + 1) * 128], dqT[:, h, m, q0:q1],
                                                    start=(m == 0), stop=False, skip_group_check=True),
                             reads=[b_dkT, b_dqT], writes=[PBb[sbk]])
                    brange = {-1: (0, 128, 128, 256), 0: (0, 256, 0, 256), 1: (128, 256, 0, 128)}.get(rel)
                    if brange is not None:
                        o0, o1, r0, r1 = brange
                        for m in range(2):
                            S.op("pe", lambda: T.matmul(Sv[:, m, o0:o1], ident_b[:], DA2[:, h, m, r0:r1], start=False, stop=(m == 1),
                                                        skip_group_check=True), reads=[b_const], writes=[PBb[sbk]])
                    bias_ap = bcol[:, 1, h:h + 1] if kt < 8 else bcol[:, 0, h:h + 1]
                    S.op("act", lambda: A.activation(PTs[pk][:, :, t_lo:256], Sv[:, :, t_lo:256], AF.Exp, bias=bias_ap),
                         reads=[b_const], writes=[PBb[sbk], b_PT[pk]])

                def diff_B(itm):
                    n, h, c2, kt, nkt, grp = itm
                    rel = kt - (8 + 2 * c2)
                    t_lo = 128 if rel == 1 else 0
                    pk = n % 3
                    obs = (4, 5) if grp % 2 == 0 else (6, 7)
                    for qb in range(t_lo // 128, 2):
                        for m in range(2):
                            S.op("pe", lambda: T.matmul(PB[obs[qb]][:, m * 129:m * 129 + 129], PTs[pk][:, m, qb * 128:(qb + 1) * 128],
                                                        dvA[:, kt, h, 0:129], start=(kt == 0 and m == 0), stop=False, skip_group_check=True),
                                 reads=[b_PT[pk], b_dvA], writes=[PBb[obs[qb]]])
                    if kt == nkt - 1:
                        for qb in range(2):
                            ob = obs[qb]
                            ov = PB[ob][:, 0:258].rearrange("p (a b) -> p a b", a=2)
                            S.op("dve", lambda: V.reciprocal(drec[:, 0:2], ov[:, :, 128]), writes=[PBb[ob], b_rec])
                            S.op("dve", lambda: V.tensor_tensor(drec[:, 2:3], drec[:, 1:2], neglam, ALU.mult), reads=[b_small], writes=[b_rec])
                            S.op("dve", lambda: V.tensor_scalar(dob[:], ov[:, 0, 0:128], drec[:, 0:1], None, ALU.mult),
                                 reads=[b_rec], writes=[PBb[ob], b_ob])
                            S.op("dve", lambda: V.scalar_tensor_tensor(dob[:], ov[:, 1, 0:128], drec[:, 2:3], dob[:], ALU.mult, ALU.add),
                                 reads=[b_rec], writes=[PBb[ob], b_ob])
                            tl = c2 * 2 + qb
                            S.op("dve", lambda: V.scalar_tensor_tensor(dsq[:], dob[:], 1.0, dob[:], ALU.mult, ALU.mult, accum_out=dss[:, tl, h:h + 1]),
                                 reads=[b_ob], writes=[b_dss])
                            S.op("dve", lambda: V.tensor_copy(y32[:, tl, h * 128:(h + 1) * 128], dob[:]), reads=[b_ob], writes=[b_dss])

                iters = []
                for h in range(4):
                    for c2 in range(4):
                        nkt = 8 + 2 * c2 + 2
                        for kt in range(nkt):
                            iters.append((len(iters), h, c2, kt, nkt, h * 4 + c2))
                S.op("dve", lambda: V.memset(dss[:], 0.0), writes=[b_dss])
                pipeline(iters, diff_A, diff_B)
                S.op("dve", lambda: V.tensor_scalar(dss[:], dss[:], 1.0 / 128.0, LN_EPS, ALU.mult, ALU.add), writes=[b_dss])
                S.op("act", lambda: A.activation(dss[:], dss[:], AF.Ln), writes=[b_dss])
                S.op("act", lambda: A.activation(dss[:], dss[:], AF.Exp, scale=-0.5), writes=[b_dss])
                for tl in range(8):
                    for h in range(4):
                        S.op("dve", lambda: V.scalar_tensor_tensor(ystage[:, tl, h * 128:(h + 1) * 128], y32[:, tl, h * 128:(h + 1) * 128],
                                                                   dss[:, tl, h:h + 1], gsub[:], ALU.mult, ALU.mult),
                             reads=[b_dss, b_small], writes=[b_ys])
                ystage_to_mixT(ystage, b_ys, 4)
                prefetch_w(OFF["sq"], 256)
                S.barrier()
                if stop == "diff":
                    S.dma("sp", dbg, mixT[:].rearrange("p a b -> p (a b)"))
                    raise StopBuild()

            with Alloc(nc) as al:
                sqT = al("sqT", [128, 8, NT], BF16)
                wpad = al("wpad", [128, 16, 128], BF16)
                skT = al("skT", [128, NT + 128], BF16)
                svA = al("svA", [128, 9, 2, 66], BF16)
                ut = al("ut", [128, 9, 512], BF16)
                pw = al("pw", [128, 4, 128], BF16)
                sPT0 = al("sPT0", [128, 4, 128], BF16)
                sPT1 = al("sPT1", [128, 4, 128], BF16)
                sPT2 = al("sPT2", [128, 4, 128], BF16)
                pooledT = al("pooledT", [128, 512], BF16)
                srec = al("srec", [128, 4], F32)
                ystage = al("ystage_s", [128, 8, 512], BF16)
                b_sqT, b_skT, b_svA, b_ut, b_pw, b_rec, b_ys, b_pl = [Buf(n) for n in "sqT skT svA ut pw srec sys pl".split()]
                PTs = [sPT0, sPT1, sPT2]
                b_PT = [Buf("sPT0"), Buf("sPT1"), Buf("sPT2")]
                S.op("dve", lambda: V.memset(svA[:, :, :, 64:65], 1.0), writes=[b_svA])
                S.dma("pool", pw[:], pool_w[li].rearrange("g c d -> c g d"), writes=[b_pw])
                b_wpad = Buf("wpad")
                S.op("dve", lambda: V.memset(wpad[:], 0.0), writes=[b_wpad])
                for grp in range(2):
                    wt, b_w = load_w(OFF["sq"] + grp * 256, 256)
                    for sl in range(4):
                        hq = grp * 4 + sl
                        g = hq // 4
                        if grp == 1 and sl == 0:
                            S.op("dve", lambda: V.memset(wpad[:], 0.0), writes=[b_wpad])
                        S.op("dve", lambda: V.tensor_copy(wpad[:, :, g * 64:(g + 1) * 64], wt[:, :, sl * 64:(sl + 1) * 64]),
                             reads=[b_w], writes=[b_wpad])
                        for tc in range(2):
                            proj_fm(0, 128, xT, b_xT, tc * 512, 512,
                                    lambda: (sqT[:, hq, tc * 512:(tc + 1) * 512], b_sqT), 0.125, wpad, b_wpad, 0, nb())
                wt, b_w = load_w(OFF["sk"], 256)
                proj_fm(0, 128, xTp, b_xTp, 7 * 128, 128, lambda: (skT[:, 0:128], b_skT), 1.0, wt, b_w, 0, nb())
                for tc in range(2):
                    proj_fm(0, 128, xT, b_xT, tc * 512, 512,
                            lambda: (skT[:, 128 + tc * 512:128 + (tc + 1) * 512], b_skT), 1.0, wt, b_w, 0, nb())
                for kt in range(9):
                    src, bs, tl = (xTp, b_xTp, 7) if kt == 0 else (xT, b_xT, kt - 1)
                    bk = nb()
                    proj_tm(src, bs, tl, 128, wt, b_w, 128, bk)
                    S.op("act", lambda: A.copy(svA[:, kt, :, 0:64], PB[bk][:, 0:128].rearrange("p (a b) -> p a b", a=2)),
                         writes=[PBb[bk], b_svA])
                for grp in range(2):
                    wt, b_w = load_w(OFF["pu"] + grp * 256, 256)
                    for kt in range(9):
                        src, bs, tl = (xTp, b_xTp, 7) if kt == 0 else (xT, b_xT, kt - 1)
                        bk = nb()
                        proj_tm(src, bs, tl, 256, wt, b_w, 0, bk)
                        S.op("act", lambda: A.copy(ut[:, kt, grp * 256:(grp + 1) * 256], PB[bk][:, 0:256]), writes=[PBb[bk], b_ut])
                def swa_A(itm):
                    n, i, g, blk = itm
                    ktl = i + blk
                    sbk = SBANKS[n % 3]
                    pk = n % 3
                    S.op("pe", lambda: T.matmul(PB[sbk][:], skT[:, ktl * 128:(ktl + 1) * 128], sqT[:, g * 4:(g + 1) * 4, i * 128:(i + 1) * 128],
                                                start=True, stop=False, skip_group_check=True),
                         reads=[b_skT, b_sqT], writes=[PBb[sbk]])
                    S.op("pe", lambda: T.matmul(PB[sbk][:], ident_b[:], swaB_b[:, blk, g * 4:(g + 1) * 4, :].rearrange("p a b -> p (a b)"),
                                                start=False, stop=True, skip_group_check=True),
                         reads=[b_const], writes=[PBb[sbk]])
                    bias_ap = pm[:, 0:1] if (i == 0 and blk == 0) else zcol[:, 0:1]
                    S.op("act", lambda: A.activation(PTs[pk][:].rearrange("p a b -> p (a b)"), PB[sbk][:], AF.Exp, bias=bias_ap),
                         reads=[b_const], writes=[PBb[sbk], b_PT[pk]])

                def swa_B(itm):
                    n, i, g, blk = itm
                    ktl = i + blk
                    pk = n % 3
                    ob = 4 + ((n // 2) % 2)
                    for hh in range(4):
                        S.op("pe", lambda: T.matmul(PB[ob][:, hh * 65:hh * 65 + 65], PTs[pk][:, hh, :], svA[:, ktl, g, 0:65],
                                                    start=(blk == 0 and hh == 0), stop=False, skip_group_check=True),
                             reads=[b_PT[pk], b_svA], writes=[PBb[ob]])
                    if blk == 1:
                        ov = PB[ob][:, 0:260].rearrange("p (a b) -> p a b", a=4)
                        S.op("dve", lambda: V.tensor_tensor(srec[:], ov[:, :, 64], esink[:, g * 4:(g + 1) * 4], ALU.add),
                             reads=[b_small], writes=[PBb[ob], b_rec])
                        S.op("dve", lambda: V.reciprocal(srec[:], srec[:]), writes=[b_rec])
                        for hh in range(4):
                            hq = g * 4 + hh
                            S.op("dve", lambda: V.tensor_scalar(ystage[:, i, hq * 64:(hq + 1) * 64], ov[:, hh, 0:64], srec[:, hh:hh + 1], None, ALU.mult),
                                 reads=[b_rec], writes=[PBb[ob], b_ys])

                iters = []
                for i in range(8):
                    for g in range(2):
                        for blk in range(2):
                            iters.append((len(iters), i, g, blk))
                pipeline(iters, swa_A, swa_B)
                ystage_to_mixT(ystage, b_ys, 12)
                for g in range(4):
                    for tc in range(2):
                        bk = nb()
                        first = True
                        for j in range(4):
                            i = tc * 4 + j
                            kc, kp = (2, 3) if i == 0 else (0, 1)
                            S.op("pe", lambda: T.matmul(PB[bk][:, j * 128:(j + 1) * 128], ut[:, i + 1, g * 128:(g + 1) * 128], poolM[:, kc, g, :],
                                                        start=first, stop=False, skip_group_check=True),
                                 reads=[b_ut, b_const], writes=[PBb[bk]])
                            first = False
                            S.op("pe", lambda: T.matmul(PB[bk][:, j * 128:(j + 1) * 128], ut[:, i, g * 128:(g + 1) * 128], poolM[:, kp, g, :],
                                                        start=False, stop=False, skip_group_check=True),
                                 reads=[b_ut, b_const], writes=[PBb[bk]])
                        S.op("act", lambda: A.copy(pooledT[:], PB[bk][:]), writes=[PBb[bk], b_pl])
                        bk2 = nb()
                        S.op("pe", lambda: T.matmul(PB[bk2][:], pw[:, g, :], pooledT[:], start=True, stop=True),
                             reads=[b_pw, b_pl], writes=[PBb[bk2]])
                        S.op("dve", lambda: V.tensor_scalar(mixT[:, 8 + g, tc * 512:(tc + 1) * 512], PB[bk2][:], pscale[:, g:g + 1], None, ALU.mult),
                             reads=[b_small], writes=[PBb[bk2], b_mixT])
                S.barrier()
                if stop == "swa":
                    S.dma("sp", dbg, mixT[:].rearrange("p a b -> p (a b)"))
                    raise StopBuild()

        with Alloc(nc) as al:
            hbuf = al("hbuf", [128, 8, D], F32)
            stats = al("stats", [128, 4, 6], F32)
            mvA = al("mvA", [128, 8, 2], F32)
            rsA = al("rsA", [128, 8, 2], F32)
            gates = al("gates", [128, 8, 16], F32)
            b_h = [Buf("h%d" % i) for i in range(8)]
            b_lnw, b_ln, b_gates = Buf("lnw"), Buf("ln"), Buf("gates")

            with Alloc(nc) as al:
                wo0 = al("wo0", [128, 16, 256], BF16)
                wo1 = al("wo1", [128, 16, 256], BF16)
                lng = al("lng", [128, D], F32)
                lnb = al("lnb", [128, D], F32)
                xr0 = al("xr0", [128, 256], F32)
                xr1 = al("xr1", [128, 256], F32)
                xr2 = al("xr2", [128, 256], F32)
                hTf = al("hTf", [128, 16, 128], F32)
                wr_s = al("wr_s", [128, 16, 20], F32)
                rl = al("rl", [128, 20], F32)
                rt = al("rt", [128, 64], F32)
                wos = [wo0, wo1]
                b_wos = [Buf("wo0"), Buf("wo1")]
                xrs = [xr0, xr1, xr2]
                b_xrs = [Buf("xr%d" % i) for i in range(3)]
                b_hTf, b_wr, b_rl = Buf("hTf"), Buf("wr"), Buf("rl")
                S.dma("sp", lng[:], lnp[li, 0], writes=[b_lnw])
                S.dma("sp", lnb[:], lnp[li, 1], writes=[b_lnw])
                S.dma("sp", wr_s[:], w_r[li].rearrange("(c p) f -> p c f", p=128), writes=[b_wr])
                n = 0
                for cg in range(8):
                    k = cg % 2
                    S.dma("pool", wos[k][:], w_o[li][:, cg * 256:(cg + 1) * 256].rearrange("(c p) f -> p c f", p=128), writes=[b_wos[k]])
                    for i in range(8):
                        xk = n % 3
                        n += 1
                        S.dma("sp", xrs[xk][:], xo_f(i, cg * 256, (cg + 1) * 256), writes=[b_xrs[xk]])
                        bk = n % 4
                        for c in range(16):
                            S.op("pe", lambda: T.matmul(PB[bk][:, 0:256], mixT[:, c, i * 128:(i + 1) * 128], wos[k][:, c, :],
                                                        start=(c == 0), stop=(c == 15)),
                                 reads=[b_mixT, b_wos[k]], writes=[PBb[bk]])
                        S.op("dve", lambda: V.scalar_tensor_tensor(hbuf[:, i, cg * 256:(cg + 1) * 256], xrs[xk][:], ALPHA, PB[bk][:, 0:256],
                                                                   ALU.mult, ALU.add),
                             reads=[b_xrs[xk]], writes=[PBb[bk], b_h[i]])
                def p2_rest(i):

                    for g in range(4):
                        bi = 4 + (g % 2)
                        for j in range(4):
                            c = g * 4 + j
                            S.op("pe", lambda: T.transpose(PB[bi][:, j * 128:(j + 1) * 128], hbuf[:, i, c * 128:(c + 1) * 128], ident[:]),
                                 reads=[b_h[i], b_const], writes=[PBb[bi]])
                        pv = PB[bi][:].rearrange("p (a b) -> p a b", a=4)
                        S.op("act", lambda: A.copy(hTf[:, g * 4:(g + 1) * 4, :], pv), writes=[PBb[bi], b_hTf])
                        S.op("act", lambda: A.copy(hT[:, g * 4:(g + 1) * 4, i * 128:(i + 1) * 128], pv), writes=[PBb[bi], b_hT])
                    for c in range(16):
                        S.op("pe", lambda: T.matmul(PB[6][:, 0:20], hTf[:, c, :], wr_s[:, c, :], start=(c == 0), stop=(c == 15)),
                             reads=[b_hTf, b_wr], writes=[PBb[6]])
                    S.op("dve", lambda: V.tensor_tensor(rl[:], PB[6][:, 0:20], rb_t, ALU.add), reads=[b_small], writes=[PBb[6], b_rl])
                    gl, el = rl[:, 0:4], rl[:, 4:20]
                    gmax, gsum, pen, em, m1, k1, m2, k2, w2, den = (rt[:, 0:1], rt[:, 1:2], rt[:, 4:8], rt[:, 8:24], rt[:, 2:3],
                                                                    rt[:, 24:40], rt[:, 3:4], rt[:, 40:56], rt[:, 56:57], rt[:, 57:58])
                    ge = rt[:, 58:62]
                    ops = [
                        lambda: V.tensor_reduce(gmax, gl, AX.X, ALU.max),
                        lambda: V.tensor_scalar(pen, gl, gmax, None, ALU.is_equal),
                        lambda: V.tensor_scalar(pen, pen, -1.0, 1e30, ALU.add, ALU.mult),
                        lambda: V.tensor_scalar(ge, gl, gmax, None, ALU.subtract),
                    ]
                    for f in ops:
                        S.op("dve", f, writes=[b_rl])
                    S.op("dve", lambda: V.memset(gsum, 0.0), writes=[b_rl])
                    S.op("act", lambda: A.activation(ge, ge, AF.Exp, accum_out=gsum), writes=[b_rl])
                    ops = [
                        lambda: V.tensor_tensor(em.rearrange("p (g e) -> p g e", g=4), el.rearrange("p (g e) -> p g e", g=4),
                                                pen.unsqueeze(2).to_broadcast([128, 4, 4]), ALU.add),
                        lambda: V.tensor_reduce(m1, em, AX.X, ALU.max),
                        lambda: V.tensor_scalar(k1, em, m1, None, ALU.is_equal),
                        lambda: V.scalar_tensor_tensor(em, k1, -1e30, em, ALU.mult, ALU.add),
                        lambda: V.tensor_reduce(m2, em, AX.X, ALU.max),
                        lambda: V.tensor_scalar(k2, em, m2, None, ALU.is_equal),
                        lambda: V.tensor_tensor(w2, m2, m1, ALU.subtract),
                    ]
                    for f in ops:
                        S.op("dve", f, writes=[b_rl])
                    S.op("act", lambda: A.activation(w2, w2, AF.Exp), writes=[b_rl])
                    ops = [
                        lambda: V.tensor_scalar(den, w2, 1.0, ALPHA, ALU.add, ALU.mult),
                        lambda: V.tensor_tensor(den, den, gsum, ALU.mult),
                        lambda: V.reciprocal(den, den),
                        lambda: V.tensor_tensor(w2, w2, den, ALU.mult),
                        lambda: V.tensor_scalar(k1, k1, den, None, ALU.mult),
                        lambda: V.scalar_tensor_tensor(gates[:, i, :], k2, w2, k1, ALU.mult, ALU.add),
                    ]
                    for f in ops[:-1]:
                        S.op("dve", f, writes=[b_rl])
                    S.op("dve", ops[-1], reads=[b_rl], writes=[b_gates])
                ln_stats_all(hbuf, b_h, b_ln, stats, mvA, rsA, LN_EPS)
                for i in range(8):
                    ln_apply_tile(hbuf, i, lng, lnb, b_h[i], b_ln, rsA, b_lnw)
                    if i >= 1:
                        p2_rest(i - 1)
                p2_rest(7)
                S.barrier()
                if stop == "p2":
                    for i in range(8):
                        S.dma("sp", out_f(i), hbuf[:, i, :])
                    S.dma("sp", dbg, hT[:].rearrange("p a b -> p (a b)"))
                    raise StopBuild()

            with Alloc(nc) as al:
                gu0 = al("gu0", [128, 16, 256], BF16)
                gu1 = al("gu1", [128, 16, 256], BF16)
                gu2 = al("gu2", [128, 16, 256], BF16)
                gu3 = al("gu3", [128, 16, 256], BF16)
                hidT = al("hidT", [128, 4, NT], BF16)
                sa0 = al("sa0", [128, 512], F32)
                sa1 = al("sa1", [128, 512], F32)
                pT = al("pT", [128, 2, NT], BF16)
                puw = al("puw", [128, 2, D], BF16)
                pin = al("pin", [128, 256], F32)
                gus = [gu0, gu1, gu2, gu3]
                b_gus = [Buf("gu%d" % i) for i in range(4)]
                mflat = mixT[:].rearrange("p a b -> p (a b)")
                dws = [mflat[:, k * 8192:(k + 1) * 8192].rearrange("p (c d) -> p c d", c=4) for k in range(2)]
                b_dws = [Buf("dw0"), Buf("dw1")]
                sas = [sa0, sa1]
                b_sas = [Buf("sa0"), Buf("sa1")]
                b_hid = [Buf("hid%d" % i) for i in range(4)]
                b_pT, b_puw, b_pin = Buf("pT"), Buf("puw"), Buf("pin")
                gn = 0
                san = 0
                yb = 0
                for e in range(16):
                    for hf in range(2):
                        kg, ku = gn % 4, (gn + 1) % 4
                        gn += 2
                        S.dma("pool", gus[kg][:], w_gate[li, e][:, hf * 256:(hf + 1) * 256].rearrange("(c p) f -> p c f", p=128), writes=[b_gus[kg]])
                        S.dma("pool", gus[ku][:], w_up[li, e][:, hf * 256:(hf + 1) * 256].rearrange("(c p) f -> p c f", p=128), writes=[b_gus[ku]])
                        for fc in range(2):
                            f4 = hf * 2 + fc
                            for tc in range(2):
                                ba, bu = (0, 1) if (fc + tc) % 2 == 0 else (2, 3)
                                for c in range(16):
                                    S.op("pe", lambda: T.matmul(PB[ba][:], gus[kg][:, c, fc * 128:(fc + 1) * 128], hT[:, c, tc * 512:(tc + 1) * 512],
                                                                start=(c == 0), stop=(c == 15)),
                                         reads=[b_gus[kg], b_hT], writes=[PBb[ba]])
                                for c in range(16):
                                    S.op("pe", lambda: T.matmul(PB[bu][:], gus[ku][:, c, fc * 128:(fc + 1) * 128], hT[:, c, tc * 512:(tc + 1) * 512],
                                                                start=(c == 0), stop=(c == 15)),
                                         reads=[b_gus[ku], b_hT], writes=[PBb[bu]])
                                sk_ = san % 2
                                san += 1
                                S.op("act", lambda: A.activation(sas[sk_][:], PB[ba][:], AF.Silu), writes=[PBb[ba], b_sas[sk_]])
                                S.op("dve", lambda: V.tensor_tensor(hidT[:, f4, tc * 512:(tc + 1) * 512], sas[sk_][:], PB[bu][:], ALU.mult),
                                     reads=[b_sas[sk_]], writes=[PBb[bu], b_hid[f4]])
                    kd = e % 2
                    S.dma("pool", dws[kd], w_down[li, e].rearrange("(c p) d -> p c d", p=128), writes=[b_dws[kd]])
                    for i in range(8):
                        for cg in range(4):
                            bk = 4 + (yb % 4)
                            yb += 1
                            for fc in range(4):
                                S.op("pe", lambda: T.matmul(PB[bk][:], hidT[:, fc, i * 128:(i + 1) * 128], dws[kd][:, fc, cg * 512:(cg + 1) * 512],
                                                            start=(fc == 0), stop=(fc == 3)),
                                     reads=[b_hid[fc], b_dws[kd]], writes=[PBb[bk]])
                            S.op("dve", lambda: V.scalar_tensor_tensor(hbuf[:, i, cg * 512:(cg + 1) * 512], PB[bk][:], gates[:, i, e:e + 1],
                                                                       hbuf[:, i, cg * 512:(cg + 1) * 512], ALU.mult, ALU.add),
                                 reads=[b_gates], writes=[PBb[bk], b_h[i]])
                S.dma("pool", puw[:], ple_uw[li].rearrange("(c p) d -> p c d", p=128), writes=[b_puw])
                for i in range(8):
                    S.dma("sp", pin[:], p_own[li, i * 128:(i + 1) * 128, :], writes=[b_pin])
                    for k in range(2):
                        S.op("pe", lambda: T.transpose(PB[0][:, k * 128:(k + 1) * 128], pin[:, k * 128:(k + 1) * 128], ident[:]),
                             reads=[b_pin, b_const], writes=[PBb[0]])
                    S.op("act", lambda: A.copy(pT[:, :, i * 128:(i + 1) * 128], PB[0][:, 0:256].rearrange("p (a b) -> p a b", a=2)),
                         writes=[PBb[0], b_pT])
                for cg in range(4):
                    kd = cg % 2
                    wv = mflat[:, kd * 8192:(kd + 1) * 8192].rearrange("p (c f) -> p c f", c=16)
                    S.dma("pool", wv, ple_gw[li][:, cg * 512:(cg + 1) * 512].rearrange("(c p) f -> p c f", p=128), writes=[b_dws[kd]])
                    for i in range(8):
                        ba, bu = (0, 1) if i % 2 == 0 else (2, 3)
                        for c in range(16):
                            S.op("pe", lambda: T.matmul(PB[ba][:], hT[:, c, i * 128:(i + 1) * 128], wv[:, c, :], start=(c == 0), stop=(c == 15)),
                                 reads=[b_hT, b_dws[kd]], writes=[PBb[ba]])
                        for k in range(2):
                            S.op("pe", lambda: T.matmul(PB[bu][:], pT[:, k, i * 128:(i + 1) * 128], puw[:, k, cg * 512:(cg + 1) * 512],
                                                        start=(k == 0), stop=(k == 1)),
                                 reads=[b_pT, b_puw], writes=[PBb[bu]])
                        sk_ = san % 2
                        san += 1
                        S.op("act", lambda: A.activation(sas[sk_][:], PB[ba][:], AF.Sigmoid), writes=[PBb[ba], b_sas[sk_]])
                        S.op("dve", lambda: V.scalar_tensor_tensor(sas[sk_][:], sas[sk_][:], 1.0 / ALPHA, PB[bu][:], ALU.mult, ALU.mult), writes=[PBb[bu], b_sas[sk_]])
                        S.op("dve", lambda: V.tensor_tensor(hbuf[:, i, cg * 512:(cg + 1) * 512], hbuf[:, i, cg * 512:(cg + 1) * 512], sas[sk_][:], ALU.add),
                             reads=[b_sas[sk_]], writes=[b_h[i]])
                S.barrier()

            with Alloc(nc) as al:
                lng = al("lng2", [128, D], F32)
                lnb = al("lnb2", [128, D], F32)
                S.dma("sp", lng[:], lnp[li, 2], writes=[b_lnw])
                S.dma("sp", lnb[:], lnp[li, 3], writes=[b_lnw])
                ln_stats_all(hbuf, b_h, b_ln, stats, mvA, rsA, LN_EPS / (ALPHA * ALPHA))
                for i in range(8):
                    ln_apply_tile(hbuf, i, lng, lnb, b_h[i], b_ln, rsA, b_lnw)
                    ev = S.dma("sp", out_f(i), hbuf[:, i, :], reads=[b_h[i]])
                    if post_out is not None:
                        post_out(i, ev)
                S.barrier()

    xo0 = rows_f(x_own)
    xp0 = lambda i: x_prev[i * 128:(i + 1) * 128, :]
    yo = lambda i: y_out[i * 128:(i + 1) * 128, :]
    if nL == 1:
        try:
            layer(0, layers[0], xo0, xp0, yo, actA, b_actA, actB, b_actB)
        except StopBuild:
            pass
    else:
        S.sems["cc"] = S._sem("cc")
        b_xall = [Buf("xall%d" % i) for i in range(8)]

        def exchange_tile(i, ev):
            S._wait("pool", {ev[0]: ev[1]})
            P.collective_compute("AllGather", ALU.bypass, replica_groups=[[0, 1], [2, 3], [4, 5], [6, 7]],
                                 ins=[xmid[i].ap().opt()], outs=[xall[i].ap().opt()]).then_inc(S.sems["cc"], 1)
            b_xall[i].last_w = ("cc", i + 1)

        layer(0, layers[0], xo0, xp0, lambda i: xmid[i].ap(), actA, b_actA, actB, b_actB, post_out=exchange_tile)
        S.dma_last["cc"] = 8
        for i in range(8):
            b_xall[i].last_w = ("cc", 8)
        layer(1, layers[1], lambda i, c0, c1: xmid[i].ap()[:, c0:c1], lambda i: xall[i].ap()[0:128, :], yo,
              actA, b_actA, actB, b_actB, xp_b=b_xall)
    S.barrier()
    S.close()
    for cm in reversed(es):
        cm.__exit__(None, None, None)
    return nc


def _prep_shared(inputs, layers):
    f = lambda a: np.ascontiguousarray(np.asarray(a, dtype=np.float32))
    L = list(layers)
    sh = {}
    sh["w_in"] = f(inputs["w_in"][L])
    sh["w_o"] = f(inputs["w_o"][L])
    sh["w_gate"] = f(inputs["w_gate"][L])
    sh["w_up"] = f(inputs["w_up"][L])
    sh["w_down"] = f(inputs["w_down"][L])
    sh["ple_gw"] = f(inputs["ple_gate_w"][L])
    sh["ple_uw"] = f(inputs["ple_up_w"][L])
    sh["pool_w"] = f(inputs["pool_w"][L])
    sh["w_r"] = f(np.concatenate([inputs["router_g_w"][L], inputs["router_e_w"][L]], axis=-1))
    nL = len(L)
    lnp = np.empty((nL, 4, 128, D), np.float32)
    small = np.zeros((nL, 128, 448), np.float32)
    for j, l in enumerate(L):
        for k, nm in enumerate(("ln1_g", "ln1_b", "ln2_g", "ln2_b")):
            lnp[j, k] = np.broadcast_to(inputs[nm][l][None, :], (128, D))
        small[j, :, 0:8] = inputs["b_f"][l][None, :]
        small[j, :, 8:16] = inputs["sinks"][l][None, :]
        for k, nm in enumerate(("lam_q1", "lam_k1", "lam_q2", "lam_k2")):
            small[j, :, 16 + 64 * k:16 + 64 * (k + 1)] = inputs[nm][l][None, :]
        small[j, :, 272:400] = inputs["diff_norm_g"][l][None, :]
        small[j, :, 400:404] = inputs["pool_scale"][l].reshape(4, 128).T
        small[j, :, 404:408] = inputs["router_g_b"][l][None, :]
        small[j, :, 408:424] = inputs["router_e_b"][l][None, :]
    sh["lnp"] = lnp
    sh["small"] = small
    cs = _static_consts()
    diffB, swaB, b31 = _bias_tiles(np.asarray(inputs["rel_table"], np.float32))
    for nm in ("ident", "cmask", "tri", "ones", "sel", "poolAc", "poolAp"):
        sh["c_" + nm] = cs[nm]
    sh["c_diffB"], sh["c_swaB"], sh["c_b31"] = diffB, swaB, b31
    return sh, cs


_PROG_CACHE = {}


def _run(layers, fused, x_full, inputs):
    key = (tuple(layers), fused)
    if key not in _PROG_CACHE:
        _PROG_CACHE[key] = build_program(list(layers), fused)
    nc = _PROG_CACHE[key]
    sh, cs = _prep_shared(inputs, layers)
    p = np.asarray(inputs["p"], np.float32)
    in_maps = []
    for c in range(8):
        b, half = c // 2, c % 2
        m = dict(sh)
        m["x_own"] = np.ascontiguousarray(x_full[b, half * NT:(half + 1) * NT])
        m["x_prev"] = np.ascontiguousarray(x_full[b, 0:NT])
        m["p_own"] = np.ascontiguousarray(p[list(layers), b, half * NT:(half + 1) * NT])
        m["c_pm"] = np.full((128, 1), 0.0 if half == 1 else NEG, np.float32)
        m["c_poolA0"] = cs["poolAc"] if half == 1 else cs["poolAf"]
        m["c_poolAp0"] = cs["poolAp"] if half == 1 else np.zeros_like(cs["poolAp"])
        in_maps.append(m)
    res = run_bass_kernel_spmd(nc, in_maps, core_ids=list(range(8)))
    out = np.empty((4, SEQ, D), np.float32)
    for c in range(8):
        b, half = c // 2, c % 2
        out[b, half * NT:(half + 1) * NT] = res.results[c]["y_out"]
    return out


def kernel(**inputs):
    x = np.asarray(inputs["x"], np.float32)
    if FUSED:
        return _run((0, 1), True, x, inputs)
    x1 = _run((0,), False, x, inputs)
    return _run((1,), False, x1, inputs)
```

```python
import math
import os
import numpy as np
import concourse.bass as bass
import concourse.mybir as mybir
from concourse.bass_utils import run_bass_kernel_spmd

F32 = mybir.dt.float32
BF16 = mybir.dt.bfloat16
AF = mybir.ActivationFunctionType
ALU = mybir.AluOpType
AX = mybir.AxisListType

D = 2048
SEQ = 2048
NT = 1024
DEPTH = 2
IN_W = 4360
OFF = dict(fq=0, fk=512, fv=1024, ff=1536, dq=1544, dk=2056, dv=2568, pu=3080, sq=3592, sk=4104, sv=4232)
ALPHA = (2 * DEPTH) ** 0.25
LN_EPS = 1e-5
NEG = -30000.0
FUSED = True


class Buf:
    __slots__ = ("name", "last_w", "readers")

    def __init__(self, name):
        self.name = name
        self.last_w = None
        self.readers = {}


class Sched:
    def __init__(self, nc, n_dma_sems=6):
        self.nc = nc
        self.sems = {}
        self.count = {}
        self.known = {}
        self.engs = {"pe": nc.tensor, "act": nc.scalar, "dve": nc.vector, "pool": nc.gpsimd, "sp": nc.sync}
        self._ctx = []
        for e in self.engs:
            self.known[e] = {}
        for e in ("pe", "act", "dve", "pool"):
            self.sems[e] = self._sem("s_" + e)
            self.count[e] = 0
        self.dma_n = {}
        self.dma_last = {}
        self.n_dma_sems = n_dma_sems
        for q in ("sp", "pool"):
            self.dma_n[q] = 0
            for i in range(n_dma_sems):
                self.sems[(q, i)] = self._sem("d_%s%d" % (q, i))
                self.dma_last[(q, i)] = 0

    def _sem(self, name):
        cm = self.nc.semaphore(name)
        h = cm.__enter__()
        self._ctx.append(cm)
        return h

    def close(self):
        for cm in reversed(self._ctx):
            cm.__exit__(None, None, None)

    def _deps(self, reads, writes):
        deps = {}

        def add(ev):
            if ev is None:
                return
            k, v = ev
            if deps.get(k, 0) < v:
                deps[k] = v
        for b in reads:
            add(b.last_w)
        for b in writes:
            add(b.last_w)
            for k, v in b.readers.items():
                add((k, v))
        return deps

    def _wait(self, ename, deps, skip_self=False):
        eng = self.engs[ename]
        kn = self.known[ename]
        for k, v in deps.items():
            if skip_self and k == ename:
                continue
            if kn.get(k, 0) >= v:
                continue
            eng.wait_ge(self.sems[k], v)
            kn[k] = v

    def _mark(self, ev, reads, writes):
        k, v = ev
        for b in writes:
            b.last_w = ev
            b.readers = {}
        for b in reads:
            if b.readers.get(k, 0) < v:
                b.readers[k] = v

    def op(self, ename, fn, reads=(), writes=()):
        deps = self._deps(reads, writes)
        self._wait(ename, deps, skip_self=(ename == "pe"))
        ins = fn()
        self.count[ename] += 1
        ins.then_inc(self.sems[ename], 1)
        ev = (ename, self.count[ename])
        self._mark(ev, reads, writes)
        return ev

    def dma(self, q, out, in_, reads=(), writes=()):
        deps = self._deps(reads, writes)
        n = self.dma_n[q]
        slot = n % self.n_dma_sems
        rnd = n // self.n_dma_sems
        key = (q, slot)
        if rnd > 0:
            deps[key] = max(deps.get(key, 0), 16 * rnd)
        self._wait(q, deps)
        self.engs[q].dma_start(out=out, in_=in_).then_inc(self.sems[key], 16)
        self.dma_n[q] = n + 1
        self.dma_last[key] = 16 * (rnd + 1)
        ev = (key, 16 * (rnd + 1))
        self._mark(ev, reads, writes)
        return ev

    def barrier(self):
        tgt = {}
        for e in ("pe", "act", "dve", "pool"):
            if self.count[e] > 0:
                tgt[e] = self.count[e]
        for k, v in self.dma_last.items():
            if v > 0:
                tgt[k] = v
        for e in self.engs:
            self._wait(e, tgt, skip_self=False)


class Alloc:
    def __init__(self, nc):
        self.nc = nc
        self.cms = []

    _n = [0]

    def __call__(self, name, shape, dt=F32):
        Alloc._n[0] += 1
        cm = self.nc.sbuf_tensor("%s_%d" % (name, Alloc._n[0]), list(shape), dt)
        t = cm.__enter__()
        self.cms.append(cm)
        return t

    def __enter__(self):
        return self

    def __exit__(self, *a):
        for cm in reversed(self.cms):
            cm.__exit__(None, None, None)
        return False


def _t5_bucket_np(n):
    n = np.asarray(n, dtype=np.int32)
    max_exact = 16
    nf = np.maximum(n, 1).astype(np.float32)
    large = max_exact + (np.log(nf / np.float32(max_exact)) / np.float32(math.log(128 / max_exact))
                         * np.float32(32 - max_exact)).astype(np.int32)
    large = np.minimum(large, 31)
    return np.where(n < max_exact, n, large)


def _static_consts():
    c = {}
    c["ident"] = np.eye(128, dtype=np.float32)
    s = np.arange(128)[:, None]
    t = np.arange(128)[None, :]
    c["cmask"] = np.where(s <= t, 0.0, NEG).astype(np.float32)
    c["tri"] = (s <= t).astype(np.float32)
    c["ones"] = np.ones((128, 128), np.float32)
    sel = np.zeros((128, 8, 128), np.float32)
    for h in range(8):
        for b in (0, 32, 64):
            sel[b + h, h, :] = 1.0
    c["sel"] = sel
    wins = (2, 4, 8, 16)
    Ac = np.zeros((4, 128, 128), np.float32)
    Ap = np.zeros((4, 128, 128), np.float32)
    Af = np.zeros((4, 128, 128), np.float32)
    for g, w in enumerate(wins):
        for tt in range(128):
            for ss in range(tt - w + 1, tt + 1):
                if ss >= 0:
                    Ac[g, ss, tt] += 1.0 / w
                    Af[g, ss, tt] += 1.0 / min(tt + 1, w)
                else:
                    Ap[g, 128 + ss, tt] += 1.0 / w
            Ac[g, tt, tt] -= 1.0
            Af[g, tt, tt] -= 1.0
    c["poolAc"], c["poolAp"], c["poolAf"] = Ac, Ap, Af
    return c


def _bias_tiles(rel_table):
    bk = _t5_bucket_np(np.arange(256))
    bbd = rel_table[bk]
    s = np.arange(128)[:, None]
    t = np.arange(128)[None, :]
    dD = t - s
    dA = 128 + t - s
    diffB = np.empty((128, 4, 2, 128), np.float32)
    for h in range(4):
        diffB[:, h, 0, :] = np.where(dD >= 0, bbd[np.clip(dD, 0, 255), h], NEG)
        diffB[:, h, 1, :] = bbd[np.clip(dA, 0, 255), h]
    swaB = np.empty((128, 2, 8, 128), np.float32)
    for h in range(8):
        swaB[:, 0, h, :] = np.where(dA < 128, bbd[np.clip(dA, 0, 127), 4 + h], NEG)
        swaB[:, 1, h, :] = np.where(dD >= 0, bbd[np.clip(dD, 0, 127), 4 + h], NEG)
    b31 = np.broadcast_to(rel_table[31, :4][None, :], (128, 4)).astype(np.float32).copy()
    return diffB, swaB, b31


class StopBuild(Exception):
    pass


def build_program(layers, fused, stop=None):
    nc = bass.Bass("TRN2", target_bir_lowering=False)
    dbg = nc.dram_tensor("dbg", [128, 16 * NT], BF16, kind="ExternalOutput").ap() if stop else None
    nL = len(layers)

    def din(name, shape):
        return nc.dram_tensor(name, list(shape), F32, kind="ExternalInput").ap()

    x_own = din("x_own", [NT, D])
    x_prev = din("x_prev", [NT, D])
    p_own = din("p_own", [nL, NT, 256])
    w_in = din("w_in", [nL, D, IN_W])
    w_o = din("w_o", [nL, D, D])
    small_w = stop is not None and stop != "p3"
    w_gate = din("w_gate", [nL, 1 if small_w else 16, D, 512])
    w_up = din("w_up", [nL, 1 if small_w else 16, D, 512])
    w_down = din("w_down", [nL, 1 if small_w else 16, 512, D])
    ple_gw = din("ple_gw", [nL, D, D])
    ple_uw = din("ple_uw", [nL, 256, D])
    pool_w = din("pool_w", [nL, 4, 128, 128])
    w_r = din("w_r", [nL, D, 20])
    lnp = din("lnp", [nL, 4, 128, D])
    small = din("small", [nL, 128, 448])
    cst = {}
    for nm, shp in (("ident", [128, 128]), ("cmask", [128, 128]), ("tri", [128, 128]), ("ones", [128, 128]),
                    ("sel", [128, 8, 128]), ("poolAc", [4, 128, 128]), ("poolAp", [4, 128, 128]),
                    ("poolA0", [4, 128, 128]), ("poolAp0", [4, 128, 128]),
                    ("diffB", [128, 4, 2, 128]), ("swaB", [128, 2, 8, 128]), ("b31", [128, 4]), ("pm", [128, 1])):
        cst[nm] = din("c_" + nm, shp)
    y_out = nc.dram_tensor("y_out", [NT, D], F32, kind="ExternalOutput").ap()
    if fused and nL == 2:
        xmid = [nc.dram_tensor("xmid%d" % i, [128, D], F32) for i in range(8)]
        xmid_b = [nc.dram_tensor("xmidb%d" % i, [128, D], BF16) for i in range(8)]
        xall_b = [nc.dram_tensor("xallb%d" % i, [256, D], BF16) for i in range(8)]

    def rows_f(ap):
        return lambda i, c0, c1: ap[i * 128:(i + 1) * 128, c0:c1]

    S = Sched(nc)
    T, V, A, P = nc.tensor, nc.vector, nc.scalar, nc.gpsimd
    es = []

    def sb(name, shape, dt=F32):
        cm = nc.sbuf_tensor(name, list(shape), dt)
        t = cm.__enter__()
        es.append(cm)
        return t

    def psum(name):
        cm = nc.psum_tensor(name, [128, 512], F32)
        t = cm.__enter__()
        es.append(cm)
        return t

    PB = [psum("pb%d" % i) for i in range(8)]
    PBb = [Buf("pb%d" % i) for i in range(8)]

    actA = sb("actA", [128, 16, NT], BF16)
    actB = sb("actB", [128, 16, NT], BF16)
    b_actA, b_actB = Buf("actA"), Buf("actB")
    ident = sb("ident", [128, 128]); b_const = Buf("const")
    ident_b = sb("ident_b", [128, 128], BF16)
    cmask_b = sb("cmask_b", [128, 128], BF16)
    tri = sb("tri", [128, 128], BF16)
    ones = sb("ones", [128, 128], BF16)
    sel_b = sb("sel_b", [128, 8, 128], BF16)
    poolM = sb("poolM", [128, 4, 4, 128], BF16)
    DA2 = sb("DA2", [128, 4, 2, 256], BF16)
    swaB_b = sb("swaB_b", [128, 2, 8, 128], BF16)
    b31 = sb("b31", [128, 4])
    pm = sb("pm", [128, 1])
    bcol = sb("bcol", [128, 2, 4])
    zcol = sb("zcol", [128, 1])
    smalls = sb("smalls", [128, 448])
    b_small = Buf("small")
    lamc = sb("lamc", [128, 8])
    esink = sb("esink", [128, 8])
    gsub = sb("gsub", [128, 128])

    lamtmp = sb("lamtmp", [128, 128])
    b_stage = Buf("stage")
    stage_cm = nc.sbuf_tensor("stage", [128, 2048], F32)
    stage = stage_cm.__enter__()

    def load_const_f32(dst, src_ap):
        S.dma("sp", dst, src_ap, writes=[b_const])

    def load_cast(dst_ap, src_ap, n_free, view=None):
        st = stage[:src_ap.shape[0], 0:n_free]
        if view is not None:
            st = view(st)
        S.dma("sp", st, src_ap, writes=[b_stage])
        S.op("dve", lambda: V.tensor_copy(dst_ap, st), reads=[b_stage], writes=[b_const])

    load_const_f32(ident[:], cst["ident"][:, :])
    load_cast(tri[:], cst["tri"][:, :], 128)
    load_cast(ones[:], cst["ones"][:, :], 128)
    load_const_f32(b31[:], cst["b31"][:, :])
    load_const_f32(pm[:], cst["pm"][:, :])
    load_cast(ident_b[:], cst["ident"][:, :], 128)
    load_cast(cmask_b[:], cst["cmask"][:, :], 128)
    load_cast(sel_b[:], cst["sel"][:, :, :], 1024, view=lambda a: a.rearrange("p (a b) -> p a b", a=8))
    for k, nm in enumerate(("poolAc", "poolAp", "poolA0", "poolAp0")):
        load_cast(poolM[:, k, :, :], cst[nm].rearrange("g s t -> s g t"), 512,
                  view=lambda a: a.rearrange("p (a b) -> p a b", a=4))
    load_cast(swaB_b[:], cst["swaB"][:, :, :, :], 2048,
              view=lambda a: a.rearrange("p (a b c) -> p a b c", a=2, b=8))
    S.dma("sp", stage[:, 0:1024].rearrange("p (h k t) -> p h k t", h=4, k=2), cst["diffB"][:, :, :, :], writes=[b_stage])
    for h in range(4):
        for m in range(2):
            S.op("dve", lambda: V.tensor_scalar(
                DA2[:, h, m, :], stage[:, h * 256:(h + 1) * 256], b31[:, h:h + 1], None, ALU.subtract),
                reads=[b_stage, b_const], writes=[b_const])
    S.op("dve", lambda: V.memset(zcol[:], 0.0), writes=[b_const])
    S.op("dve", lambda: V.tensor_copy(bcol[:, 0, :], b31[:]), reads=[b_const], writes=[b_const])
    S.op("dve", lambda: V.tensor_scalar(bcol[:, 1, :], b31[:], pm[:, 0:1], None, ALU.add), reads=[b_const], writes=[b_const])

    S.barrier()
    stage_cm.__exit__(None, None, None)
    bank_rr = [0]

    def transposes_to_T(src_tile_fn, nchunks, dst, dst_buf, tok0, src_bufs, banks, extra=None):
        for g in range(nchunks // 4):
            bi = banks[g % len(banks)]
            for j in range(4):
                c = g * 4 + j
                S.op("pe", lambda: T.transpose(PB[bi][:, j * 128:(j + 1) * 128], src_tile_fn(c), ident[:]),
                     reads=src_bufs + [b_const], writes=[PBb[bi]])
            pv = PB[bi][:].rearrange("p (a b) -> p a b", a=4)
            if extra is not None:
                extra(g, pv, PBb[bi])
            eng = "act" if g % 2 == 0 else "dve"
            if eng == "act":
                S.op("act", lambda: A.copy(dst[:, g * 4:(g + 1) * 4, tok0:tok0 + 128], pv), writes=[PBb[bi], dst_buf])
            else:
                S.op("dve", lambda: V.tensor_copy(dst[:, g * 4:(g + 1) * 4, tok0:tok0 + 128], pv), writes=[PBb[bi], dst_buf])

    def ln_stats_all(hb, b_hs, b_ln, stats, mvA, rsA, eps):
        for i in range(8):
            for c in range(4):
                S.op("dve", lambda: V.bn_stats(stats[:, c, :], hb[:, i, c * 512:(c + 1) * 512]), reads=[b_hs[i]], writes=[b_ln])
            S.op("dve", lambda: V.bn_aggr(mvA[:, i, :], stats[:]), writes=[b_ln])
        S.op("dve", lambda: V.tensor_scalar(rsA[:, :, 0], mvA[:, :, 1], eps, None, ALU.add), writes=[b_ln])
        S.op("act", lambda: A.activation(rsA[:, :, 0], rsA[:, :, 0], AF.Ln), writes=[b_ln])
        S.op("act", lambda: A.activation(rsA[:, :, 0], rsA[:, :, 0], AF.Exp, scale=-0.5), writes=[b_ln])
        S.op("dve", lambda: V.scalar_tensor_tensor(rsA[:, :, 1], mvA[:, :, 0], -1.0, rsA[:, :, 0], ALU.mult, ALU.mult), writes=[b_ln])

    def ln_apply_tile(hb, i, gt, bt, b_h, b_ln, rsA, b_lnw):
        S.op("act", lambda: A.activation(hb[:, i, :], hb[:, i, :], AF.Identity, bias=rsA[:, i, 1:2], scale=rsA[:, i, 0:1]),
             reads=[b_ln], writes=[b_h])
        S.op("dve", lambda: V.tensor_tensor(hb[:, i, :], hb[:, i, :], gt[:], ALU.mult), reads=[b_lnw], writes=[b_h])
        S.op("dve", lambda: V.tensor_tensor(hb[:, i, :], hb[:, i, :], bt[:], ALU.add), reads=[b_lnw], writes=[b_h])

    def layer(li, l, xo_f, xp_f, out_f, xT, b_xT, mixT, b_mixT, xp_b=None, post_out=None, xo_T_f=None):
        lam_init = 0.8 - 0.6 * math.exp(-0.3 * l)
        hT, b_hT = xT, b_xT

        S.dma("sp", smalls[:], small[li], writes=[b_small])
        bf_t = smalls[:, 0:8]
        sink_t = smalls[:, 8:16]
        lam_v = smalls[:, 16:272].rearrange("p (a b) -> p a b", a=4)
        gsub_raw = smalls[:, 272:400]
        pscale = smalls[:, 400:404]
        rb_t = smalls[:, 404:424]
        S.op("dve", lambda: V.tensor_tensor(lamtmp[:, 0:64], lam_v[:, 0, :], lam_v[:, 1, :], ALU.mult), reads=[b_small], writes=[b_stage])
        S.op("dve", lambda: V.tensor_tensor(lamtmp[:, 64:128], lam_v[:, 2, :], lam_v[:, 3, :], ALU.mult), reads=[b_small], writes=[b_stage])
        S.op("dve", lambda: V.tensor_reduce(lamc[:, 0:2], lamtmp[:, 0:128].rearrange("p (a b) -> p a b", a=2), AX.X, ALU.add),
             reads=[b_stage], writes=[b_small])
        S.op("act", lambda: A.activation(lamc[:, 2:4], lamc[:, 0:2], AF.Exp), writes=[b_small])
        S.op("dve", lambda: V.tensor_tensor(lamc[:, 4:5], lamc[:, 3:4], lamc[:, 2:3], ALU.subtract), writes=[b_small])
        S.op("dve", lambda: V.tensor_scalar(lamc[:, 5:6], lamc[:, 4:5], -lam_init, None, ALU.add), writes=[b_small])
        S.op("act", lambda: A.activation(esink[:], sink_t, AF.Exp), reads=[b_small], writes=[b_small])
        S.op("dve", lambda: V.tensor_scalar(gsub[:], gsub_raw, 1.0 - lam_init, None, ALU.mult), writes=[b_small])
        neglam = lamc[:, 5:6]

        with Alloc(nc) as al:
            xTp = al("xTp", [128, 16, NT], BF16)
            wr0 = al("wr0", [128, 16, 256], BF16)
            wr1 = al("wr1", [128, 16, 256], BF16)
            b_xTp = Buf("xTp")
            wring = [wr0, wr1]
            b_wring = [Buf("wr0"), Buf("wr1")]
            wn = [0]

            pref = {}

            def load_w(c0, ncols):
                if (c0, ncols) in pref:
                    return pref.pop((c0, ncols))
                k = wn[0] % 2
                wn[0] += 1
                S.dma("pool", wring[k][:, :, 0:ncols], w_in[li][:, c0:c0 + ncols].rearrange("(c p) f -> p c f", p=128),
                      writes=[b_wring[k]])
                return wring[k], b_wring[k]

            def prefetch_w(c0, ncols):
                r = load_w(c0, ncols)
                pref[(c0, ncols)] = r

            with Alloc(nc) as al:
                xin = [al("xin%d" % j, [128, D], BF16) for j in range(4)]
                b_xin = [Buf("xin%d" % j) for j in range(4)]
                nt = 0
                for which, (src, dst, bdst) in enumerate((((xo_T_f if xo_T_f is not None else (lambda i: xo_f(i, 0, D))), xT, b_xT), (xp_f, xTp, b_xTp))):
                    for i in range(8):
                        k = (which * 8 + i) % 4
                        S.dma("pool", xin[k][:], src(i), reads=([xp_b[i]] if (which == 1 and xp_b is not None) else []), writes=[b_xin[k]])
                        for g in range(2):
                            bi = nt % 2
                            nt += 1
                            pbb = PB[bi][:].bitcast(BF16)
                            for j in range(8):
                                c = g * 8 + j
                                S.op("pe", lambda: T.transpose(pbb[:, j * 128:(j + 1) * 128], xin[k][:, c * 128:(c + 1) * 128], ident_b[:]),
                                     reads=[b_xin[k], b_const], writes=[PBb[bi]])
                            pv = pbb.rearrange("p (a b) -> p a b", a=8)
                            if g == 0:
                                S.op("act", lambda: A.copy(dst[:, 0:8, i * 128:(i + 1) * 128], pv), writes=[PBb[bi], bdst])
                            else:
                                S.op("dve", lambda: V.tensor_copy(dst[:, 8:16, i * 128:(i + 1) * 128], pv), writes=[PBb[bi], bdst])
                prefetch_w(OFF["fq"], 256)
                S.barrier()
                if stop == "p0":
                    S.dma("sp", dbg, xT[:].rearrange("p a b -> p (a b)"))
                    raise StopBuild()

            def proj_fm(c0, M, srcT, b_src, tok0, ntok, dst_fn, scale, wt, b_w, wc0, bank):
                for c in range(16):
                    S.op("pe", lambda: T.matmul(PB[bank][0:M, 0:ntok], wt[:, c, wc0:wc0 + M], srcT[:, c, tok0:tok0 + ntok],
                                                start=(c == 0), stop=(c == 15)),
                         reads=[b_w, b_src], writes=[PBb[bank]])
                dst, b_dst = dst_fn()
                if isinstance(dst, tuple):
                    S.op("act", lambda: A.activation(dst[0], PB[bank][0:64, 0:ntok], AF.Copy, scale=scale),
                         writes=[PBb[bank], b_dst])
                    S.op("act", lambda: A.activation(dst[1], PB[bank][64:128, 0:ntok], AF.Copy, scale=scale),
                         writes=[PBb[bank], b_dst])
                else:
                    S.op("act", lambda: A.activation(dst, PB[bank][0:M, 0:ntok], AF.Copy, scale=scale),
                         writes=[PBb[bank], b_dst])

            def proj_tm(srcT, b_src, tile, ncols, wt, b_w, wc0, bank):
                for c in range(16):
                    S.op("pe", lambda: T.matmul(PB[bank][:, 0:ncols], srcT[:, c, tile * 128:(tile + 1) * 128], wt[:, c, wc0:wc0 + ncols],
                                                start=(c == 0), stop=(c == 15)),
                         reads=[b_w, b_src], writes=[PBb[bank]])

            def nb():
                bank_rr[0] ^= 1
                return bank_rr[0]

            SBANKS = (2, 3, 0)

            def pipeline(iters, fa, fb, depth=2):
                N = len(iters)
                for n in range(min(depth, N)):
                    fa(iters[n])
                for n in range(N):
                    if n + depth < N:
                        fa(iters[n + depth])
                    fb(iters[n])

            def ystage_to_mixT(ystage, b_ys, base_chunk):
                pbb = PB[7][:].bitcast(BF16)
                for i in range(8):
                    for cc in range(4):
                        S.op("pe", lambda: T.transpose(pbb[:, cc * 128:(cc + 1) * 128], ystage[:, i, cc * 128:(cc + 1) * 128], ident_b[:]),
                             reads=[b_ys, b_const], writes=[PBb[7]])
                    S.op("dve", lambda: V.tensor_copy(mixT[:, base_chunk:base_chunk + 4, i * 128:(i + 1) * 128],
                                                      pbb[:, 0:512].rearrange("p (a b) -> p a b", a=4)),
                         writes=[PBb[7], b_mixT])

            with Alloc(nc) as al:
                fkT = al("fkT", [128, 4, 2 * NT], BF16)
                fvA = al("fvA", [128, 16, 8, 66], BF16)
                fqT = al("fqT", [128, 4, 2, NT], BF16)
                zf = al("zf", [128, 16, 8], F32)
                lsf = al("lsf", [128, 16, 8], F32)
                ls3 = al("ls3", [128, 16, 3, 8], BF16)
                tmpf = al("tmpf", [128, 16, 8], F32)
                cf = al("cf", [128, 16, 8], F32)
                negc = al("negc", [128, 16, 8], F32)
                cpad = al("cpad", [128, 8, 128], BF16)
                c3 = al("c3", [128, NT], BF16)
                PT0 = al("PT0", [128, 512], BF16)
                PT1 = al("PT1", [128, 512], BF16)
                PT2 = al("PT2", [128, 512], BF16)
                rec = al("rec", [128, 8], F32)
                ystage = al("ystage", [128, 8, 512], BF16)
                b_fkT, b_fvA, b_fqT, b_c, b_c3, b_rec, b_ys = [Buf(n) for n in "fkT fvA fqT c c3 rec ys".split()]
                PTs = [PT0, PT1, PT2]
                b_PT = [Buf("PT%d" % i) for i in range(3)]
                S.op("dve", lambda: V.memset(fvA[:, :, :, 64:65], 1.0), writes=[b_fvA])
                S.op("dve", lambda: V.memset(fqT[:], 0.0), writes=[b_fqT])
                for grp in range(2):
                    wt, b_w = load_w(OFF["fq"] + grp * 256, 256)
                    for sl in range(2):
                        pr = grp * 2 + sl
                        for tc in range(2):
                            proj_fm(0, 128, xT, b_xT, tc * 512, 512,
                                    lambda: ((fqT[0:64, pr, 0, tc * 512:(tc + 1) * 512], fqT[64:128, pr, 1, tc * 512:(tc + 1) * 512]), b_fqT),
                                    0.125, wt, b_w, sl * 128, nb())
                for grp in range(2):
                    wt, b_w = load_w(OFF["fk"] + grp * 256, 256)
                    for sl in range(2):
                        pr = grp * 2 + sl
                        for half, (src, bs) in enumerate(((xTp, b_xTp), (xT, b_xT))):
                            for tc in range(2):
                                proj_fm(0, 128, src, bs, tc * 512, 512,
                                        lambda: (fkT[:, pr, half * NT + tc * 512: half * NT + (tc + 1) * 512], b_fkT),
                                        1.0, wt, b_w, sl * 128, nb())
                for grp in range(2):
                    wt, b_w = load_w(OFF["fv"] + grp * 256, 256)
                    for kt in range(16):
                        src, bs = (xTp, b_xTp) if kt < 8 else (xT, b_xT)
                        bk = nb()
                        proj_tm(src, bs, kt % 8, 256, wt, b_w, 0, bk)
                        S.op("act", lambda: A.copy(fvA[:, kt, grp * 4:(grp + 1) * 4, 0:64],
                                                   PB[bk][:, 0:256].rearrange("p (a b) -> p a b", a=4)),
                             writes=[PBb[bk], b_fvA])
                wt, b_w = load_w(OFF["ff"] + 8 - 256, 256)
                for kt in range(16):
                    src, bs = (xTp, b_xTp) if kt < 8 else (xT, b_xT)
                    bk = nb()
                    proj_tm(src, bs, kt % 8, 8, wt, b_w, 248, bk)
                    S.op("dve", lambda: V.tensor_tensor(zf[:, kt, :], PB[bk][:, 0:8], bf_t, ALU.add),
                         reads=[b_small], writes=[PBb[bk], b_c])
                if stop == "fox_proj":
                    S.barrier()
                    S.dma("sp", dbg[:, 0:4 * NT], fqT[:].rearrange("p a b -> p (a b)"))
                    S.dma("sp", dbg[:, 4 * NT:12 * NT], fkT[:].rearrange("p a b -> p (a b)"))
                    raise StopBuild()
                S.op("dve", lambda: V.tensor_scalar(tmpf[:], zf[:], -1.0, None, ALU.mult), writes=[b_c])
                S.op("dve", lambda: V.tensor_tensor(tmpf[:], tmpf[:], zf[:], ALU.max), writes=[b_c])
                S.op("act", lambda: A.activation(tmpf[:], tmpf[:], AF.Exp, scale=-1.0), writes=[b_c])
                S.op("act", lambda: A.activation(tmpf[:], tmpf[:], AF.Ln, bias=1.0), writes=[b_c])
                S.op("dve", lambda: V.tensor_scalar(lsf[:], zf[:], 0.0, None, ALU.min), writes=[b_c])
                S.op("dve", lambda: V.tensor_tensor(lsf[:], lsf[:], tmpf[:], ALU.subtract), writes=[b_c])
                S.op("dve", lambda: V.tensor_copy(ls3[:, :, 0, :], lsf[:]), writes=[b_c])
                S.op("dve", lambda: V.tensor_tensor(tmpf[:], lsf[:], ls3[:, :, 0, :], ALU.subtract), writes=[b_c])
                S.op("dve", lambda: V.tensor_copy(ls3[:, :, 1, :], tmpf[:]), writes=[b_c])
                S.op("dve", lambda: V.tensor_tensor(tmpf[:], tmpf[:], ls3[:, :, 1, :], ALU.subtract), writes=[b_c])
                S.op("dve", lambda: V.tensor_copy(ls3[:, :, 2, :], tmpf[:]), writes=[b_c])
                first = True
                for kt in range(16):
                    for k2 in range(kt + 1):
                        lhs = tri if k2 == kt else ones
                        S.op("pe", lambda: T.matmul(PB[6][:, kt * 24:(kt + 1) * 24], lhs[:], ls3[:, k2, :, :], start=first, stop=False,
                                                    skip_group_check=True),
                             reads=[b_c, b_const], writes=[PBb[6]])
                        first = False
                p6v = PB[6][:, 0:384].rearrange("p (a k b) -> p a k b", a=16, k=3)
                S.op("dve", lambda: V.tensor_copy(cf[:], p6v[:, :, 0, :]), writes=[PBb[6], b_c])
                S.op("dve", lambda: V.tensor_tensor(cf[:], cf[:], p6v[:, :, 1, :], ALU.add), writes=[PBb[6], b_c])
                S.op("dve", lambda: V.tensor_tensor(cf[:], cf[:], p6v[:, :, 2, :], ALU.add), writes=[PBb[6], b_c])
                S.op("dve", lambda: V.tensor_scalar(negc[:, 0:8, :], cf[:, 0:8, :], -1.0, pm[:, 0:1], ALU.mult, ALU.add),
                     reads=[b_const], writes=[b_c])
                S.op("dve", lambda: V.tensor_scalar(negc[:, 8:16, :], cf[:, 8:16, :], -1.0, None, ALU.mult), writes=[b_c])
                S.op("dve", lambda: V.memset(cpad[:], 0.0), writes=[b_c3])
                cown = cf[:, 8:16, :]
                S.op("dve", lambda: V.tensor_copy(cpad[:, :, 0:8], cown), reads=[b_c], writes=[b_c3])
                S.op("dve", lambda: V.tensor_tensor(tmpf[:, 0:8, :], cown, cpad[:, :, 0:8], ALU.subtract), reads=[b_c], writes=[b_c3])
                S.op("dve", lambda: V.tensor_copy(cpad[:, :, 32:40], tmpf[:, 0:8, :]), writes=[b_c3])
                S.op("dve", lambda: V.tensor_tensor(tmpf[:, 0:8, :], tmpf[:, 0:8, :], cpad[:, :, 32:40], ALU.subtract), writes=[b_c3])
                S.op("dve", lambda: V.tensor_copy(cpad[:, :, 64:72], tmpf[:, 0:8, :]), writes=[b_c3])
                pb6b = PB[6][:].bitcast(BF16)
                for i in range(8):
                    S.op("pe", lambda: T.transpose(pb6b[:, i * 128:(i + 1) * 128], cpad[:, i, :], ident_b[:]),
                         reads=[b_c3, b_const], writes=[PBb[6]])
                S.op("dve", lambda: V.tensor_copy(c3[:], pb6b[:, :]), writes=[PBb[6], b_c3])
                if stop == "fox_c":
                    S.barrier()
                    S.dma("sp", dbg[:, 0:NT], c3[:, :])
                    raise StopBuild()
                def fox_A(itm):
                    n, h, cq, kt, nkt, grp = itm
                    pr, hh = h // 2, h % 2
                    rel = kt - (8 + 4 * cq)
                    t_lo = 0 if rel <= 0 else 128 * rel
                    sbk = SBANKS[n % 3]
                    pk = n % 3
                    q0 = cq * 512 + t_lo
                    S.op("pe", lambda: T.matmul(PB[sbk][:, t_lo:512], fkT[:, pr, kt * 128:(kt + 1) * 128], fqT[:, pr, hh, q0:cq * 512 + 512],
                                                start=True, stop=False, skip_group_check=True),
                         reads=[b_fkT, b_fqT], writes=[PBb[sbk]])
                    S.op("pe", lambda: T.matmul(PB[sbk][:, t_lo:512], sel_b[:, h, :], c3[:, q0:cq * 512 + 512],
                                                start=False, stop=(rel < 0), skip_group_check=True),
                         reads=[b_c3, b_const], writes=[PBb[sbk]])
                    if rel >= 0:
                        S.op("pe", lambda: T.matmul(PB[sbk][:, t_lo:t_lo + 128], ident_b[:], cmask_b[:],
                                                    start=False, stop=True, skip_group_check=True),
                             reads=[b_const], writes=[PBb[sbk]])
                    S.op("act", lambda: A.activation(PTs[pk][:, t_lo:512], PB[sbk][:, t_lo:512], AF.Exp, bias=negc[:, kt, h:h + 1]),
                         reads=[b_c], writes=[PBb[sbk], b_PT[pk]])

                def fox_B(itm):
                    n, h, cq, kt, nkt, grp = itm
                    rel = kt - (8 + 4 * cq)
                    t_lo = 0 if rel <= 0 else 128 * rel
                    pk = n % 3
                    ob = 4 + (grp % 2)
                    for qb in range(t_lo // 128, 4):
                        S.op("pe", lambda: T.matmul(PB[ob][:, qb * 65:qb * 65 + 65], PTs[pk][:, qb * 128:(qb + 1) * 128], fvA[:, kt, h, 0:65],
                                                    start=(kt == 0 and qb == 0), stop=False, skip_group_check=True),
                             reads=[b_PT[pk], b_fvA], writes=[PBb[ob]])
                    if kt == nkt - 1:
                        ov = PB[ob][:, 0:260].rearrange("p (a b) -> p a b", a=4)
                        S.op("dve", lambda: V.reciprocal(rec[:, 0:4], ov[:, :, 64]), writes=[PBb[ob], b_rec])
                        for qb in range(4):
                            S.op("dve", lambda: V.tensor_scalar(ystage[:, cq * 4 + qb, h * 64:(h + 1) * 64], ov[:, qb, 0:64],
                                                                rec[:, qb:qb + 1], None, ALU.mult),
                                 reads=[b_rec], writes=[PBb[ob], b_ys])

                iters = []
                for h in range(int(os.environ.get("FOXH", "8"))):
                    for cq in range(2):
                        nkt = 8 + 4 * cq + 4
                        for kt in range(nkt):
                            iters.append((len(iters), h, cq, kt, nkt, h * 2 + cq))
                pipeline(iters, fox_A, fox_B)
                if os.environ.get("FOXT", "1") == "1":
                    ystage_to_mixT(ystage, b_ys, 0)
                prefetch_w(OFF["dq"], 256)
                S.barrier()
                if stop == "fox":
                    S.dma("sp", dbg, mixT[:].rearrange("p a b -> p (a b)"))
                    raise StopBuild()

            with Alloc(nc) as al:
                dkT = al("dkT", [128, 4, 2 * NT], BF16)
                dvA = al("dvA", [128, 16, 4, 130], BF16)
                dqT = al("dqT", [128, 4, 2, NT], BF16)
                dPT0 = al("dPT0", [128, 2, 256], BF16)
                dPT1 = al("dPT1", [128, 2, 256], BF16)
                dPT2 = al("dPT2", [128, 2, 256], BF16)
                drec = al("drec", [128, 4], F32)
                dob = al("dob", [128, 128], F32)
                dsq = al("dsq", [128, 128], F32)
                dss = al("dss", [128, 8, 4], F32)
                y32 = al("y32", [128, 8, 512], F32)
                b_dss = Buf("dss")
                ystage = al("ystage_d", [128, 8, 512], BF16)
                b_dkT, b_dvA, b_dqT, b_rec, b_ys, b_ob = [Buf(n) for n in "dkT dvA dqT drec dys dob".split()]
                PTs = [dPT0, dPT1, dPT2]
                b_PT = [Buf("dPT%d" % i) for i in range(3)]
                S.op("dve", lambda: V.memset(dvA[:, :, :, 128:129], 1.0), writes=[b_dvA])
                S.op("dve", lambda: V.memset(dqT[:], 0.0), writes=[b_dqT])
                for grp in range(2):
                    wt, b_w = load_w(OFF["dq"] + grp * 256, 256)
                    for sl in range(2):
                        hd = grp * 2 + sl
                        for tc in range(2):
                            proj_fm(0, 128, xT, b_xT, tc * 512, 512,
                                    lambda: ((dqT[0:64, hd, 0, tc * 512:(tc + 1) * 512], dqT[64:128, hd, 1, tc * 512:(tc + 1) * 512]), b_dqT),
                                    0.125, wt, b_w, sl * 128, nb())
                for grp in range(2):
                    wt, b_w = load_w(OFF["dk"] + grp * 256, 256)
                    for sl in range(2):
                        hd = grp * 2 + sl
                        for half, (src, bs) in enumerate(((xTp, b_xTp), (xT, b_xT))):
                            for tc in range(2):
                                proj_fm(0, 128, src, bs, tc * 512, 512,
                                        lambda: (dkT[:, hd, half * NT + tc * 512: half * NT + (tc + 1) * 512], b_dkT),
                                        1.0, wt, b_w, sl * 128, nb())
                for grp in range(2):
                    wt, b_w = load_w(OFF["dv"] + grp * 256, 256)
                    for kt in range(16):
                        src, bs = (xTp, b_xTp) if kt < 8 else (xT, b_xT)
                        bk = nb()
                        proj_tm(src, bs, kt % 8, 256, wt, b_w, 0, bk)
                        S.op("act", lambda: A.copy(dvA[:, kt, grp * 2:(grp + 1) * 2, 0:128],
                                                   PB[bk][:, 0:256].rearrange("p (a b) -> p a b", a=2)),
                             writes=[PBb[bk], b_dvA])
                def diff_A(itm):
                    n, h, c2, kt, nkt, grp = itm
                    rel = kt - (8 + 2 * c2)
                    t_lo = 128 if rel == 1 else 0
                    sbk = SBANKS[n % 3]
                    pk = n % 3
                    q0 = c2 * 256 + t_lo
                    q1 = c2 * 256 + 256
                    Sv = PB[sbk][:].rearrange("p (m t) -> p m t", m=2)
                    for m in range(2):
                        S.op("pe", lambda: T.matmul(Sv[:, m, t_lo:256], dkT[:, h, kt * 128:(kt + 1) * 128], dqT[:, h, m, q0:q1],
                                                    start=(m == 0), stop=False, skip_group_check=True),
                             reads=[b_dkT, b_dqT], writes=[PBb[sbk]])
                    brange = {-1: (0, 128, 128, 256), 0: (0, 256, 0, 256), 1: (128, 256, 0, 128)}.get(rel)
                    if brange is not None:
                        o0, o1, r0, r1 = brange
                        for m in range(2):
                            S.op("pe", lambda: T.matmul(Sv[:, m, o0:o1], ident_b[:], DA2[:, h, m, r0:r1], start=False, stop=(m == 1),
                                                        skip_group_check=True), reads=[b_const], writes=[PBb[sbk]])
                    bias_ap = bcol[:, 1, h:h + 1] if kt < 8 else bcol[:, 0, h:h + 1]
                    S.op("act", lambda: A.activation(PTs[pk][:, :, t_lo:256], Sv[:, :, t_lo:256], AF.Exp, bias=bias_ap),
                         reads=[b_const], writes=[PBb[sbk], b_PT[pk]])

                def diff_B(itm):
                    n, h, c2, kt, nkt, grp = itm
                    rel = kt - (8 + 2 * c2)
                    t_lo = 128 if rel == 1 else 0
                    pk = n % 3
                    obs = (4, 5) if grp % 2 == 0 else (6, 7)
                    for qb in range(t_lo // 128, 2):
                        for m in range(2):
                            S.op("pe", lambda: T.matmul(PB[obs[qb]][:, m * 129:m * 129 + 129], PTs[pk][:, m, qb * 128:(qb + 1) * 128],
                                                        dvA[:, kt, h, 0:129], start=(kt == 0 and m == 0), stop=False, skip_group_check=True),
                                 reads=[b_PT[pk], b_dvA], writes=[PBb[obs[qb]]])
                    if kt == nkt - 1:
                        for qb in range(2):
                            ob = obs[qb]
                            ov = PB[ob][:, 0:258].rearrange("p (a b) -> p a b", a=2)
                            S.op("dve", lambda: V.reciprocal(drec[:, 0:2], ov[:, :, 128]), writes=[PBb[ob], b_rec])
                            S.op("dve", lambda: V.tensor_tensor(drec[:, 2:3], drec[:, 1:2], neglam, ALU.mult), reads=[b_small], writes=[b_rec])
                            S.op("dve", lambda: V.tensor_scalar(dob[:], ov[:, 0, 0:128], drec[:, 0:1], None, ALU.mult),
                                 reads=[b_rec], writes=[PBb[ob], b_ob])
                            S.op("dve", lambda: V.scalar_tensor_tensor(dob[:], ov[:, 1, 0:128], drec[:, 2:3], dob[:], ALU.mult, ALU.add),
                                 reads=[b_rec], writes=[PBb[ob], b_ob])
                            tl = c2 * 2 + qb
                            S.op("dve", lambda: V.scalar_tensor_tensor(dsq[:], dob[:], 1.0, dob[:], ALU.mult, ALU.mult, accum_out=dss[:, tl, h:h + 1]),
                                 reads=[b_ob], writes=[b_dss])
                            S.op("dve", lambda: V.tensor_copy(y32[:, tl, h * 128:(h + 1) * 128], dob[:]), reads=[b_ob], writes=[b_dss])

                iters = []
                for h in range(4):
                    for c2 in range(4):
                        nkt = 8 + 2 * c2 + 2
                        for kt in range(nkt):
                            iters.append((len(iters), h, c2, kt, nkt, h * 4 + c2))
                S.op("dve", lambda: V.memset(dss[:], 0.0), writes=[b_dss])
                pipeline(iters, diff_A, diff_B)
                S.op("dve", lambda: V.tensor_scalar(dss[:], dss[:], 1.0 / 128.0, LN_EPS, ALU.mult, ALU.add), writes=[b_dss])
                S.op("act", lambda: A.activation(dss[:], dss[:], AF.Ln), writes=[b_dss])
                S.op("act", lambda: A.activation(dss[:], dss[:], AF.Exp, scale=-0.5), writes=[b_dss])
                for tl in range(8):
                    for h in range(4):
                        S.op("dve", lambda: V.scalar_tensor_tensor(ystage[:, tl, h * 128:(h + 1) * 128], y32[:, tl, h * 128:(h + 1) * 128],
                                                                   dss[:, tl, h:h + 1], gsub[:], ALU.mult, ALU.mult),
                             reads=[b_dss, b_small], writes=[b_ys])
                ystage_to_mixT(ystage, b_ys, 4)
                prefetch_w(OFF["sq"], 256)
                S.barrier()
                if stop == "diff":
                    S.dma("sp", dbg, mixT[:].rearrange("p a b -> p (a b)"))
                    raise StopBuild()

            with Alloc(nc) as al:
                sqT = al("sqT", [128, 8, NT], BF16)
                wpad = al("wpad", [128, 16, 128], BF16)
                skT = al("skT", [128, NT + 128], BF16)
                svA = al("svA", [128, 9, 2, 66], BF16)
                ut = al("ut", [128, 9, 512], BF16)
                pw = al("pw", [128, 4, 128], BF16)
                sPT0 = al("sPT0", [128, 4, 128], BF16)
                sPT1 = al("sPT1", [128, 4, 128], BF16)
                sPT2 = al("sPT2", [128, 4, 128], BF16)
                pooledT = al("pooledT", [128, 512], BF16)
                srec = al("srec", [128, 4], F32)
                ystage = al("ystage_s", [128, 8, 512], BF16)
                b_sqT, b_skT, b_svA, b_ut, b_pw, b_rec, b_ys, b_pl = [Buf(n) for n in "sqT skT svA ut pw srec sys pl".split()]
                PTs = [sPT0, sPT1, sPT2]
                b_PT = [Buf("sPT0"), Buf("sPT1"), Buf("sPT2")]
                S.op("dve", lambda: V.memset(svA[:, :, :, 64:65], 1.0), writes=[b_svA])
                S.dma("pool", pw[:], pool_w[li].rearrange("g c d -> c g d"), writes=[b_pw])
                b_wpad = Buf("wpad")
                S.op("dve", lambda: V.memset(wpad[:], 0.0), writes=[b_wpad])
                for grp in range(2):
                    wt, b_w = load_w(OFF["sq"] + grp * 256, 256)
                    for sl in range(4):
                        hq = grp * 4 + sl
                        g = hq // 4
                        if grp == 1 and sl == 0:
                            S.op("dve", lambda: V.memset(wpad[:], 0.0), writes=[b_wpad])
                        S.op("dve", lambda: V.tensor_copy(wpad[:, :, g * 64:(g + 1) * 64], wt[:, :, sl * 64:(sl + 1) * 64]),
                             reads=[b_w], writes=[b_wpad])
                        for tc in range(2):
                            proj_fm(0, 128, xT, b_xT, tc * 512, 512,
                                    lambda: (sqT[:, hq, tc * 512:(tc + 1) * 512], b_sqT), 0.125, wpad, b_wpad, 0, nb())
                wt, b_w = load_w(OFF["sk"], 256)
                proj_fm(0, 128, xTp, b_xTp, 7 * 128, 128, lambda: (skT[:, 0:128], b_skT), 1.0, wt, b_w, 0, nb())
                for tc in range(2):
                    proj_fm(0, 128, xT, b_xT, tc * 512, 512,
                            lambda: (skT[:, 128 + tc * 512:128 + (tc + 1) * 512], b_skT), 1.0, wt, b_w, 0, nb())
                for kt in range(9):
                    src, bs, tl = (xTp, b_xTp, 7) if kt == 0 else (xT, b_xT, kt - 1)
                    bk = nb()
                    proj_tm(src, bs, tl, 128, wt, b_w, 128, bk)
                    S.op("act", lambda: A.copy(svA[:, kt, :, 0:64], PB[bk][:, 0:128].rearrange("p (a b) -> p a b", a=2)),
                         writes=[PBb[bk], b_svA])
                for grp in range(2):
                    wt, b_w = load_w(OFF["pu"] + grp * 256, 256)
                    for kt in range(9):
                        src, bs, tl = (xTp, b_xTp, 7) if kt == 0 else (xT, b_xT, kt - 1)
                        bk = nb()
                        proj_tm(src, bs, tl, 256, wt, b_w, 0, bk)
                        S.op("act", lambda: A.copy(ut[:, kt, grp * 256:(grp + 1) * 256], PB[bk][:, 0:256]), writes=[PBb[bk], b_ut])
                def swa_A(itm):
                    n, i, g, blk = itm
                    ktl = i + blk
                    sbk = SBANKS[n % 3]
                    pk = n % 3
                    S.op("pe", lambda: T.matmul(PB[sbk][:], skT[:, ktl * 128:(ktl + 1) * 128], sqT[:, g * 4:(g + 1) * 4, i * 128:(i + 1) * 128],
                                                start=True, stop=False, skip_group_check=True),
                         reads=[b_skT, b_sqT], writes=[PBb[sbk]])
                    S.op("pe", lambda: T.matmul(PB[sbk][:], ident_b[:], swaB_b[:, blk, g * 4:(g + 1) * 4, :].rearrange("p a b -> p (a b)"),
                                                start=False, stop=True, skip_group_check=True),
                         reads=[b_const], writes=[PBb[sbk]])
                    bias_ap = pm[:, 0:1] if (i == 0 and blk == 0) else zcol[:, 0:1]
                    S.op("act", lambda: A.activation(PTs[pk][:].rearrange("p a b -> p (a b)"), PB[sbk][:], AF.Exp, bias=bias_ap),
                         reads=[b_const], writes=[PBb[sbk], b_PT[pk]])

                def swa_B(itm):
                    n, i, g, blk = itm
                    ktl = i + blk
                    pk = n % 3
                    ob = 4 + ((n // 2) % 2)
                    for hh in range(4):
                        S.op("pe", lambda: T.matmul(PB[ob][:, hh * 65:hh * 65 + 65], PTs[pk][:, hh, :], svA[:, ktl, g, 0:65],
                                                    start=(blk == 0 and hh == 0), stop=False, skip_group_check=True),
                             reads=[b_PT[pk], b_svA], writes=[PBb[ob]])
                    if blk == 1:
                        ov = PB[ob][:, 0:260].rearrange("p (a b) -> p a b", a=4)
                        S.op("dve", lambda: V.tensor_tensor(srec[:], ov[:, :, 64], esink[:, g * 4:(g + 1) * 4], ALU.add),
                             reads=[b_small], writes=[PBb[ob], b_rec])
                        S.op("dve", lambda: V.reciprocal(srec[:], srec[:]), writes=[b_rec])
                        for hh in range(4):
                            hq = g * 4 + hh
                            S.op("dve", lambda: V.tensor_scalar(ystage[:, i, hq * 64:(hq + 1) * 64], ov[:, hh, 0:64], srec[:, hh:hh + 1], None, ALU.mult),
                                 reads=[b_rec], writes=[PBb[ob], b_ys])

                iters = []
                for i in range(8):
                    for g in range(2):
                        for blk in range(2):
                            iters.append((len(iters), i, g, blk))
                pipeline(iters, swa_A, swa_B)
                ystage_to_mixT(ystage, b_ys, 12)
                for g in range(4):
                    for tc in range(2):
                        bk = nb()
                        first = True
                        for j in range(4):
                            i = tc * 4 + j
                            kc, kp = (2, 3) if i == 0 else (0, 1)
                            S.op("pe", lambda: T.matmul(PB[bk][:, j * 128:(j + 1) * 128], ut[:, i + 1, g * 128:(g + 1) * 128], poolM[:, kc, g, :],
                                                        start=first, stop=False, skip_group_check=True),
                                 reads=[b_ut, b_const], writes=[PBb[bk]])
                            first = False
                            S.op("pe", lambda: T.matmul(PB[bk][:, j * 128:(j + 1) * 128], ut[:, i, g * 128:(g + 1) * 128], poolM[:, kp, g, :],
                                                        start=False, stop=False, skip_group_check=True),
                                 reads=[b_ut, b_const], writes=[PBb[bk]])
                        S.op("act", lambda: A.copy(pooledT[:], PB[bk][:]), writes=[PBb[bk], b_pl])
                        bk2 = nb()
                        S.op("pe", lambda: T.matmul(PB[bk2][:], pw[:, g, :], pooledT[:], start=True, stop=True),
                             reads=[b_pw, b_pl], writes=[PBb[bk2]])
                        S.op("dve", lambda: V.tensor_scalar(mixT[:, 8 + g, tc * 512:(tc + 1) * 512], PB[bk2][:], pscale[:, g:g + 1], None, ALU.mult),
                             reads=[b_small], writes=[PBb[bk2], b_mixT])
                S.barrier()
                if stop == "swa":
                    S.dma("sp", dbg, mixT[:].rearrange("p a b -> p (a b)"))
                    raise StopBuild()

        with Alloc(nc) as al:
            hbuf = al("hbuf", [128, 8, D], F32)
            stats = al("stats", [128, 4, 6], F32)
            mvA = al("mvA", [128, 8, 2], F32)
            rsA = al("rsA", [128, 8, 2], F32)
            gates = al("gates", [128, 8, 16], F32)
            b_h = [Buf("h%d" % i) for i in range(8)]
            b_lnw, b_ln, b_gates = Buf("lnw"), Buf("ln"), Buf("gates")

            with Alloc(nc) as al:
                wo0 = al("wo0", [128, 16, 256], BF16)
                wo1 = al("wo1", [128, 16, 256], BF16)
                lng = al("lng", [128, D], F32)
                lnb = al("lnb", [128, D], F32)
                xr0 = al("xr0", [128, 256], F32)
                xr1 = al("xr1", [128, 256], F32)
                xr2 = al("xr2", [128, 256], F32)
                hTf = al("hTf", [128, 16, 128], F32)
                wr_s = al("wr_s", [128, 16, 20], F32)
                rl = al("rl", [128, 20], F32)
                rt = al("rt", [128, 64], F32)
                wos = [wo0, wo1]
                b_wos = [Buf("wo0"), Buf("wo1")]
                xrs = [xr0, xr1, xr2]
                b_xrs = [Buf("xr%d" % i) for i in range(3)]
                b_hTf, b_wr, b_rl = Buf("hTf"), Buf("wr"), Buf("rl")
                S.dma("sp", lng[:], lnp[li, 0], writes=[b_lnw])
                S.dma("sp", lnb[:], lnp[li, 1], writes=[b_lnw])
                S.dma("sp", wr_s[:], w_r[li].rearrange("(c p) f -> p c f", p=128), writes=[b_wr])
                n = 0
                for cg in range(8):
                    k = cg % 2
                    S.dma("pool", wos[k][:], w_o[li][:, cg * 256:(cg + 1) * 256].rearrange("(c p) f -> p c f", p=128), writes=[b_wos[k]])
                    for i in range(8):
                        xk = n % 3
                        n += 1
                        S.dma("sp", xrs[xk][:], xo_f(i, cg * 256, (cg + 1) * 256), writes=[b_xrs[xk]])
                        bk = n % 4
                        for c in range(16):
                            S.op("pe", lambda: T.matmul(PB[bk][:, 0:256], mixT[:, c, i * 128:(i + 1) * 128], wos[k][:, c, :],
                                                        start=(c == 0), stop=(c == 15)),
                                 reads=[b_mixT, b_wos[k]], writes=[PBb[bk]])
                        S.op("dve", lambda: V.scalar_tensor_tensor(hbuf[:, i, cg * 256:(cg + 1) * 256], xrs[xk][:], ALPHA, PB[bk][:, 0:256],
                                                                   ALU.mult, ALU.add),
                             reads=[b_xrs[xk]], writes=[PBb[bk], b_h[i]])
                def p2_rest(i):

                    for g in range(4):
                        bi = 4 + (g % 2)
                        for j in range(4):
                            c = g * 4 + j
                            S.op("pe", lambda: T.transpose(PB[bi][:, j * 128:(j + 1) * 128], hbuf[:, i, c * 128:(c + 1) * 128], ident[:]),
                                 reads=[b_h[i], b_const], writes=[PBb[bi]])
                        pv = PB[bi][:].rearrange("p (a b) -> p a b", a=4)
                        S.op("act", lambda: A.copy(hTf[:, g * 4:(g + 1) * 4, :], pv), writes=[PBb[bi], b_hTf])
                        S.op("act", lambda: A.copy(hT[:, g * 4:(g + 1) * 4, i * 128:(i + 1) * 128], pv), writes=[PBb[bi], b_hT])
                    for c in range(16):
                        S.op("pe", lambda: T.matmul(PB[6][:, 0:20], hTf[:, c, :], wr_s[:, c, :], start=(c == 0), stop=(c == 15)),
                             reads=[b_hTf, b_wr], writes=[PBb[6]])
                    S.op("dve", lambda: V.tensor_tensor(rl[:], PB[6][:, 0:20], rb_t, ALU.add), reads=[b_small], writes=[PBb[6], b_rl])
                    gl, el = rl[:, 0:4], rl[:, 4:20]
                    gmax, gsum, pen, em, m1, k1, m2, k2, w2, den = (rt[:, 0:1], rt[:, 1:2], rt[:, 4:8], rt[:, 8:24], rt[:, 2:3],
                                                                    rt[:, 24:40], rt[:, 3:4], rt[:, 40:56], rt[:, 56:57], rt[:, 57:58])
                    ge = rt[:, 58:62]
                    ops = [
                        lambda: V.tensor_reduce(gmax, gl, AX.X, ALU.max),
                        lambda: V.tensor_scalar(pen, gl, gmax, None, ALU.is_equal),
                        lambda: V.tensor_scalar(pen, pen, -1.0, 1e30, ALU.add, ALU.mult),
                        lambda: V.tensor_scalar(ge, gl, gmax, None, ALU.subtract),
                    ]
                    for f in ops:
                        S.op("dve", f, writes=[b_rl])
                    S.op("dve", lambda: V.memset(gsum, 0.0), writes=[b_rl])
                    S.op("act", lambda: A.activation(ge, ge, AF.Exp, accum_out=gsum), writes=[b_rl])
                    ops = [
                        lambda: V.tensor_tensor(em.rearrange("p (g e) -> p g e", g=4), el.rearrange("p (g e) -> p g e", g=4),
                                                pen.unsqueeze(2).to_broadcast([128, 4, 4]), ALU.add),
                        lambda: V.tensor_reduce(m1, em, AX.X, ALU.max),
                        lambda: V.tensor_scalar(k1, em, m1, None, ALU.is_equal),
                        lambda: V.scalar_tensor_tensor(em, k1, -1e30, em, ALU.mult, ALU.add),
                        lambda: V.tensor_reduce(m2, em, AX.X, ALU.max),
                        lambda: V.tensor_scalar(k2, em, m2, None, ALU.is_equal),
                        lambda: V.tensor_tensor(w2, m2, m1, ALU.subtract),
                    ]
                    for f in ops:
                        S.op("dve", f, writes=[b_rl])
                    S.op("act", lambda: A.activation(w2, w2, AF.Exp), writes=[b_rl])
                    ops = [
                        lambda: V.tensor_scalar(den, w2, 1.0, ALPHA, ALU.add, ALU.mult),
                        lambda: V.tensor_tensor(den, den, gsum, ALU.mult),
                        lambda: V.reciprocal(den, den),
                        lambda: V.tensor_tensor(w2, w2, den, ALU.mult),
                        lambda: V.tensor_scalar(k1, k1, den, None, ALU.mult),
                        lambda: V.scalar_tensor_tensor(gates[:, i, :], k2, w2, k1, ALU.mult, ALU.add),
                    ]
                    for f in ops[:-1]:
                        S.op("dve", f, writes=[b_rl])
                    S.op("dve", ops[-1], reads=[b_rl], writes=[b_gates])
                ln_stats_all(hbuf, b_h, b_ln, stats, mvA, rsA, LN_EPS)
                for i in range(8):
                    ln_apply_tile(hbuf, i, lng, lnb, b_h[i], b_ln, rsA, b_lnw)
                    if i >= 1:
                        p2_rest(i - 1)
                p2_rest(7)
                S.barrier()
                if stop == "p2":
                    for i in range(8):
                        S.dma("sp", out_f(i), hbuf[:, i, :])
                    S.dma("sp", dbg, hT[:].rearrange("p a b -> p (a b)"))
                    raise StopBuild()

            with Alloc(nc) as al:
                gu0 = al("gu0", [128, 16, 256], BF16)
                gu1 = al("gu1", [128, 16, 256], BF16)
                gu2 = al("gu2", [128, 16, 256], BF16)
                gu3 = al("gu3", [128, 16, 256], BF16)
                hidT = al("hidT", [128, 4, NT], BF16)
                sa0 = al("sa0", [128, 512], F32)
                sa1 = al("sa1", [128, 512], F32)
                pT = al("pT", [128, 2, NT], BF16)
                puw = al("puw", [128, 2, D], BF16)
                pin = al("pin", [128, 256], F32)
                gus = [gu0, gu1, gu2, gu3]
                b_gus = [Buf("gu%d" % i) for i in range(4)]
                mflat = mixT[:].rearrange("p a b -> p (a b)")
                dws = [mflat[:, k * 8192:(k + 1) * 8192].rearrange("p (c d) -> p c d", c=4) for k in range(2)]
                b_dws = [Buf("dw0"), Buf("dw1")]
                sas = [sa0, sa1]
                b_sas = [Buf("sa0"), Buf("sa1")]
                b_hid = [Buf("hid%d" % i) for i in range(4)]
                b_pT, b_puw, b_pin = Buf("pT"), Buf("puw"), Buf("pin")
                gn = 0
                san = 0
                yb = 0
                for e in range(16):
                    for hf in range(2):
                        kg, ku = gn % 4, (gn + 1) % 4
                        gn += 2
                        S.dma("pool", gus[kg][:], w_gate[li, e][:, hf * 256:(hf + 1) * 256].rearrange("(c p) f -> p c f", p=128), writes=[b_gus[kg]])
                        S.dma("pool", gus[ku][:], w_up[li, e][:, hf * 256:(hf + 1) * 256].rearrange("(c p) f -> p c f", p=128), writes=[b_gus[ku]])
                        for fc in range(2):
                            f4 = hf * 2 + fc
                            for tc in range(2):
                                ba, bu = (0, 1) if (fc + tc) % 2 == 0 else (2, 3)
                                for c in range(16):
                                    S.op("pe", lambda: T.matmul(PB[ba][:], gus[kg][:, c, fc * 128:(fc + 1) * 128], hT[:, c, tc * 512:(tc + 1) * 512],
                                                                start=(c == 0), stop=(c == 15)),
                                         reads=[b_gus[kg], b_hT], writes=[PBb[ba]])
                                for c in range(16):
                                    S.op("pe", lambda: T.matmul(PB[bu][:], gus[ku][:, c, fc * 128:(fc + 1) * 128], hT[:, c, tc * 512:(tc + 1) * 512],
                                                                start=(c == 0), stop=(c == 15)),
                                         reads=[b_gus[ku], b_hT], writes=[PBb[bu]])
                                sk_ = san % 2
                                san += 1
                                S.op("act", lambda: A.activation(sas[sk_][:], PB[ba][:], AF.Silu), writes=[PBb[ba], b_sas[sk_]])
                                S.op("dve", lambda: V.tensor_tensor(hidT[:, f4, tc * 512:(tc + 1) * 512], sas[sk_][:], PB[bu][:], ALU.mult),
                                     reads=[b_sas[sk_]], writes=[PBb[bu], b_hid[f4]])
                    kd = e % 2
                    S.dma("pool", dws[kd], w_down[li, e].rearrange("(c p) d -> p c d", p=128), writes=[b_dws[kd]])
                    for i in range(8):
                        for cg in range(4):
                            bk = 4 + (yb % 4)
                            yb += 1
                            for fc in range(4):
                                S.op("pe", lambda: T.matmul(PB[bk][:], hidT[:, fc, i * 128:(i + 1) * 128], dws[kd][:, fc, cg * 512:(cg + 1) * 512],
                                                            start=(fc == 0), stop=(fc == 3)),
                                     reads=[b_hid[fc], b_dws[kd]], writes=[PBb[bk]])
                            S.op("dve", lambda: V.scalar_tensor_tensor(hbuf[:, i, cg * 512:(cg + 1) * 512], PB[bk][:], gates[:, i, e:e + 1],
                                                                       hbuf[:, i, cg * 512:(cg + 1) * 512], ALU.mult, ALU.add),
                                 reads=[b_gates], writes=[PBb[bk], b_h[i]])
                S.dma("pool", puw[:], ple_uw[li].rearrange("(c p) d -> p c d", p=128), writes=[b_puw])
                for i in range(8):
                    S.dma("sp", pin[:], p_own[li, i * 128:(i + 1) * 128, :], writes=[b_pin])
                    for k in range(2):
                        S.op("pe", lambda: T.transpose(PB[0][:, k * 128:(k + 1) * 128], pin[:, k * 128:(k + 1) * 128], ident[:]),
                             reads=[b_pin, b_const], writes=[PBb[0]])
                    S.op("act", lambda: A.copy(pT[:, :, i * 128:(i + 1) * 128], PB[0][:, 0:256].rearrange("p (a b) -> p a b", a=2)),
                         writes=[PBb[0], b_pT])
                for cg in range(4):
                    kd = cg % 2
                    wv = mflat[:, kd * 8192:(kd + 1) * 8192].rearrange("p (c f) -> p c f", c=16)
                    S.dma("pool", wv, ple_gw[li][:, cg * 512:(cg + 1) * 512].rearrange("(c p) f -> p c f", p=128), writes=[b_dws[kd]])
                    for i in range(8):
                        ba, bu = (0, 1) if i % 2 == 0 else (2, 3)
                        for c in range(16):
                            S.op("pe", lambda: T.matmul(PB[ba][:], hT[:, c, i * 128:(i + 1) * 128], wv[:, c, :], start=(c == 0), stop=(c == 15)),
                                 reads=[b_hT, b_dws[kd]], writes=[PBb[ba]])
                        for k in range(2):
                            S.op("pe", lambda: T.matmul(PB[bu][:], pT[:, k, i * 128:(i + 1) * 128], puw[:, k, cg * 512:(cg + 1) * 512],
                                                        start=(k == 0), stop=(k == 1)),
                                 reads=[b_pT, b_puw], writes=[PBb[bu]])
                        sk_ = san % 2
                        san += 1
                        S.op("act", lambda: A.activation(sas[sk_][:], PB[ba][:], AF.Sigmoid), writes=[PBb[ba], b_sas[sk_]])
                        S.op("dve", lambda: V.scalar_tensor_tensor(sas[sk_][:], sas[sk_][:], 1.0 / ALPHA, PB[bu][:], ALU.mult, ALU.mult), writes=[PBb[bu], b_sas[sk_]])
                        S.op("dve", lambda: V.tensor_tensor(hbuf[:, i, cg * 512:(cg + 1) * 512], hbuf[:, i, cg * 512:(cg + 1) * 512], sas[sk_][:], ALU.add),
                             reads=[b_sas[sk_]], writes=[b_h[i]])
                S.barrier()

            with Alloc(nc) as al:
                lng = al("lng2", [128, D], F32)
                lnb = al("lnb2", [128, D], F32)
                S.dma("sp", lng[:], lnp[li, 2], writes=[b_lnw])
                S.dma("sp", lnb[:], lnp[li, 3], writes=[b_lnw])
                ln_stats_all(hbuf, b_h, b_ln, stats, mvA, rsA, LN_EPS / (ALPHA * ALPHA))
                for i in range(8):
                    ln_apply_tile(hbuf, i, lng, lnb, b_h[i], b_ln, rsA, b_lnw)
                    ev = S.dma("sp", out_f(i), hbuf[:, i, :], reads=[b_h[i]])
                    if post_out is not None:
                        post_out(i, hbuf[:, i, :], b_h[i])
                S.barrier()

    xo0 = rows_f(x_own)
    xp0 = lambda i: x_prev[i * 128:(i + 1) * 128, :]
    yo = lambda i: y_out[i * 128:(i + 1) * 128, :]
    if nL == 1:
        try:
            layer(0, layers[0], xo0, xp0, yo, actA, b_actA, actB, b_actB)
        except StopBuild:
            pass
    else:
        for i in range(8):
            S.sems[("cc", i)] = S._sem("cc%d" % i)
        b_xall = [Buf("xall%d" % i) for i in range(8)]

        def exchange_tile(i, h_ap, b_hi):
            ev = S.dma("pool", xmid_b[i].ap(), h_ap, reads=[b_hi])
            S._wait("pool", {ev[0]: ev[1]})
            P.collective_compute("AllGather", ALU.bypass, replica_groups=[[0, 1], [2, 3], [4, 5], [6, 7]],
                                 ins=[xmid_b[i].ap().opt()], outs=[xall_b[i].ap().opt()]).then_inc(S.sems[("cc", i)], 1)
            b_xall[i].last_w = (("cc", i), 1)

        layer(0, layers[0], xo0, xp0, lambda i: xmid[i].ap(), actA, b_actA, actB, b_actB, post_out=exchange_tile)
        for i in range(8):
            S.dma_last[("cc", i)] = 1
        layer(1, layers[1], lambda i, c0, c1: xmid[i].ap()[:, c0:c1], lambda i: xall_b[i].ap()[0:128, :], yo,
              actA, b_actA, actB, b_actB, xp_b=b_xall, xo_T_f=lambda i: xmid_b[i].ap())
    S.barrier()
    S.close()
    for cm in reversed(es):
        cm.__exit__(None, None, None)
    return nc


def _prep_shared(inputs, layers):
    f = lambda a: np.ascontiguousarray(np.asarray(a, dtype=np.float32))
    L = list(layers)
    sh = {}
    sh["w_in"] = f(inputs["w_in"][L])
    sh["w_o"] = f(inputs["w_o"][L])
    sh["w_gate"] = f(inputs["w_gate"][L])
    sh["w_up"] = f(inputs["w_up"][L])
    sh["w_down"] = f(inputs["w_down"][L])
    sh["ple_gw"] = f(inputs["ple_gate_w"][L])
    sh["ple_uw"] = f(inputs["ple_up_w"][L])
    sh["pool_w"] = f(inputs["pool_w"][L])
    sh["w_r"] = f(np.concatenate([inputs["router_g_w"][L], inputs["router_e_w"][L]], axis=-1))
    nL = len(L)
    lnp = np.empty((nL, 4, 128, D), np.float32)
    small = np.zeros((nL, 128, 448), np.float32)
    for j, l in enumerate(L):
        for k, nm in enumerate(("ln1_g", "ln1_b", "ln2_g", "ln2_b")):
            lnp[j, k] = np.broadcast_to(inputs[nm][l][None, :], (128, D))
        small[j, :, 0:8] = inputs["b_f"][l][None, :]
        small[j, :, 8:16] = inputs["sinks"][l][None, :]
        for k, nm in enumerate(("lam_q1", "lam_k1", "lam_q2", "lam_k2")):
            small[j, :, 16 + 64 * k:16 + 64 * (k + 1)] = inputs[nm][l][None, :]
        small[j, :, 272:400] = inputs["diff_norm_g"][l][None, :]
        small[j, :, 400:404] = inputs["pool_scale"][l].reshape(4, 128).T
        small[j, :, 404:408] = inputs["router_g_b"][l][None, :]
        small[j, :, 408:424] = inputs["router_e_b"][l][None, :]
    sh["lnp"] = lnp
    sh["small"] = small
    cs = _static_consts()
    diffB, swaB, b31 = _bias_tiles(np.asarray(inputs["rel_table"], np.float32))
    for nm in ("ident", "cmask", "tri", "ones", "sel", "poolAc", "poolAp"):
        sh["c_" + nm] = cs[nm]
    sh["c_diffB"], sh["c_swaB"], sh["c_b31"] = diffB, swaB, b31
    return sh, cs


_PROG_CACHE = {}


def _run(layers, fused, x_full, inputs):
    key = (tuple(layers), fused)
    if key not in _PROG_CACHE:
        _PROG_CACHE[key] = build_program(list(layers), fused)
    nc = _PROG_CACHE[key]
    sh, cs = _prep_shared(inputs, layers)
    p = np.asarray(inputs["p"], np.float32)
    in_maps = []
    for c in range(8):
        b, half = c // 2, c % 2
        m = dict(sh)
        m["x_own"] = np.ascontiguousarray(x_full[b, half * NT:(half + 1) * NT])
        m["x_prev"] = np.ascontiguousarray(x_full[b, 0:NT])
        m["p_own"] = np.ascontiguousarray(p[list(layers), b, half * NT:(half + 1) * NT])
        m["c_pm"] = np.full((128, 1), 0.0 if half == 1 else NEG, np.float32)
        m["c_poolA0"] = cs["poolAc"] if half == 1 else cs["poolAf"]
        m["c_poolAp0"] = cs["poolAp"] if half == 1 else np.zeros_like(cs["poolAp"])
        in_maps.append(m)
    res = run_bass_kernel_spmd(nc, in_maps, core_ids=list(range(8)))
    out = np.empty((4, SEQ, D), np.float32)
    for c in range(8):
        b, half = c // 2, c % 2
        out[b, half * NT:(half + 1) * NT] = res.results[c]["y_out"]
    return out


def kernel(**inputs):
    x = np.asarray(inputs["x"], np.float32)
    if FUSED:
        return _run((0, 1), True, x, inputs)
    x1 = _run((0,), False, x, inputs)
    return _run((1,), False, x1, inputs)
```
